# Optimizing a Trainium2 kernel written in Bass

```python
import jax, jax.numpy as jnp
from jax import lax
import numpy as np


D_MODEL = 2048
BATCH = 1
SEQ = 8192
DEPTH = 4

GRID_W = 64
CTX_LEN = 256
EPS = 1e-6
ROPE_THETA = 10000.0

HEAD_DIM = 128
ATTN_HEADS = 8
ATTN_KV_HEADS = 2
ATTN_WIDTH = ATTN_HEADS * HEAD_DIM
KV_WIDTH = ATTN_KV_HEADS * HEAD_DIM
Q_BLOCK = 128

GLA_HEADS = 4
GLA_VALUE_WIDTH = D_MODEL - ATTN_WIDTH
GLA_KEY_WIDTH = GLA_VALUE_WIDTH // 2
GLA_DK = GLA_KEY_WIDTH // GLA_HEADS
GLA_DV = GLA_VALUE_WIDTH // GLA_HEADS
GLA_GATE_RANK = 16
GLA_GATE_TEMP = 16.0
GLA_CHUNK = 64

MIX_WIDTH = ATTN_WIDTH + GLA_VALUE_WIDTH
IN_WIDTH = ATTN_WIDTH + 2 * KV_WIDTH + 2 * GLA_KEY_WIDTH + 2 * GLA_VALUE_WIDTH + 2 * GLA_GATE_RANK

N_EXPERTS = 16
EXPERT_FF = 3 * D_MODEL // 4
CAPACITY_FACTOR = 2

kernel_name = 'hybrid_gqa_gla_ecmoe_diffusion_trunk'


def rmsnorm(x, g):
    x32 = x.astype(jnp.float32)
    y = x32 * lax.rsqrt(jnp.mean(x32 * x32, axis=-1, keepdims=True) + EPS)
    return (y * g.astype(jnp.float32)).astype(x.dtype)


def modulation(cvec, w_mod, b_mod):
    m = jax.nn.silu(cvec) @ w_mod + b_mod
    return jnp.split(m[:, None, :], 6, axis=-1)


def modulate(x, g, shift, scale):
    return rmsnorm(x, g) * (1.0 + scale) + shift


def axial_rope(rows, dtype):
    row = jnp.repeat(jnp.arange(rows, dtype=jnp.float32), GRID_W)
    col = jnp.tile(jnp.arange(GRID_W, dtype=jnp.float32), rows)
    half = HEAD_DIM // 2
    inv_freq = ROPE_THETA ** (-jnp.arange(0, half, 2, dtype=jnp.float32) / half)
    ang = jnp.concatenate([row[:, None] * inv_freq, col[:, None] * inv_freq], axis=-1)
    return jnp.cos(ang).astype(dtype), jnp.sin(ang).astype(dtype)


def apply_rope(x, cos, sin):
    x2 = x.reshape(x.shape[:-1] + (HEAD_DIM // 2, 2))
    x0, x1 = x2[..., 0], x2[..., 1]
    c = cos[None, :, None, :].astype(x.dtype)
    s = sin[None, :, None, :].astype(x.dtype)
    return jnp.stack([x0 * c - x1 * s, x0 * s + x1 * c], axis=-1).reshape(x.shape)


def mixer_inputs(h, w_in, q_gain, k_gain, w_a2, b_a, rope):
    B, L, _ = h.shape
    widths = [ATTN_WIDTH, KV_WIDTH, KV_WIDTH, GLA_KEY_WIDTH, GLA_KEY_WIDTH,
              GLA_VALUE_WIDTH, GLA_VALUE_WIDTH, GLA_GATE_RANK, GLA_GATE_RANK]
    offsets = np.cumsum(widths)[:-1].tolist()
    p = h @ w_in
    q, k, v, gq, gk, gv, r, af, ab = jnp.split(p, offsets, axis=-1)
    q = rmsnorm(q.reshape(B, L, ATTN_HEADS, HEAD_DIM), q_gain)
    k = rmsnorm(k.reshape(B, L, ATTN_KV_HEADS, HEAD_DIM), k_gain)
    v = v.reshape(B, L, ATTN_KV_HEADS, HEAD_DIM)
    if rope is not None:
        q = apply_rope(q, *rope)
        k = apply_rope(k, *rope)
    gq = gq.reshape(B, L, GLA_HEADS, GLA_DK) * (GLA_DK ** -0.5)
    gk = gk.reshape(B, L, GLA_HEADS, GLA_DK)
    gv = gv.reshape(B, L, GLA_HEADS, GLA_DV)
    la_f = jax.nn.log_sigmoid((af @ w_a2[0] + b_a[0]).astype(jnp.float32)) / GLA_GATE_TEMP
    la_b = jax.nn.log_sigmoid((ab @ w_a2[1] + b_a[1]).astype(jnp.float32)) / GLA_GATE_TEMP
    la_f = la_f.reshape(B, L, GLA_HEADS, GLA_DK)
    la_b = la_b.reshape(B, L, GLA_HEADS, GLA_DK)
    return (q, k, v), (gq, gk, gv, la_f, la_b), r


def softmax_attend(qb, k, v):
    s = jnp.einsum('bqkgd,bskd->bkgqs', qb, k, preferred_element_type=jnp.float32) * (HEAD_DIM ** -0.5)
    p = jax.nn.softmax(s, axis=-1).astype(v.dtype)
    return jnp.einsum('bkgqs,bskd->bqkgd', p, v)


def attention_group(lat, ctx, with_ctx_out):
    q, k, v = lat
    cq, ck, cv = ctx
    B, T = q.shape[:2]
    G = ATTN_HEADS // ATTN_KV_HEADS
    k_all = jnp.concatenate([ck, k], axis=1)
    v_all = jnp.concatenate([cv, v], axis=1)
    qb = q.reshape(B, T // Q_BLOCK, Q_BLOCK, ATTN_KV_HEADS, G, HEAD_DIM).swapaxes(0, 1)
    o = lax.map(lambda blk: softmax_attend(blk, k_all, v_all), qb)
    o_lat = o.swapaxes(0, 1).reshape(B, T, ATTN_WIDTH)
    o_ctx = None
    if with_ctx_out:
        C = cq.shape[1]
        o_ctx = softmax_attend(cq.reshape(B, C, ATTN_KV_HEADS, G, HEAD_DIM), ck, cv).reshape(B, C, ATTN_WIDTH)
    return o_lat, o_ctx


def gla_scan(q, k, v, log_a, s0):
    B, L, H, dk = q.shape
    dv = v.shape[-1]
    nc = L // GLA_CHUNK

    def to_chunks(a):
        return a.reshape(B, nc, GLA_CHUNK, H, a.shape[-1]).transpose(1, 0, 3, 2, 4)

    qc, kc, vc, ac = to_chunks(q), to_chunks(k), to_chunks(v), to_chunks(log_a)
    mask = jnp.tril(jnp.ones((GLA_CHUNK, GLA_CHUNK), dtype=bool))

    def step(S, inp):
        qi, ki, vi, ai = inp
        qi = qi.astype(jnp.float32)
        ki = ki.astype(jnp.float32)
        vi = vi.astype(jnp.float32)
        b = jnp.cumsum(ai, axis=2)
        diff = b[:, :, :, None, :] - b[:, :, None, :, :]
        decay = jnp.exp(jnp.where(mask[:, :, None], diff, -jnp.inf))
        A = jnp.einsum('bhid,bhjd,bhijd->bhij', qi, ki, decay)
        o = jnp.einsum('bhij,bhjv->bhiv', A, vi) + jnp.einsum('bhid,bhdv->bhiv', qi * jnp.exp(b), S)
        b_last = b[:, :, -1, :]
        S_new = jnp.exp(b_last)[..., None] * S + jnp.einsum(
            'bhjd,bhjv->bhdv', ki * jnp.exp(b_last[:, :, None, :] - b), vi)
        return S_new, o

    S_final, o = lax.scan(step, s0, (qc, kc, vc, ac))
    o = o.transpose(1, 0, 3, 2, 4).reshape(B, L, H, dv).astype(v.dtype)
    return o, S_final


def bidirectional_gla(lat, ctx):
    gq, gk, gv, la_f, la_b = lat
    cq, ck, cv, cla_f, cla_b = ctx
    B = gq.shape[0]
    s0 = jnp.zeros((B, GLA_HEADS, GLA_DK, GLA_DV), jnp.float32)
    flip = lambda a: jnp.flip(a, axis=1)
    o_cf, s_cf = gla_scan(cq, ck, cv, cla_f, s0)
    o_lf, _ = gla_scan(gq, gk, gv, la_f, s_cf)
    o_cb, s_cb = gla_scan(flip(cq), flip(ck), flip(cv), flip(cla_b), s0)
    o_lb, _ = gla_scan(flip(gq), flip(gk), flip(gv), flip(la_b), s_cb)
    return o_lf + flip(o_lb), o_cf + flip(o_cb)


def mixer_output(attn_o, gla_o, r, gla_gain, w_out):
    B, L = attn_o.shape[:2]
    gla = rmsnorm(gla_o, gla_gain).reshape(B, L, GLA_VALUE_WIDTH) * jax.nn.silu(r)
    return jnp.concatenate([attn_o, gla], axis=-1) @ w_out


def expert_choice_ffn(h, w_router, w_gate, w_up, w_down):
    B, L, D = h.shape
    cap = CAPACITY_FACTOR * L // N_EXPERTS
    aff = jax.nn.softmax((h @ w_router).astype(jnp.float32), axis=-1)
    gate_vals, idx = lax.top_k(jnp.swapaxes(aff, 1, 2), cap)
    flat_idx = idx.reshape(B, N_EXPERTS * cap)
    xg = jnp.take_along_axis(h, flat_idx[..., None], axis=1).reshape(B, N_EXPERTS, cap, D)
    hid = jax.nn.silu(jnp.einsum('becd,edf->becf', xg, w_gate)) * jnp.einsum('becd,edf->becf', xg, w_up)
    out = jnp.einsum('becf,efd->becd', hid, w_down) * gate_vals[..., None].astype(h.dtype)
    return jax.vmap(lambda i, o: jnp.zeros((L, D), h.dtype).at[i].add(o))(
        flat_idx, out.reshape(B, N_EXPERTS * cap, D))


def setup_inputs(seed: int = 0) -> dict:
    key = jax.random.key(seed)
    ks = jax.random.split(key, 20)
    f32 = jnp.float32

    def nrm(k, shape, scale):
        return jax.random.normal(k, shape, f32) * scale

    def gain(k, shape):
        return 1.0 + 0.02 * jax.random.normal(k, shape, f32)

    return {
        'x': nrm(ks[0], (BATCH, SEQ, D_MODEL), 1.0),
        'c': nrm(ks[1], (BATCH, D_MODEL), 1.0),
        'ctx': nrm(ks[2], (BATCH, CTX_LEN, D_MODEL), 1.0),
        'c_ctx': nrm(ks[3], (D_MODEL,), 1.0),
        'w_mod': nrm(ks[4], (DEPTH, D_MODEL, 6 * D_MODEL), 0.5 * D_MODEL ** -0.5),
        'b_mod': nrm(ks[5], (DEPTH, 6 * D_MODEL), 0.02),
        'norm_mix': gain(ks[6], (DEPTH, D_MODEL)),
        'w_in': nrm(ks[7], (DEPTH, D_MODEL, IN_WIDTH), D_MODEL ** -0.5),
        'q_gain': gain(ks[8], (DEPTH, HEAD_DIM)),
        'k_gain': gain(ks[9], (DEPTH, HEAD_DIM)),
        'w_gla_a2': nrm(ks[10], (DEPTH, 2, GLA_GATE_RANK, GLA_KEY_WIDTH), GLA_GATE_RANK ** -0.5),
        'b_gla_a': nrm(ks[11], (DEPTH, 2, GLA_KEY_WIDTH), 0.1),
        'gla_gain': gain(ks[12], (DEPTH, GLA_DV)),
        'w_out': nrm(ks[13], (DEPTH, MIX_WIDTH, D_MODEL), MIX_WIDTH ** -0.5),
        'norm_ffn': gain(ks[14], (DEPTH, D_MODEL)),
        'w_router': nrm(ks[15], (DEPTH, D_MODEL, N_EXPERTS), D_MODEL ** -0.5),
        'w_gate': nrm(ks[16], (DEPTH, N_EXPERTS, D_MODEL, EXPERT_FF), D_MODEL ** -0.5),
        'w_up': nrm(ks[17], (DEPTH, N_EXPERTS, D_MODEL, EXPERT_FF), D_MODEL ** -0.5),
        'w_down': nrm(ks[18], (DEPTH, N_EXPERTS, EXPERT_FF, D_MODEL), EXPERT_FF ** -0.5),
        'final_norm': gain(ks[19], (D_MODEL,)),
    }


def reference(x, c, ctx, c_ctx, w_mod, b_mod, norm_mix, w_in, q_gain, k_gain, w_gla_a2, b_gla_a,
              gla_gain, w_out, norm_ffn, w_router, w_gate, w_up, w_down, final_norm):
    T = x.shape[1]
    rows = T // GRID_W
    rope = axial_rope(rows, x.dtype)
    xc = ctx
    for l in range(DEPTH):
        last = l == DEPTH - 1
        sh1, sc1, g1, sh2, sc2, g2 = modulation(c, w_mod[l], b_mod[l])
        csh1, csc1, cg1, csh2, csc2, cg2 = modulation(c_ctx[None, :], w_mod[l], b_mod[l])
        lat_attn, lat_gla, lat_r = mixer_inputs(modulate(x, norm_mix[l], sh1, sc1), w_in[l], q_gain[l],
                                                k_gain[l], w_gla_a2[l], b_gla_a[l], rope)
        ctx_attn, ctx_gla, ctx_r = mixer_inputs(modulate(xc, norm_mix[l], csh1, csc1), w_in[l], q_gain[l],
                                                k_gain[l], w_gla_a2[l], b_gla_a[l], None)
        a_lat, a_ctx = attention_group(lat_attn, ctx_attn, not last)
        o_lat, o_ctx = bidirectional_gla(lat_gla, ctx_gla)
        x = x + g1 * mixer_output(a_lat, o_lat, lat_r, gla_gain[l], w_out[l])
        x = x + g2 * expert_choice_ffn(modulate(x, norm_ffn[l], sh2, sc2), w_router[l], w_gate[l],
                                       w_up[l], w_down[l])
        if not last:
            xc = xc + cg1 * mixer_output(a_ctx, o_ctx, ctx_r, gla_gain[l], w_out[l])
            xc = xc + cg2 * expert_choice_ffn(modulate(xc, norm_ffn[l], csh2, csc2), w_router[l],
                                              w_gate[l], w_up[l], w_down[l])
    return rmsnorm(x, final_norm)
```

```python
import numpy as np
from contextlib import ExitStack
import concourse.bass as bass
import concourse.mybir as mybir
from concourse.bass_utils import run_bass_kernel_spmd

F32 = mybir.dt.float32
BF16 = mybir.dt.bfloat16
I32 = mybir.dt.int32
U32 = mybir.dt.uint32
AF = mybir.ActivationFunctionType
ALU = mybir.AluOpType
AX = mybir.AxisListType

ENGINES = ["sync", "scalar", "vector", "gpsimd", "tensor"]
SEM_EPOCH = 20000
NDSLOT = 6


class Prog:
    def __init__(self, nc, same_engine_sync=True):
        self.nc = nc
        self.stack = ExitStack()
        self.items = {e: [] for e in ENGINES}
        self.cur = {}
        self.sems = {}
        self.known = {e: {} for e in ENGINES}
        self.lastw = {}
        self.readers = {}
        self.same_engine_sync = same_engine_sync
        self.final = {}
        self.nsem = 0
        self.drr = {}

    def const_eps(self):
        if not hasattr(self, "_epsb"):
            self._epsb = self.sb("epsb", [128, 1], F32)
            self.op("gpsimd", lambda e: e.memset(self._epsb[:], EPS), writes=[self._epsb])
        return self._epsb

    def reg(self, e, val):
        if not hasattr(self, "_regs"):
            self._regs = {}
        if val not in self._regs:
            self._regs[val] = e.to_reg(val)
        return self._regs[val]

    def sb(self, name, shape, dt):
        return self.stack.enter_context(self.nc.sbuf_tensor(name, shape, dt))

    def ps(self, name, shape, dt):
        return self.stack.enter_context(self.nc.psum_tensor(name, shape, dt))

    def _sem(self, key):
        if key not in self.sems:
            self.nsem += 1
            self.sems[key] = self.stack.enter_context(
                self.nc.semaphore("s_%s_%s_%d" % key))
        return self.sems[key]

    def _bump(self, kind, eng, amount):
        st = self.cur.setdefault((kind, eng), [0, 0])
        if st[1] + amount > SEM_EPOCH:
            st[0] += 1
            st[1] = 0
        st[1] += amount
        key = (kind, eng, st[0])
        self._sem(key)
        self.final[key] = st[1]
        return key, st[1]

    @staticmethod
    def _k(x):
        if isinstance(x, (str, int)):
            return x
        if isinstance(x, tuple):
            return tuple(Prog._k(y) for y in x)
        return x.name

    def _deps(self, eng, reads, writes):
        reads = [self._k(r) for r in reads]
        writes = [self._k(w) for w in writes]
        need = {}

        def add(dep):
            k, c = dep
            if need.get(k, 0) < c:
                need[k] = c
        for r in reads:
            if r in self.lastw:
                add(self.lastw[r])
        for w in writes:
            if w in self.lastw:
                add(self.lastw[w])
            for d in self.readers.get(w, ()):
                add(d)
        waits = []
        for k, c in need.items():
            kind, e2, _ = k
            if kind == "c" and e2 == eng:
                if eng == "tensor" or not self.same_engine_sync:
                    continue
            if self.known[eng].get(k, 0) >= c:
                continue
            self.known[eng][k] = c
            waits.append((k, c))
        return waits

    def _record(self, reads, writes, dep):
        reads = [self._k(r) for r in reads]
        writes = [self._k(w) for w in writes]
        for r in reads:
            self.readers.setdefault(r, []).append(dep)
        for w in writes:
            self.lastw[w] = dep
            self.readers[w] = []

    def op(self, eng, fn, reads=(), writes=()):
        waits = self._deps(eng, reads, writes)
        key, c = self._bump("c", eng, 1)
        self.items[eng].append((waits, fn, key, 1))
        self._record(reads, writes, (key, c))

    def _dma_slot(self, eng, waits):
        rr = self.drr.get(eng, 0)
        self.drr[eng] = (rr + 1) % NDSLOT
        slot = "%s%d" % (eng, rr)
        st = self.cur.setdefault(("d", slot), [0, 0])
        if st[1] > 0:
            pk = ("d", slot, st[0])
            if self.known[eng].get(pk, 0) < st[1]:
                self.known[eng][pk] = st[1]
                waits.append((pk, st[1]))
        return self._bump("d", slot, 16)

    def dma(self, eng, out, in_, reads=(), writes=(), out_final=False, **kw):
        return self.dma_fn(eng, lambda e: e.dma_start(out=out, in_=in_, **kw), reads, writes, out_final)

    def dma_fn(self, eng, fn, reads=(), writes=(), out_final=False):
        waits = self._deps(eng, reads, writes)
        key, c = self._dma_slot(eng, waits)
        self.items[eng].append((waits, fn, key, 16))
        self._record(reads, writes, (key, c))

    def finish(self):
        nc = self.nc
        fin = dict(self.final)
        items = self.items
        sems = self.sems

        def emit(eng_name, e, extra=None):
            for waits, fn, key, inc in items[eng_name]:
                for k, c in waits:
                    e.wait_ge(sems[k], c)
                ins = fn(e)
                ins.then_inc(sems[key], inc)
            if extra:
                for k, c in extra.items():
                    e.wait_ge(sems[k], c)

        with nc.Block() as block:
            @block.sync
            def _(e):
                emit("sync", e, fin)

            @block.scalar
            def _(e):
                emit("scalar", e)

            @block.vector
            def _(e):
                emit("vector", e)

            @block.gpsimd
            def _(e):
                emit("gpsimd", e)

            @block.tensor
            def _(e):
                emit("tensor", e)
        self.stack.close()


def kernel(**inputs):
    raise NotImplementedError


D = 2048
SEQ = 8192
CTX = 256
DEPTH = 4
NCORES = 8
INW = 4640
EPS = 1e-6
NT = 9
NE = 16
FF = 1536


def make_ident(P, name, dt):
    idf = P.sb(name + "_f", [128, 128], F32)
    P.op("gpsimd", lambda e: e.memset(idf[:], 1.0), writes=[idf])
    P.op("gpsimd", lambda e: e.affine_select(out=idf[:], in_=idf[:], pattern=[[-1, 128]],
                                             compare_op=ALU.is_equal, fill=0.0, base=0,
                                             channel_multiplier=1), reads=[idf], writes=[idf])
    if dt == F32:
        return idf
    idn = P.sb(name, [128, 128], dt)
    P.op("vector", lambda e: e.tensor_copy(out=idn[:], in_=idf[:]), reads=[idf], writes=[idn])
    return idn


def build_kmod():
    nc = bass.Bass("TRN2", target_bir_lowering=False)
    CW = 6 * D // NCORES
    cc = nc.dram_tensor("cc", [128, 16, 2], F32, kind="ExternalInput").ap()
    wm = nc.dram_tensor("wm", [DEPTH, D, CW], F32, kind="ExternalInput").ap()
    bm = nc.dram_tensor("bm", [DEPTH, 1, CW], F32, kind="ExternalInput").ap()
    out = nc.dram_tensor("mods", [DEPTH, 2, CW], F32, kind="ExternalOutput").ap()
    P = Prog(nc)
    cs = P.sb("cs", [128, 16, 2], F32)
    sc = P.sb("scs", [128, 16, 2], F32)
    P.dma("sync", cs[:], cc, writes=[cs])
    P.op("scalar", lambda e: e.activation(out=sc[:], in_=cs[:], func=AF.Silu), reads=[cs], writes=[sc])
    wts = [P.sb("w%d" % i, [128, 16, 512], F32) for i in range(2)]
    pss = [P.ps("ps%d" % i, [2, 512], F32) for i in range(2)]
    bias = P.sb("bias", [2, DEPTH, CW], F32)
    for r in range(2):
        P.dma("sync", bias[r:r + 1, :, :], bm.rearrange("l o n -> o l n"), writes=[(bias, r)])
    res = P.sb("res", [2, DEPTH, CW], F32)
    it = 0
    for l in range(DEPTH):
        for cb in range(CW // 512):
            w = wts[it % 2]
            ps = pss[it % 2]
            P.dma("sync" if it % 2 == 0 else "scalar", w[:],
                  wm[l].rearrange("(kc p) n -> p kc n", p=128)[:, :, cb * 512:(cb + 1) * 512],
                  writes=[w])
            for kc in range(16):
                P.op("tensor", lambda e, w=w, ps=ps, kc=kc: e.matmul(
                    ps[:], lhsT=sc[:, kc, :], rhs=w[:, kc, :], start=(kc == 0), stop=(kc == 15)),
                    reads=[sc, w], writes=[ps])
            P.op("vector", lambda e, ps=ps, l=l, cb=cb: e.tensor_tensor(
                out=res[:, l, cb * 512:(cb + 1) * 512], in0=ps[:], in1=bias[:, l, cb * 512:(cb + 1) * 512],
                op=ALU.add), reads=[ps, (bias, 0), (bias, 1)], writes=[(res, it)])
            it += 1
    P.dma("sync", out.rearrange("l r n -> r l n"), res[:], reads=[(res, i) for i in range(it)],
          out_final=True)
    P.finish()
    return nc


def run_kmod(inputs):
    c2 = np.stack([inputs["c"][0], inputs["c_ctx"]], axis=-1)
    cc = np.ascontiguousarray(c2.reshape(16, 128, 2).transpose(1, 0, 2))
    CW = 6 * D // NCORES
    nc = build_kmod()
    maps = []
    for i in range(NCORES):
        maps.append({
            "cc": cc,
            "wm": np.ascontiguousarray(inputs["w_mod"][:, :, i * CW:(i + 1) * CW]),
            "bm": np.ascontiguousarray(inputs["b_mod"][:, None, i * CW:(i + 1) * CW]),
        })
    res = run_bass_kernel_spmd(nc, maps, core_ids=list(range(NCORES)))
    mods = np.concatenate([r["mods"] for r in res.results], axis=-1)
    return mods


OBF_W = 2560
OF_W = 3072


def emit_rsqrt(P, out, in_, scale, rkey, wkey):
    epsb = P.const_eps()
    n = in_.shape[0]
    P.op("scalar", lambda e: e.activation(out=out, in_=in_, func=AF.Ln, scale=scale, bias=epsb[:n, :]),
         reads=[rkey, epsb], writes=[wkey])
    P.op("scalar", lambda e: e.activation(out=out, in_=out, func=AF.Exp, scale=-0.5),
         reads=[wkey], writes=[wkey])


def emit_modvec(P, vec, name):
    A = P.sb(name, [128, 2, 16], F32)
    for j in range(2):
        P.op("vector", lambda e, j=j: e.scalar_tensor_tensor(
            out=A[:, j, :], in0=vec[:, 1 + 2 * j, :], scalar=1.0, in1=vec[:, 0, :],
            op0=ALU.add, op1=ALU.mult), reads=[vec], writes=[A])
    return A


def declare_combine_inputs(nc):
    oes = [nc.dram_tensor("oe%d" % e, [NSLOT, D], F32, kind="ExternalInput").ap() for e in range(NE)]
    cpos = nc.dram_tensor("cpos", [128, NT, NE], I32, kind="ExternalInput").ap()
    caff = nc.dram_tensor("caff", [128, NT, NE], F32, kind="ExternalInput").ap()
    g2v = nc.dram_tensor("g2v", [2, D], F32, kind="ExternalInput").ap()
    return oes, cpos, caff, g2v


class Combiner:
    def __init__(self, P, oes, cpos, caff, g2v):
        self.P, self.oes, self.g2v = P, oes, g2v
        self.pos = P.sb("cpos_s", [128, NT, NE], I32)
        P.dma("sync", self.pos[:], cpos, writes=[self.pos])
        self.aff = P.sb("caff_s", [128, NT, NE], F32)
        P.dma("sync", self.aff[:], caff, writes=[self.aff])
        self.g2 = P.sb("g2_s", [128, D], F32)
        self.acc = P.sb("cacc", [128, D], F32)
        self.gbl = [P.sb("cgb%d" % i, [128, D], F32) for i in range(4)]
        self.n = 0
        self.g2_loaded = None

    def emit(self, t, xt):
        P = self.P
        j = 1 if t == NT - 1 else 0
        if self.g2_loaded != j:
            P.dma("sync", self.g2[:], self.g2v[j:j + 1, :].partition_broadcast(128), writes=[self.g2])
            self.g2_loaded = j
        acc = self.acc
        for e_ in range(NE):
            gb = self.gbl[self.n % 4]
            self.n += 1
            P.op("gpsimd", lambda e, gb=gb: e.memset(gb[:], 0.0), writes=[gb])
            P.dma_fn("gpsimd", lambda e, gb=gb, e_=e_, t=t: e.indirect_dma_start(
                out=gb[:, :], out_offset=None, in_=self.oes[e_],
                in_offset=bass.IndirectOffsetOnAxis(ap=self.pos[:, t, e_:e_ + 1], axis=0),
                bounds_check=P.reg(e, NSLOT - 1), oob_is_err=False), reads=[self.pos, gb], writes=[gb])
            gate = self.aff[:, t, e_:e_ + 1]
            if e_ == 0:
                P.op("vector", lambda e, gb=gb, gate=gate: e.tensor_scalar(
                    out=acc[:], in0=gb[:], scalar1=gate, scalar2=None, op0=ALU.mult), reads=[gb, self.aff], writes=[acc])
            else:
                P.op("vector", lambda e, gb=gb, gate=gate: e.scalar_tensor_tensor(
                    out=acc[:], in0=gb[:], scalar=gate, in1=acc[:], op0=ALU.mult, op1=ALU.add),
                    reads=[gb, acc, self.aff], writes=[acc])
        P.op("vector", lambda e: e.tensor_tensor(out=self.acc[:], in0=self.acc[:], in1=self.g2[:], op=ALU.mult),
             reads=[self.acc, self.g2], writes=[self.acc])
        P.op("vector", lambda e, xt=xt: e.tensor_tensor(out=xt[:], in0=xt[:], in1=self.acc[:], op=ALU.add),
             reads=[xt, self.acc], writes=[xt])


def build_ka(combine=False):
    nc = bass.Bass("TRN2", target_bir_lowering=False)
    x_in = nc.dram_tensor("x_in", [NT * 128, D], F32, kind="ExternalInput").ap()
    w_in = nc.dram_tensor("w_in", [D, INW], F32, kind="ExternalInput").ap()
    vecs = nc.dram_tensor("vecs", [128, 5, 16], F32, kind="ExternalInput").ap()
    gains = nc.dram_tensor("gains", [128, 2, 128], F32, kind="ExternalInput").ap()
    rope = nc.dram_tensor("rope", [128, NT, 2, 64], F32, kind="ExternalInput").ap()
    wa2 = nc.dram_tensor("wa2", [16, 2, 512], F32, kind="ExternalInput").ap()
    ba = nc.dram_tensor("ba", [1, 2, 512], F32, kind="ExternalInput").ap()
    out_bf = nc.dram_tensor("out_bf", [NT * 128, OBF_W], BF16, kind="ExternalOutput").ap()
    out_f = nc.dram_tensor("out_f", [NT * 128, OF_W], F32, kind="ExternalOutput").ap()
    P = Prog(nc)
    comb = x_new = None
    if combine:
        comb = Combiner(P, *declare_combine_inputs(nc))
        x_new = nc.dram_tensor("x_new", [NT * 128, D], F32, kind="ExternalOutput").ap()
    emit_ka_body(P, x_in, w_in, vecs, gains, rope, wa2, ba, out_bf, out_f, comb, x_new)
    P.finish()
    return nc


def emit_ka_body(P, x_in, w_in, vecs, gains, rope, wa2, ba, out_bf, out_f, comb=None, x_new=None):
    identf = make_ident(P, "identf", F32)
    vec = P.sb("vec", [128, 5, 16], F32)
    P.dma("sync", vec[:], vecs, writes=[vec])
    A = emit_modvec(P, vec, "Amod")
    gn = P.sb("gn", [128, 2, 128], F32)
    P.dma("sync", gn[:], gains, writes=[gn])
    rp = P.sb("rp", [128, NT, 2, 64], F32)
    P.dma("sync", rp[:], rope, writes=[rp])
    wa2s = P.sb("wa2s", [16, 2, 512], F32)
    P.dma("sync", wa2s[:], wa2, writes=[wa2s])
    bas = P.sb("bas", [1, 2, 512], F32)
    P.dma("sync", bas[:], ba, writes=[bas])
    ones1 = P.sb("ones1", [1, 128], F32)
    P.op("gpsimd", lambda e: e.memset(ones1[:], 1.0), writes=[ones1])

    hT = P.sb("hT", [128, NT, 16, 128], BF16)
    xts = [P.sb("xt%d" % i, [128, D], F32) for i in range(2)]
    junk = P.sb("junk", [128, D], BF16)
    ss = P.sb("ss", [128, NT], F32)
    rstd = P.sb("rstd", [128, NT], F32)
    tps = [P.ps("tp%d" % i, [128, 8, 128], F32) for i in range(2)]
    def phase1(t):
        xt = xts[t % 2]
        P.dma("sync", xt[:], x_in[t * 128:(t + 1) * 128, :], writes=[xt])
        if comb is not None:
            comb.emit(t, xt)
            P.dma("sync", x_new[t * 128:(t + 1) * 128, :], xt[:], reads=[xt])
        P.op("scalar", lambda e, xt=xt, t=t: e.activation(
            out=junk[:], in_=xt[:], func=AF.Square, accum_out=ss[:, t:t + 1]),
            reads=[xt], writes=[junk, (ss, t)])
        emit_rsqrt(P, rstd[:, t:t + 1], ss[:, t:t + 1], 1.0 / D, (ss, t), (rstd, t))
        P.op("vector", lambda e, xt=xt, t=t: e.tensor_scalar(
            out=xt[:], in0=xt[:], scalar1=rstd[:, t:t + 1], scalar2=None, op0=ALU.mult),
            reads=[xt, (rstd, t)], writes=[xt])
        mi = 1 if t == NT - 1 else 0
        for half in range(2):
            tp = tps[half]
            for j in range(8):
                kc = half * 8 + j
                P.op("tensor", lambda e, tp=tp, j=j, kc=kc, xt=xt: e.transpose(
                    out=tp[:, j, :], in_=xt[:, kc * 128:(kc + 1) * 128], identity=identf[:]),
                    reads=[xt, identf], writes=[tp])
            for j in range(8):
                kc = half * 8 + j
                P.op("scalar", lambda e, tp=tp, j=j, kc=kc, t=t, mi=mi: e.activation(
                    out=hT[:, t, kc, :], in_=tp[:, j, :], func=AF.Identity,
                    scale=A[:, mi, kc:kc + 1], bias=vec[:, 2 + 2 * mi, kc:kc + 1]),
                    reads=[tp, A, vec], writes=[(hT, t)])

    for t in range(NT):
        phase1(t)

    wbs = [P.sb("wb%d" % i, [128, 16, 512], BF16) for i in range(2)]
    mps = [P.ps("mp%d" % i, [128, 512], F32) for i in range(2)]
    zp = P.ps("zp", [128, 512], F32)
    tpa = P.ps("tpa", [16, 128], F32)
    sq = P.sb("sq", [128, 512], F32)
    ssq = P.sb("ssq", [128, 4], F32)
    qn = P.sb("qn", [128, 512], F32)
    rt = [P.sb("rt%d" % i, [128, 256], F32) for i in range(2)]
    sbf = [P.sb("sbf%d" % i, [128, 512], BF16) for i in range(4)]
    sf = [P.sb("sf%d" % i, [128, 512], F32) for i in range(4)]
    asb = P.sb("asb", [128, 32], F32)
    aT = P.sb("aT", [16, 128], F32)
    ez = P.sb("ez", [128, 512], F32)
    cnt = {"bf": 0, "f": 0}
    w_v = w_in.rearrange("(kc p) n -> p kc n", p=128)

    def stage(kind):
        i = cnt[kind]
        cnt[kind] += 1
        return (sbf if kind == "bf" else sf)[i % 4]

    def qk_epi(mp, c0, nh, gi, t, dst):
        w = nh * 128
        P.op("scalar", lambda e: e.activation(out=sq[:, :w], in_=mp[:, c0:c0 + w], func=AF.Square),
             reads=[mp], writes=[sq])
        P.op("vector", lambda e: e.tensor_reduce(
            out=ssq[:, :nh], in_=sq[:, :w].rearrange("p (h d) -> p h d", h=nh), axis=AX.X, op=ALU.add),
            reads=[sq], writes=[ssq])
        emit_rsqrt(P, ssq[:, :nh], ssq[:, :nh], 1.0 / 128, ssq, ssq)
        q3 = qn[:, :w].rearrange("p (h d) -> p h d", h=nh)
        P.op("vector", lambda e: e.tensor_tensor(
            out=q3, in0=mp[:, c0:c0 + w].rearrange("p (h d) -> p h d", h=nh),
            in1=ssq[:, :nh].unsqueeze(2).broadcast_to([128, nh, 128]), op=ALU.mult),
            reads=[mp, ssq], writes=[qn])
        P.op("vector", lambda e: e.tensor_tensor(
            out=q3, in0=q3, in1=gn[:, gi, :].unsqueeze(1).broadcast_to([128, nh, 128]), op=ALU.mult),
            reads=[qn, gn], writes=[qn])
        q4 = qn[:, :w].rearrange("p (h i two) -> p h i two", h=nh, two=2)
        d4 = dst.rearrange("p (h i two) -> p h i two", h=nh, two=2)
        cosb = rp[:, t, 0, :].unsqueeze(1).broadcast_to([128, nh, 64])
        sinb = rp[:, t, 1, :].unsqueeze(1).broadcast_to([128, nh, 64])
        r0 = rt[0][:, :nh * 64].rearrange("p (h i) -> p h i", h=nh)
        r1 = rt[1][:, :nh * 64].rearrange("p (h i) -> p h i", h=nh)
        for (o, a, ca, b, cb_, op) in ((0, 0, cosb, 1, sinb, ALU.subtract), (1, 0, sinb, 1, cosb, ALU.add)):
            P.op("vector", lambda e, a=a, ca=ca: e.tensor_tensor(out=r0, in0=q4[:, :, :, a], in1=ca, op=ALU.mult),
                 reads=[qn, rp], writes=[rt[0]])
            P.op("gpsimd", lambda e, b=b, cb_=cb_: e.tensor_tensor(out=r1, in0=q4[:, :, :, b], in1=cb_, op=ALU.mult),
                 reads=[qn, rp], writes=[rt[1]])
            P.op("vector", lambda e, o=o, op=op: e.tensor_tensor(out=d4[:, :, :, o], in0=r0, in1=r1, op=op),
                 reads=[rt[0], rt[1]], writes=[dst.tensor])

    it = 0
    for cb in range(10):
        n = 512 if cb < 9 else 32
        wb = wbs[cb % 2]
        for q in range(4):
            P.dma("gpsimd", wb[:, 4 * q:4 * q + 4, :n], w_v[:, 4 * q:4 * q + 4, cb * 512:cb * 512 + n],
                  writes=[(wb, q)])
        for t in range(NT):
            mp = mps[it % 2]
            it += 1
            for kc in range(16):
                P.op("tensor", lambda e, mp=mp, t=t, kc=kc, wb=wb, n=n: e.matmul(
                    mp[:, :n], lhsT=hT[:, t, kc, :], rhs=wb[:, kc, :n], start=(kc == 0), stop=(kc == 15)),
                    reads=[(hT, t), (wb, kc // 4)], writes=[mp])
            rows = slice(t * 128, (t + 1) * 128)
            if cb in (0, 1):
                st = stage("bf")
                qk_epi(mp, 0, 4, 0, t, st[:, :])
                P.dma("sync", out_bf[rows, cb * 512:(cb + 1) * 512], st[:], reads=[st])
            elif cb == 2:
                st = stage("bf")
                qk_epi(mp, 0, 2, 1, t, st[:, 0:256])
                P.op("scalar", lambda e, st=st, mp=mp: e.activation(out=st[:, 256:512], in_=mp[:, 256:512], func=AF.Copy),
                     reads=[mp], writes=[st])
                P.dma("sync", out_bf[rows, 1024:1536], st[:], reads=[st])
            elif cb in (3, 4):
                st = stage("f")
                sc_ = 128 ** -0.5 if cb == 3 else 1.0
                P.op("scalar", lambda e, st=st, mp=mp, sc_=sc_: e.activation(out=st[:], in_=mp[:], func=AF.Copy, scale=sc_),
                     reads=[mp], writes=[st])
                P.dma("sync", out_f[rows, (cb - 3) * 512:(cb - 2) * 512], st[:], reads=[st])
            elif cb in (5, 6):
                st = stage("bf")
                P.op("scalar", lambda e, st=st, mp=mp: e.activation(out=st[:], in_=mp[:], func=AF.Copy),
                     reads=[mp], writes=[st])
                P.dma("sync", out_bf[rows, 1536 + (cb - 5) * 512:1536 + (cb - 4) * 512], st[:], reads=[st])
            elif cb in (7, 8):
                st = stage("f")
                P.op("scalar", lambda e, st=st, mp=mp: e.activation(out=st[:], in_=mp[:], func=AF.Silu),
                     reads=[mp], writes=[st])
                P.dma("sync", out_f[rows, 1024 + (cb - 7) * 512:1024 + (cb - 6) * 512], st[:], reads=[st])
            else:
                P.op("scalar", lambda e, mp=mp: e.activation(out=asb[:], in_=mp[:, :32], func=AF.Copy),
                     reads=[mp], writes=[asb])
                for d in range(2):
                    P.op("tensor", lambda e, d=d: e.transpose(out=tpa[:], in_=asb[:, d * 16:(d + 1) * 16], identity=identf[:]),
                         reads=[asb, identf], writes=[tpa])
                    P.op("vector", lambda e: e.tensor_copy(out=aT[:], in_=tpa[:]), reads=[tpa], writes=[aT])
                    P.op("tensor", lambda e, d=d: e.matmul(zp[:], lhsT=aT[:], rhs=wa2s[:, d, :], start=True, stop=False),
                         reads=[aT, wa2s], writes=[zp])
                    P.op("tensor", lambda e, d=d: e.matmul(zp[:], lhsT=ones1[:], rhs=bas[:, d, :], start=False, stop=True),
                         reads=[ones1, bas], writes=[zp])
                    st = stage("f")
                    P.op("scalar", lambda e: e.activation(out=ez[:], in_=zp[:], func=AF.Exp, scale=-1.0),
                         reads=[zp], writes=[ez])
                    P.op("scalar", lambda e: e.activation(out=ez[:], in_=ez[:], func=AF.Ln, bias=1.0),
                         reads=[ez], writes=[ez])
                    P.op("vector", lambda e, st=st: e.tensor_scalar(out=st[:], in0=ez[:], scalar1=-1.0 / 16, scalar2=None, op0=ALU.mult),
                         reads=[ez], writes=[st])
                    P.dma("sync", out_f[rows, 2048 + d * 512:2048 + (d + 1) * 512], st[:], reads=[st])


def fm(v):
    return np.ascontiguousarray(np.asarray(v).reshape(16, 128).T)


def rope_tables():
    rows = SEQ // 64
    row = np.repeat(np.arange(rows, dtype=np.float32), 64)
    col = np.tile(np.arange(64, dtype=np.float32), rows)
    inv = (np.float32(10000.0) ** (-np.arange(0, 64, 2, dtype=np.float32) / np.float32(64))).astype(np.float32)
    ang = np.concatenate([row[:, None] * inv, col[:, None] * inv], axis=-1).astype(np.float32)
    return np.cos(ang).astype(np.float32), np.sin(ang).astype(np.float32)


def core_tokens(a_lat, a_ctx, i):
    return np.concatenate([a_lat[i * 1024:(i + 1) * 1024], a_ctx[(i % 2) * 128:(i % 2) * 128 + 128]], axis=0)


def ka_static_inputs(inputs, mods, l):
    m_lat, m_ctx = mods[l, 0], mods[l, 1]
    vec = np.stack([fm(inputs["norm_mix"][l]), fm(m_lat[D:2 * D]), fm(m_lat[0:D]),
                    fm(m_ctx[D:2 * D]), fm(m_ctx[0:D])], axis=1).astype(np.float32)
    gains = np.ascontiguousarray(np.broadcast_to(
        np.stack([inputs["q_gain"][l], inputs["k_gain"][l]])[None], (128, 2, 128))).astype(np.float32)
    cos, sin = rope_tables()
    ropes = []
    for i in range(NCORES):
        c = core_tokens(cos, np.ones((CTX, 64), np.float32), i)
        s = core_tokens(sin, np.zeros((CTX, 64), np.float32), i)
        r = np.stack([c, s], axis=1).reshape(NT, 128, 2, 64).transpose(1, 0, 2, 3)
        ropes.append(np.ascontiguousarray(r))
    wa2 = np.ascontiguousarray(inputs["w_gla_a2"][l].transpose(1, 0, 2))
    ba = np.ascontiguousarray(inputs["b_gla_a"][l][None])
    return vec, gains, ropes, wa2, ba


def combine_maps(mods, l_prev, kd_outs, aff_lat, aff_ctx):
    oes = {}
    for i, (oe, pl, pc) in enumerate(kd_outs):
        oes["oe%d" % (2 * i)] = np.ascontiguousarray(oe[0])
        oes["oe%d" % (2 * i + 1)] = np.ascontiguousarray(oe[1])
    pos_lat = np.concatenate([o[1].transpose(2, 0, 1).reshape(SEQ, 2) for o in kd_outs], axis=1)
    pos_ctx = np.concatenate([o[2].transpose(2, 0, 1).reshape(CTX, 2) for o in kd_outs], axis=1)
    g2v = np.stack([mods[l_prev, 0][5 * D:6 * D], mods[l_prev, 1][5 * D:6 * D]]).astype(np.float32)
    maps = []
    for i in range(NCORES):
        cp = core_tokens(pos_lat, pos_ctx, i).reshape(NT, 128, NE).transpose(1, 0, 2)
        m = dict(oes)
        m["cpos"] = np.ascontiguousarray(cp).astype(np.int32)
        m["caff"] = np.ascontiguousarray(core_tokens(aff_lat, aff_ctx, i).reshape(NT, 128, NE).transpose(1, 0, 2))
        m["g2v"] = g2v
        maps.append(m)
    return maps


def run_ka(inputs, mods, l, x_lat, x_ctx, nc=None, comb_maps=None):
    nc = nc or build_ka(combine=comb_maps is not None)
    vec, gains, ropes, wa2, ba = ka_static_inputs(inputs, mods, l)
    w_in = np.ascontiguousarray(inputs["w_in"][l])
    maps = []
    for i in range(NCORES):
        maps.append({"x_in": core_tokens(x_lat, x_ctx, i), "w_in": w_in, "vecs": vec, "gains": gains,
                     "rope": ropes[i], "wa2": wa2, "ba": ba})
        if comb_maps is not None:
            maps[-1].update(comb_maps[i])
    res = run_bass_kernel_spmd(nc, maps, core_ids=list(range(NCORES)))
    if comb_maps is not None:
        return [(r["out_bf"], r["out_f"], r["x_new"]) for r in res.results]
    return [(r["out_bf"], r["out_f"]) for r in res.results]


def gather_tokens(per_core):
    lat = np.concatenate([a[:1024] for a in per_core], axis=0)
    ctx = np.concatenate([per_core[0][1024:], per_core[1][1024:]], axis=0)
    return lat, ctx


NTOK = CTX + SEQ
NTT = NTOK // 128
GG = 6


def build_kb(do_gla=True, do_attn=True, nblk=None, ngrp=None):
    nc = bass.Bass("TRN2", target_bir_lowering=False)
    qT = nc.dram_tensor("qT", [128, NTOK], BF16, kind="ExternalInput").ap()
    kT = nc.dram_tensor("kT", [128, NTOK], BF16, kind="ExternalInput").ap()
    v = nc.dram_tensor("v", [NTOK, 128], BF16, kind="ExternalInput").ap()
    gqT = nc.dram_tensor("gqT", [128, NTOK], F32, kind="ExternalInput").ap()
    gkT = nc.dram_tensor("gkT", [128, NTOK], F32, kind="ExternalInput").ap()
    gk = nc.dram_tensor("gk", [NTOK, 128], F32, kind="ExternalInput").ap()
    gv = nc.dram_tensor("gv", [NTOK, 256], BF16, kind="ExternalInput").ap()
    la = nc.dram_tensor("la", [NTOK, 128], F32, kind="ExternalInput").ap()
    oT = nc.dram_tensor("oT", [128, NTOK], BF16, kind="ExternalOutput").ap()
    og = nc.dram_tensor("og", [NTOK, 256], F32, kind="ExternalOutput").ap()
    P = Prog(nc)
    if do_gla and do_attn:
        emit_attn(P, qT, kT, v, oT, nblk, side=gla_gen(P, gqT, gkT, gk, gv, la, og, ngrp))
    elif do_gla:
        emit_gla(P, gqT, gkT, gk, gv, la, og, ngrp)
    elif do_attn:
        emit_attn(P, qT, kT, v, oT, nblk)
    P.finish()
    return nc


class PV:
    def __init__(self, t, c0, n):
        self.t, self.c0, self.n, self.name = t, c0, n, t.name

    def __getitem__(self, key):
        if not isinstance(key, tuple):
            key = (key, slice(None))
        rows, cols = key
        start = cols.start or 0
        stop = self.n if cols.stop is None else cols.stop
        return self.t[rows, self.c0 + start:self.c0 + stop]


def make_tri(P, name, upper):
    m = P.sb(name, [128, 128], F32)
    P.op("gpsimd", lambda e: e.memset(m[:], 0.0), writes=[m])
    for b in range(2):
        blk = m[b * 64:(b + 1) * 64, b * 64:(b + 1) * 64]
        P.op("gpsimd", lambda e, blk=blk: e.memset(blk, 1.0), reads=[m], writes=[m])
        if upper:
            P.op("gpsimd", lambda e, blk=blk: e.affine_select(
                out=blk, in_=blk, pattern=[[1, 64]], compare_op=ALU.is_ge, fill=0.0, base=0,
                channel_multiplier=-1), reads=[m], writes=[m])
        else:
            P.op("gpsimd", lambda e, blk=blk: e.affine_select(
                out=blk, in_=blk, pattern=[[-1, 64]], compare_op=ALU.is_gt, fill=0.0, base=0,
                channel_multiplier=1), reads=[m], writes=[m])
    return m


def gla_gen(P, gqT, gkT, gk, gv, la, og, ngrp_lim=None):
    U2 = make_tri(P, "U2", True)
    L2 = make_tri(P, "L2", False)
    ngrp = ngrp_lim or NTT // GG
    gqT_s = [P.sb("gqT_s%d" % i, [128, GG * 128], F32) for i in range(2)]
    gkT_s = [P.sb("gkT_s%d" % i, [128, GG * 128], F32) for i in range(2)]
    gk_s = [P.sb("gk_s%d" % i, [128, GG, 128], F32) for i in range(2)]
    gv_s = [P.sb("gv_s%d" % i, [128, GG, 256], BF16) for i in range(2)]
    la_s = [P.sb("la_s%d" % i, [128, GG, 128], F32) for i in range(2)]
    bankA = P.ps("gla_bankA", [128, 512], F32)
    bankB = P.ps("gla_bankB", [128, 512], F32)
    bT_ps, bl_ps, at_ps = PV(bankA, 0, 128), PV(bankA, 128, 128), PV(bankA, 256, 128)
    o_ps = [PV(bankB, 0, 256)] * 2
    ds_ps = [P.ps("ds_ps%d" % i, [128, 256], F32) for i in range(2)]
    eb = [P.sb("eb%d" % i, [128, 128], F32) for i in range(2)]
    enb = P.sb("enb", [128, 128], F32)
    ekh = P.sb("ekh", [128, 128], F32)
    qtT = [P.sb("qtT%d" % i, [128, 128], BF16) for i in range(2)]
    ktT = P.sb("ktT", [128, 128], BF16)
    kh = [P.sb("kh%d" % i, [128, 128], BF16) for i in range(2)]
    atm = P.sb("atm", [128, 128], BF16)
    S = [P.sb("S%d" % i, [128, 256], F32) for i in range(2)]
    Sb = [P.sb("Sb%d" % i, [128, 256], BF16) for i in range(4)]
    ost = [P.sb("ost%d" % i, [128, 256], F32) for i in range(2)]
    P.op("gpsimd", lambda e: e.memset(S[0][:], 0.0), writes=[S[0]])
    P.op("gpsimd", lambda e: e.memset(Sb[0][:], 0.0), writes=[Sb[0]])
    sc = 0
    def load_group(g):
        b = g % 2
        r0 = g * GG * 128
        P.dma("sync", gqT_s[b][:], gqT[:, r0:r0 + GG * 128], writes=[gqT_s[b]])
        P.dma("sync", gkT_s[b][:], gkT[:, r0:r0 + GG * 128], writes=[gkT_s[b]])
        P.dma("sync", gk_s[b][:], gk[r0:r0 + GG * 128, :].rearrange("(t p) d -> p t d", p=128), writes=[gk_s[b]])
        P.dma("sync", gv_s[b][:], gv[r0:r0 + GG * 128, :].rearrange("(t p) d -> p t d", p=128), writes=[gv_s[b]])
        P.dma("sync", la_s[b][:], la[r0:r0 + GG * 128, :].rearrange("(t p) d -> p t d", p=128), writes=[la_s[b]])

    load_group(0)
    for g in range(ngrp):
        b = g % 2
        if g + 1 < ngrp:
            load_group(g + 1)
        gq_b, gkT_b, gk_b, gv_b = gqT_s[b], gkT_s[b], gk_s[b], gv_s[b]
        for tt in range(GG):
            ti = g * GG + tt
            p2 = ti % 2
            la_t = la_s[b][:, tt, :]
            cols = slice(tt * 128, (tt + 1) * 128)
            P.op("tensor", lambda e, la_t=la_t: e.matmul(bT_ps[:], lhsT=la_t, rhs=U2[:], start=True, stop=True),
                 reads=[la_s[b], U2], writes=[bT_ps])
            P.op("tensor", lambda e, la_t=la_t: e.matmul(bl_ps[:], lhsT=L2[:], rhs=la_t, start=True, stop=True),
                 reads=[la_s[b], L2], writes=[bl_ps])
            yield
            ebt = eb[p2]
            P.op("scalar", lambda e, ebt=ebt: e.activation(out=ebt[:], in_=bT_ps[:], func=AF.Exp), reads=[bT_ps], writes=[ebt])
            P.op("scalar", lambda e: e.activation(out=enb[:], in_=bT_ps[:], func=AF.Exp, scale=-1.0), reads=[bT_ps], writes=[enb])
            P.op("scalar", lambda e: e.activation(out=ekh[:], in_=bl_ps[:], func=AF.Exp), reads=[bl_ps], writes=[ekh])
            yield
            q_t = qtT[p2]
            kh_t = kh[p2]
            P.op("vector", lambda e, q_t=q_t, ebt=ebt, cols=cols, gq_b=gq_b: e.tensor_tensor(out=q_t[:], in0=gq_b[:, cols], in1=ebt[:], op=ALU.mult),
                 reads=[gqT_s[b], ebt], writes=[q_t])
            P.op("vector", lambda e, cols=cols, gkT_b=gkT_b: e.tensor_tensor(out=ktT[:], in0=gkT_b[:, cols], in1=enb[:], op=ALU.mult),
                 reads=[gkT_s[b], enb], writes=[ktT])
            P.op("gpsimd", lambda e, kh_t=kh_t, tt=tt, gk_b=gk_b: e.tensor_tensor(out=kh_t[:], in0=gk_b[:, tt, :], in1=ekh[:], op=ALU.mult),
                 reads=[gk_s[b], ekh], writes=[kh_t])
            yield
            P.op("tensor", lambda e, q_t=q_t: e.matmul(at_ps[:], lhsT=ktT[:], rhs=q_t[:], start=True, stop=True),
                 reads=[ktT, q_t], writes=[at_ps])
            yield
            P.op("vector", lambda e: e.tensor_tensor(out=atm[:], in0=at_ps[:], in1=U2[:], op=ALU.mult),
                 reads=[at_ps, U2], writes=[atm])
            yield
            ops_ = o_ps[p2]
            gv_t = gv_s[b][:, tt, :]
            P.op("tensor", lambda e, ops_=ops_, gv_t=gv_t: e.matmul(ops_[:], lhsT=atm[:], rhs=gv_t, start=True, stop=False),
                 reads=[atm, gv_s[b]], writes=[ops_])
            for c in range(2):
                rs = slice(c * 64, (c + 1) * 64)
                Sb_c = Sb[sc % 4]
                P.op("tensor", lambda e, ops_=ops_, q_t=q_t, rs=rs, Sb_c=Sb_c, c=c: e.matmul(
                    ops_[rs, :], lhsT=q_t[:, rs], rhs=Sb_c[:], start=False, stop=True),
                    reads=[q_t, Sb_c], writes=[ops_])
                dsp = ds_ps[sc % 2]
                P.op("tensor", lambda e, dsp=dsp, kh_t=kh_t, rs=rs, tt=tt, gv_b=gv_b: e.matmul(
                    dsp[:], lhsT=kh_t[rs, :], rhs=gv_b[rs, tt, :], start=True, stop=True),
                    reads=[kh_t, gv_s[b]], writes=[dsp])
                yield
                S_cur, S_nxt = S[sc % 2], S[(sc + 1) % 2]
                col = c * 64 + 63
                P.op("vector", lambda e, S_cur=S_cur, S_nxt=S_nxt, ebt=ebt, col=col, dsp=dsp: e.scalar_tensor_tensor(
                    out=S_nxt[:], in0=S_cur[:], scalar=ebt[:, col:col + 1], in1=dsp[:], op0=ALU.mult, op1=ALU.add),
                    reads=[S_cur, ebt, dsp], writes=[S_nxt])
                Sb_n = Sb[(sc + 1) % 4]
                P.op("gpsimd", lambda e, Sb_n=Sb_n, S_nxt=S_nxt: e.tensor_copy(out=Sb_n[:], in_=S_nxt[:]),
                     reads=[S_nxt], writes=[Sb_n])
                sc += 1
                yield
            o_t = ost[p2]
            P.op("scalar", lambda e, o_t=o_t, ops_=ops_: e.activation(out=o_t[:], in_=ops_[:], func=AF.Copy),
                 reads=[ops_], writes=[o_t])
            P.dma("sync", og[ti * 128:(ti + 1) * 128, :], o_t[:], reads=[o_t])
            yield


def emit_gla(P, gqT, gkT, gk, gv, la, og, ngrp_lim=None):
    for _ in gla_gen(P, gqT, gkT, gk, gv, la, og, ngrp_lim):
        pass


def emit_attn(P, qT, kT, v, oT, nblk=None, side=None):
    q_s = P.sb("q_s", [128, NTOK], BF16)
    k_s = P.sb("k_s", [128, NTOK], BF16)
    v_s = P.sb("v_s", [128, NTT, 128], BF16)
    for j in range(4):
        cs = slice(j * (NTOK // 4), (j + 1) * (NTOK // 4))
        P.dma("sync", q_s[:, cs], qT[:, cs], writes=[(q_s, j)])
        P.dma("scalar", k_s[:, cs], kT[:, cs], writes=[(k_s, j)])
    vv = v.rearrange("(t p) d -> p t d", p=128)
    for j in range(3):
        ts_ = slice(j * 22, (j + 1) * 22)
        P.dma("sync", v_s[:, ts_, :], vv[:, ts_, :], writes=[(v_s, j)])
    ones = P.sb("ones_f32", [128, 128], F32)
    P.op("gpsimd", lambda e: e.memset(ones[:], 1.0), writes=[ones])
    pacc = [P.sb("pacc%d" % i, [128, 512], F32) for i in range(2)]
    st_ps = [P.ps("st_ps%d" % i, [128, 512], F32) for i in range(2)]
    oa_ps = [P.ps("oa_ps", [128, 512], F32)] * 2
    dn_ps = [P.ps("dn_ps", [128, 512], F32)] * 2
    pT = [P.sb("pT%d" % i, [128, 512], BF16) for i in range(3)]
    rden = P.sb("rden", [128, 512], F32)
    ob = [P.sb("ob%d" % i, [128, 512], BF16) for i in range(2)]
    blocks = [(0, CTX, 2)] + [(CTX + i * 512, 512, NTT) for i in range(SEQ // 512)]
    qkeys = [(q_s, j) for j in range(4)]
    if nblk:
        blocks = blocks[:nblk]
    iters = [(bi, q0, n, ns, s_) for bi, (q0, n, ns) in enumerate(blocks) for s_ in range(ns)]

    def emit_s(idx):
        bi, q0, n, ns, s_ = iters[idx]
        sp = st_ps[idx % 2]
        kk = (k_s, (s_ * 128) // (NTOK // 4))
        kk2 = (k_s, (s_ * 128 + 127) // (NTOK // 4))
        P.op("tensor", lambda e: e.matmul(sp[:, :n], lhsT=k_s[:, s_ * 128:(s_ + 1) * 128], rhs=q_s[:, q0:q0 + n],
                                          start=True, stop=True), reads=[kk, kk2] + qkeys, writes=[sp])

    emit_s(0)
    for idx, (bi, q0, n, ns, s_) in enumerate(iters):
        if idx + 1 < len(iters):
            emit_s(idx + 1)
        oa, dn = oa_ps[0], dn_ps[0]
        sp, pt = st_ps[idx % 2], pT[idx % 3]
        P.op("scalar", lambda e, sp=sp, pt=pt, n=n: e.activation(
            out=pt[:, :n], in_=sp[:, :n], func=AF.Exp, scale=128 ** -0.5), reads=[sp], writes=[pt])
        P.op("tensor", lambda e, pt=pt, s_=s_, n=n, ns=ns: e.matmul(
            oa[:, :n], lhsT=v_s[:, s_, :], rhs=pt[:, :n], start=(s_ == 0), stop=(s_ == ns - 1)),
            reads=[(v_s, s_ // 22), pt], writes=[oa])
        pa = pacc[bi % 2]
        if s_ == 0:
            P.op("vector", lambda e, pa=pa, pt=pt, n=n: e.tensor_copy(out=pa[:, :n], in_=pt[:, :n]), reads=[pt], writes=[pa])
        else:
            P.op("vector", lambda e, pa=pa, pt=pt, n=n: e.tensor_tensor(out=pa[:, :n], in0=pa[:, :n], in1=pt[:, :n], op=ALU.add),
                 reads=[pt, pa], writes=[pa])
        if s_ == ns - 1:
            P.op("tensor", lambda e, pa=pa, n=n: e.matmul(dn[:, :n], lhsT=ones[:], rhs=pa[:, :n], start=True, stop=True),
                 reads=[ones, pa], writes=[dn])
        if s_ == ns - 1:
            o_b = ob[bi % 2]
            P.op("vector", lambda e, n=n: e.reciprocal(out=rden[:, :n], in_=dn[:, :n]), reads=[dn], writes=[rden])
            P.op("vector", lambda e, o_b=o_b, n=n: e.tensor_tensor(out=o_b[:, :n], in0=oa[:, :n], in1=rden[:, :n], op=ALU.mult),
                 reads=[oa, rden], writes=[o_b])
            P.dma("sync", oT[:, q0:q0 + n], o_b[:, :n], reads=[o_b])
        if side is not None:
            next(side, None)
    if side is not None:
        for _ in side:
            pass


def run_kb(bf_lat, bf_ctx, f_lat, f_ctx, nc=None):
    nc = nc or build_kb()
    bf = np.concatenate([bf_ctx, bf_lat], axis=0)
    f = np.concatenate([f_ctx, f_lat], axis=0)
    rev = np.concatenate([np.arange(CTX)[::-1], CTX + np.arange(SEQ)[::-1]])
    maps = []
    for i in range(NCORES):
        kv, h, d = i // 4, i % 4, i // 4
        order = rev if d == 1 else np.arange(NTOK)
        la = f[:, 2048 + d * 512 + h * 128:2048 + d * 512 + (h + 1) * 128][order]
        gq = f[:, h * 128:(h + 1) * 128][order]
        gk = f[:, 512 + h * 128:512 + (h + 1) * 128][order]
        gv = bf[:, 1536 + h * 256:1536 + (h + 1) * 256][order]
        maps.append({
            "qT": np.ascontiguousarray(bf[:, i * 128:(i + 1) * 128].T),
            "kT": np.ascontiguousarray(bf[:, 1024 + kv * 128:1024 + (kv + 1) * 128].T),
            "v": np.ascontiguousarray(bf[:, 1280 + kv * 128:1280 + (kv + 1) * 128]),
            "gqT": np.ascontiguousarray(gq.T), "gkT": np.ascontiguousarray(gk.T),
            "gk": np.ascontiguousarray(gk), "gv": np.ascontiguousarray(gv), "la": np.ascontiguousarray(la),
        })
    res = run_bass_kernel_spmd(nc, maps, core_ids=list(range(NCORES)))
    inv = np.argsort(rev)
    attnT = [r["oT"] for r in res.results]
    og = [r["og"] if i < 4 else r["og"][inv] for i, r in enumerate(res.results)]
    return attnT, og


def build_kc():
    nc = bass.Bass("TRN2", target_bir_lowering=False)
    R = NT * 128
    attnT = nc.dram_tensor("attnT", [128, 8, R], BF16, kind="ExternalInput").ap()
    ogf = nc.dram_tensor("ogf", [R, 1024], F32, kind="ExternalInput").ap()
    ogb = nc.dram_tensor("ogb", [R, 1024], F32, kind="ExternalInput").ap()
    sr = nc.dram_tensor("sr", [R, 1024], F32, kind="ExternalInput").ap()
    x_in = nc.dram_tensor("x_in", [R, D], F32, kind="ExternalInput").ap()
    w_out = nc.dram_tensor("w_out", [D, D], F32, kind="ExternalInput").ap()
    w_r = nc.dram_tensor("w_r", [D, NE], F32, kind="ExternalInput").ap()
    vecs = nc.dram_tensor("vecs", [7, D], F32, kind="ExternalInput").ap()
    ggain = nc.dram_tensor("ggain", [1, 256], F32, kind="ExternalInput").ap()
    x_out = nc.dram_tensor("x_out", [R, D], F32, kind="ExternalOutput").ap()
    h2_out = nc.dram_tensor("h2_out", [R, D], BF16, kind="ExternalOutput").ap()
    aff_out = nc.dram_tensor("aff_out", [R, NE], F32, kind="ExternalOutput").ap()
    P = Prog(nc)
    identf = make_ident(P, "identf", F32)
    identb = P.sb("identb", [128, 128], BF16)
    P.op("vector", lambda e: e.tensor_copy(out=identb[:], in_=identf[:]), reads=[identf], writes=[identb])
    wo = P.sb("wo", [128, 16, D], BF16)
    wv = w_out.rearrange("(kc p) n -> p kc n", p=128)
    for q in range(16):
        P.dma("gpsimd", wo[:, q, :].rearrange("p (a n) -> p a n", a=4), wv[:, q, :].rearrange("p (a n) -> p a n", a=4),
              writes=[(wo, q)])
    wr = P.sb("wr", [128, 16, NE], F32)
    P.dma("sync", wr[:], w_r.rearrange("(kc p) n -> p kc n", p=128), writes=[wr])
    gg = P.sb("gg", [128, 256], F32)
    P.dma("sync", gg[:], ggain.partition_broadcast(128), writes=[gg])
    nf = P.sb("nf", [128, D], F32)
    P.dma("sync", nf[:], vecs[0:1, :].partition_broadcast(128), writes=[nf])
    g1 = P.sb("g1", [128, D], F32)
    A2 = P.sb("A2", [128, D], F32)
    B2 = P.sb("B2", [128, D], F32)

    def load_vecs(j):
        P.dma("sync", g1[:], vecs[1 + 3 * j:2 + 3 * j, :].partition_broadcast(128), writes=[g1])
        P.dma("sync", A2[:], vecs[2 + 3 * j:3 + 3 * j, :].partition_broadcast(128), writes=[A2])
        P.dma("sync", B2[:], vecs[3 + 3 * j:4 + 3 * j, :].partition_broadcast(128), writes=[B2])
        P.op("vector", lambda e: e.scalar_tensor_tensor(out=A2[:], in0=A2[:], scalar=1.0, in1=nf[:], op0=ALU.add, op1=ALU.mult),
             reads=[A2, nf], writes=[A2])

    xts = [P.sb("xt%d" % i, [128, D], F32) for i in range(2)]
    of_ts = [P.sb("of_t%d" % i, [128, 1024], F32) for i in range(2)]
    ob_ts = [P.sb("ob_t%d" % i, [128, 1024], F32) for i in range(2)]
    sr_ts = [P.sb("sr_t%d" % i, [128, 1024], F32) for i in range(2)]
    sq = P.sb("sq", [128, 1024], F32)
    gsss = [P.sb("gss%d" % i, [128, 4], F32) for i in range(2)]
    glb = P.sb("glb", [128, 1024], BF16)
    catT = [P.sb("catT%d" % i, [128, 16, 128], BF16) for i in range(2)]
    tpb = P.ps("tpb", [128, 8, 128], BF16)
    yps = [P.ps("yps%d" % i, [128, 512], F32) for i in range(2)]
    tpf = [P.ps("tpf%d" % i, [128, 4, 128], F32) for i in range(2)]
    lg_ps = P.ps("lg_ps", [128, NE], F32)
    ss2 = P.sb("ss2", [128, 1], F32)
    junk = P.sb("junk", [128, D], BF16)
    h2 = P.sb("h2", [128, D], F32)
    h2b = [P.sb("h2b%d" % i, [128, D], BF16) for i in range(2)]
    h2T = P.sb("h2T", [128, 16, 128], F32)
    mx = P.sb("mx", [128, 1], F32)
    ssum = P.sb("ssum", [128, 1], F32)
    ex = P.sb("ex", [128, NE], F32)
    affs = [P.sb("affs%d" % i, [128, NE], F32) for i in range(2)]
    tmp = P.sb("tmp", [128, 512], F32)
    itc = [0]

    def part1(t):
        rows = slice(t * 128, (t + 1) * 128)
        xt = xts[t % 2]
        ct = catT[t % 2]
        of_t, ob_t, sr_t, gss = of_ts[t % 2], ob_ts[t % 2], sr_ts[t % 2], gsss[t % 2]
        P.dma("sync", xt[:], x_in[rows, :], writes=[xt])
        P.dma("sync", ct[:, 0:8, :], attnT[:, :, rows], writes=[(ct, 0)])
        P.dma("sync", of_t[:], ogf[rows, :], writes=[of_t])
        P.dma("sync", ob_t[:], ogb[rows, :], writes=[ob_t])
        P.dma("sync", sr_t[:], sr[rows, :], writes=[sr_t])
        P.op("vector", lambda e: e.tensor_tensor(out=of_t[:], in0=of_t[:], in1=ob_t[:], op=ALU.add),
             reads=[of_t, ob_t], writes=[of_t])
        P.op("gpsimd", lambda e: e.tensor_tensor(out=sq[:], in0=of_t[:], in1=of_t[:], op=ALU.mult),
             reads=[of_t], writes=[sq])
        P.op("vector", lambda e: e.tensor_reduce(out=gss[:], in_=sq[:].rearrange("p (h d) -> p h d", h=4), axis=AX.X, op=ALU.add),
             reads=[sq], writes=[gss])
        emit_rsqrt(P, gss[:], gss[:], 1.0 / 256, gss, gss)
        o3 = of_t[:].rearrange("p (h d) -> p h d", h=4)
        P.op("gpsimd", lambda e: e.tensor_tensor(out=sr_t[:].rearrange("p (h d) -> p h d", h=4), in0=sr_t[:].rearrange("p (h d) -> p h d", h=4),
                                                 in1=gg[:].unsqueeze(1).broadcast_to([128, 4, 256]), op=ALU.mult),
             reads=[sr_t, gg], writes=[sr_t])
        P.op("vector", lambda e, o3=o3: e.tensor_tensor(out=o3, in0=o3, in1=gss[:].unsqueeze(2).broadcast_to([128, 4, 256]), op=ALU.mult),
             reads=[of_t, gss], writes=[of_t])
        P.op("vector", lambda e: e.tensor_tensor(out=glb[:], in0=of_t[:], in1=sr_t[:], op=ALU.mult),
             reads=[of_t, sr_t], writes=[glb])
        for j in range(8):
            P.op("tensor", lambda e, j=j: e.transpose(out=tpb[:, j, :], in_=glb[:, j * 128:(j + 1) * 128], identity=identb[:]),
                 reads=[glb, identb], writes=[tpb])
        P.op("scalar", lambda e, ct=ct: e.activation(out=ct[:, 8:16, :], in_=tpb[:], func=AF.Copy), reads=[tpb], writes=[(ct, 1)])

    def part2(t):
        rows = slice(t * 128, (t + 1) * 128)
        xt = xts[t % 2]
        ct = catT[t % 2]
        if t == 0:
            load_vecs(0)
        if t == NT - 1:
            load_vecs(1)
        for cb in range(4):
            yp = yps[itc[0] % 2]
            itc[0] += 1
            cs = slice(cb * 512, (cb + 1) * 512)
            for kc in range(16):
                P.op("tensor", lambda e, yp=yp, ct=ct, kc=kc, cs=cs: e.matmul(
                    yp[:], lhsT=ct[:, kc, :], rhs=wo[:, kc, cs], start=(kc == 0), stop=(kc == 15)),
                    reads=[(ct, kc // 8), (wo, kc)], writes=[yp])
            P.op("vector", lambda e, yp=yp, cs=cs: e.tensor_tensor(out=tmp[:], in0=yp[:], in1=g1[:, cs], op=ALU.mult),
                 reads=[yp, g1], writes=[tmp])
            P.op("gpsimd", lambda e, xt=xt, cs=cs: e.tensor_tensor(out=xt[:, cs], in0=xt[:, cs], in1=tmp[:], op=ALU.add),
                 reads=[tmp, xt], writes=[xt])
        P.dma("sync", x_out[rows, :], xt[:], reads=[xt])
        P.op("scalar", lambda e, xt=xt: e.activation(out=junk[:], in_=xt[:], func=AF.Square, accum_out=ss2[:]),
             reads=[xt], writes=[junk, ss2])
        emit_rsqrt(P, ss2[:], ss2[:], 1.0 / D, ss2, ss2)
        P.op("vector", lambda e, xt=xt: e.scalar_tensor_tensor(out=h2[:], in0=xt[:], scalar=ss2[:, 0:1], in1=A2[:], op0=ALU.mult, op1=ALU.mult),
             reads=[xt, ss2, A2], writes=[h2])
        P.op("gpsimd", lambda e: e.tensor_tensor(out=h2[:], in0=h2[:], in1=B2[:], op=ALU.add), reads=[h2, B2], writes=[h2])
        hb = h2b[t % 2]
        P.op("scalar", lambda e, hb=hb: e.activation(out=hb[:], in_=h2[:], func=AF.Copy), reads=[h2], writes=[hb])
        P.dma("sync", h2_out[rows, :], hb[:], reads=[hb])
        for q in range(4):
            tp = tpf[q % 2]
            for j in range(4):
                kc = q * 4 + j
                P.op("tensor", lambda e, tp=tp, j=j, kc=kc: e.transpose(out=tp[:, j, :], in_=h2[:, kc * 128:(kc + 1) * 128], identity=identf[:]),
                     reads=[h2, identf], writes=[tp])
            P.op("scalar", lambda e, tp=tp, q=q: e.activation(out=h2T[:, q * 4:(q + 1) * 4, :], in_=tp[:], func=AF.Copy),
                 reads=[tp], writes=[(h2T, q)])
        for kc in range(16):
            P.op("tensor", lambda e, kc=kc: e.matmul(lg_ps[:], lhsT=h2T[:, kc, :], rhs=wr[:, kc, :], start=(kc == 0), stop=(kc == 15)),
                 reads=[(h2T, kc // 4), wr], writes=[lg_ps])
        P.op("vector", lambda e: e.tensor_reduce(out=mx[:], in_=lg_ps[:], axis=AX.X, op=ALU.max), reads=[lg_ps], writes=[mx])
        P.op("vector", lambda e: e.tensor_scalar(out=mx[:], in0=mx[:], scalar1=-1.0, scalar2=None, op0=ALU.mult), reads=[mx], writes=[mx])
        P.op("scalar", lambda e: e.activation(out=ex[:], in_=lg_ps[:], func=AF.Exp, bias=mx[:, 0:1], accum_out=ssum[:]),
             reads=[lg_ps, mx], writes=[ex, ssum])
        P.op("vector", lambda e: e.reciprocal(out=ssum[:], in_=ssum[:]), reads=[ssum], writes=[ssum])
        af = affs[t % 2]
        P.op("vector", lambda e, af=af: e.tensor_scalar(out=af[:], in0=ex[:], scalar1=ssum[:, 0:1], scalar2=None, op0=ALU.mult),
             reads=[ex, ssum], writes=[af])
        P.dma("sync", aff_out[rows, :], af[:], reads=[af])

    part1(0)
    for t in range(NT):
        if t + 1 < NT:
            part1(t + 1)
        part2(t)
    P.finish()
    return nc


def run_kc(inputs, mods, l, attnT, og, sr_lat, sr_ctx, x_lat, x_ctx, nc=None):
    nc = nc or build_kc()
    m_lat, m_ctx = mods[l, 0], mods[l, 1]
    vecs = np.stack([inputs["norm_ffn"][l], m_lat[2 * D:3 * D], m_lat[4 * D:5 * D], m_lat[3 * D:4 * D],
                     m_ctx[2 * D:3 * D], m_ctx[4 * D:5 * D], m_ctx[3 * D:4 * D]]).astype(np.float32)
    ggain = np.ascontiguousarray(inputs["gla_gain"][l][None]).astype(np.float32)
    aT = np.stack(attnT, axis=1)
    ogf = np.concatenate(og[0:4], axis=1)
    ogb = np.concatenate(og[4:8], axis=1)
    w_out = np.ascontiguousarray(inputs["w_out"][l])
    w_r = np.ascontiguousarray(inputs["w_router"][l])
    maps = []
    for i in range(NCORES):
        tok = np.concatenate([CTX + np.arange(i * 1024, (i + 1) * 1024), (i % 2) * 128 + np.arange(128)])
        maps.append({"attnT": np.ascontiguousarray(aT[:, :, tok]), "ogf": ogf[tok], "ogb": ogb[tok],
                     "sr": core_tokens(sr_lat, sr_ctx, i), "x_in": core_tokens(x_lat, x_ctx, i),
                     "w_out": w_out, "w_r": w_r, "vecs": vecs, "ggain": ggain})
    res = run_bass_kernel_spmd(nc, maps, core_ids=list(range(NCORES)))
    return [(r["x_out"], r["h2_out"], r["aff_out"]) for r in res.results]


CAP = 2 * SEQ // NE
CCAP = 2 * CTX // NE
NSLOT = CAP + CCAP
ROWW = D
GW = 16
NBIS = 30
OOB0 = 4096


def build_kd():
    nc = bass.Bass("TRN2", target_bir_lowering=False)
    aff_l = nc.dram_tensor("aff_l", [128, 2, 64], F32, kind="ExternalInput").ap()
    aff_c = nc.dram_tensor("aff_c", [128, 2, 2], F32, kind="ExternalInput").ap()
    h2l = nc.dram_tensor("h2l", [SEQ, D], BF16, kind="ExternalInput").ap()
    h2c = nc.dram_tensor("h2c", [CTX, D], BF16, kind="ExternalInput").ap()
    wg = nc.dram_tensor("wg", [2, D, FF], F32, kind="ExternalInput").ap()
    wu = nc.dram_tensor("wu", [2, D, FF], F32, kind="ExternalInput").ap()
    wd = nc.dram_tensor("wd", [2, FF, D], F32, kind="ExternalInput").ap()
    xg = [nc.dram_tensor("xg%d" % i, [NSLOT, ROWW], BF16, kind="Internal").ap() for i in range(2)]
    out_e = nc.dram_tensor("out_e", [2, NSLOT, D], F32, kind="ExternalOutput").ap()
    pos_l = nc.dram_tensor("pos_l", [128, 2, 64], I32, kind="ExternalOutput").ap()
    pos_c = nc.dram_tensor("pos_c", [128, 2, 2], I32, kind="ExternalOutput").ap()
    P = Prog(nc)
    identf = make_ident(P, "identf", F32)
    identb = P.sb("identb", [128, 128], BF16)
    P.op("vector", lambda e: e.tensor_copy(out=identb[:], in_=identf[:]), reads=[identf], writes=[identb])
    ones = P.sb("ones_f", [128, 128], F32)
    P.op("gpsimd", lambda e: e.memset(ones[:], 1.0), writes=[ones])
    SU = P.sb("SU", [128, 128], F32)
    P.op("gpsimd", lambda e: e.memset(SU[:], 1.0), writes=[SU])
    P.op("gpsimd", lambda e: e.affine_select(out=SU[:], in_=SU[:], pattern=[[1, 128]], compare_op=ALU.is_gt, fill=0.0,
                                             base=0, channel_multiplier=-1), reads=[SU], writes=[SU])
    oobv = P.sb("oobv", [128, 1], F32)
    P.op("gpsimd", lambda e: e.iota(oobv[:], pattern=[[0, 1]], base=OOB0, channel_multiplier=1,
                                    allow_small_or_imprecise_dtypes=True), writes=[oobv])
    zeros = P.sb("zeros", [128, 64], F32)
    P.op("gpsimd", lambda e: e.memset(zeros[:], 0.0), writes=[zeros])
    affl = P.sb("affl", [128, 2, 64], F32)
    affc = P.sb("affc", [128, 2, 2], F32)
    P.dma("sync", affl[:], aff_l, writes=[affl])
    P.dma("sync", affc[:], aff_c, writes=[affc])
    pairs = [(affl, 0, 64, 0, CAP), (affl, 1, 64, 0, CAP), (affc, 0, 2, CAP, CCAP), (affc, 1, 2, CAP, CCAP)]
    kvec = P.sb("kvec", [128, 4], F32)
    P.op("gpsimd", lambda e: e.memset(kvec[:, 0:2], CAP - 0.5), writes=[kvec])
    P.op("gpsimd", lambda e: e.memset(kvec[:, 2:4], CCAP - 0.5), reads=[kvec], writes=[kvec])
    lo = P.sb("lo", [128, 4], F32)
    P.op("gpsimd", lambda e: e.memset(lo[:], 0.0), writes=[lo])
    mid = P.sb("mid", [128, 4], F32)
    pc = P.sb("pc", [128, 4], F32)
    ge = P.sb("ge", [128, 4], F32)
    junk = P.sb("junkb", [128, 64], F32)
    sm_ps = P.ps("sm_ps", [128, 512], F32)
    cnt_ps = PV(sm_ps, 0, 4)
    for k in range(NBIS):
        step = 2.0 ** -(k + 1)
        P.op("vector", lambda e, step=step: e.tensor_scalar(out=mid[:], in0=lo[:], scalar1=step, scalar2=None, op0=ALU.add),
             reads=[lo], writes=[mid])
        for j, (a, ee, n, base, cap) in enumerate(pairs):
            P.op("vector", lambda e, a=a, ee=ee, n=n, j=j: e.tensor_scalar(
                out=junk[:, :n], in0=a[:, ee, :], scalar1=mid[:, j:j + 1], scalar2=0.0, op0=ALU.is_ge, op1=ALU.add,
                accum_out=pc[:, j:j + 1]), reads=[a, mid], writes=[junk, (pc, j)])
        P.op("tensor", lambda e: e.matmul(cnt_ps[:], lhsT=ones[:], rhs=pc[:], start=True, stop=True),
             reads=[ones] + [(pc, j) for j in range(4)], writes=[cnt_ps])
        P.op("vector", lambda e: e.tensor_tensor(out=ge[:], in0=cnt_ps[:], in1=kvec[:], op=ALU.is_ge),
             reads=[cnt_ps, kvec], writes=[ge])
        P.op("vector", lambda e, step=step: e.scalar_tensor_tensor(out=lo[:], in0=ge[:], scalar=step, in1=lo[:],
                                                                    op0=ALU.mult, op1=ALU.add),
             reads=[ge, lo], writes=[lo])
    M = P.sb("Msel", [128, 64], F32)
    Tsb = P.sb("Tsb", [128, 64], F32)
    cum = P.sb("cum", [128, 64], F32)
    pos = P.sb("posf", [128, 64], F32)
    sel = P.sb("sel", [128, 64], F32)
    posl = P.sb("posl", [128, 2, 64], I32)
    posc = P.sb("posc", [128, 2, 2], I32)
    wi_ps, t_ps = PV(sm_ps, 64, 64), PV(sm_ps, 128, 64)
    for j, (a, ee, n, base, cap) in enumerate(pairs):
        dst = (posl if n == 64 else posc)
        P.op("vector", lambda e, a=a, ee=ee, n=n, j=j: e.tensor_scalar(
            out=M[:, :n], in0=a[:, ee, :], scalar1=lo[:, j:j + 1], scalar2=None, op0=ALU.is_ge), reads=[a, lo], writes=[M])
        P.op("tensor", lambda e, n=n: e.matmul(wi_ps[:, :n], lhsT=SU[:], rhs=M[:, :n], start=True, stop=True),
             reads=[SU, M], writes=[sm_ps])
        P.op("tensor", lambda e, n=n: e.matmul(t_ps[:, :n], lhsT=ones[:], rhs=M[:, :n], start=True, stop=True),
             reads=[ones, M], writes=[sm_ps])
        P.op("vector", lambda e, n=n: e.tensor_copy(out=Tsb[:, :n], in_=t_ps[:, :n]), reads=[sm_ps], writes=[Tsb])
        P.op("vector", lambda e, n=n: e.tensor_tensor_scan(out=cum[:, :n], data0=Tsb[:, :n], data1=zeros[:, :n], initial=0.0,
                                                            op0=ALU.add, op1=ALU.add), reads=[Tsb, zeros], writes=[cum])
        P.op("vector", lambda e, n=n: e.tensor_tensor(out=pos[:, :n], in0=wi_ps[:, :n], in1=cum[:, :n], op=ALU.add),
             reads=[sm_ps, cum], writes=[pos])
        P.op("vector", lambda e, n=n, base=base: e.scalar_tensor_tensor(out=pos[:, :n], in0=pos[:, :n], scalar=float(base), in1=Tsb[:, :n],
                                                                         op0=ALU.add, op1=ALU.subtract), reads=[pos, Tsb], writes=[pos])
        P.op("vector", lambda e, n=n, base=base, cap=cap: e.scalar_tensor_tensor(
            out=sel[:, :n], in0=pos[:, :n], scalar=float(base + cap) - 0.5, in1=M[:, :n], op0=ALU.is_lt, op1=ALU.mult),
            reads=[pos, M], writes=[sel])
        P.op("vector", lambda e, n=n: e.tensor_scalar(out=pos[:, :n], in0=pos[:, :n], scalar1=oobv[:, 0:1], scalar2=None, op0=ALU.subtract),
             reads=[pos, oobv], writes=[pos])
        P.op("vector", lambda e, n=n: e.tensor_tensor(out=pos[:, :n], in0=pos[:, :n], in1=sel[:, :n], op=ALU.mult),
             reads=[pos, sel], writes=[pos])
        P.op("vector", lambda e, n=n: e.tensor_scalar(out=pos[:, :n], in0=pos[:, :n], scalar1=oobv[:, 0:1], scalar2=None, op0=ALU.add),
             reads=[pos, oobv], writes=[pos])
        P.op("vector", lambda e, n=n, dst=dst, ee=ee: e.tensor_copy(out=dst[:, ee, :], in_=pos[:, :n]), reads=[pos], writes=[dst])
    P.dma("sync", pos_l, posl[:], reads=[posl])
    P.dma("sync", pos_c, posc[:], reads=[posc])
    rbs = [P.sb("rowbuf%d" % i, [128, ROWW], BF16) for i in range(4)]
    for n in range(66):
        rb = rbs[n % 4]
        if n < 64:
            src, a, pp, nn = h2l[n * 128:(n + 1) * 128, :], affl, posl, n
        else:
            src, a, pp, nn = h2c[(n - 64) * 128:(n - 63) * 128, :], affc, posc, n - 64
        P.dma("sync", rb[:, :D], src, writes=[(rb, 0)])
        for ee in range(2):
            P.dma_fn("gpsimd", lambda e, rb=rb, pp=pp, nn=nn, ee=ee: e.indirect_dma_start(
                out=xg[ee], out_offset=bass.IndirectOffsetOnAxis(ap=pp[:, ee, nn:nn + 1], axis=0),
                in_=rb[:, :], in_offset=None, bounds_check=P.reg(e, NSLOT - 1), oob_is_err=False),
                reads=[(rb, 0), pp], writes=[("xg", ee, n)])
    XgT = P.sb("XgT", [128, 16, NSLOT], BF16)
    hidT = P.sb("hidT", [128, 12, NSLOT], BF16)
    xts = [P.sb("xgt%d" % i, [128, ROWW], BF16) for i in range(2)]
    tpb = P.ps("tpb", [128, 8, 128], BF16)
    g_ps = [P.ps("g_ps%d" % i, [128, 512], F32) for i in range(2)]
    u_ps = [P.ps("u_ps%d" % i, [128, 512], F32) for i in range(2)]
    o_ps = [P.ps("o_ps%d" % i, [128, 512], F32) for i in range(2)]
    wgc = [P.sb("wgc%d" % i, [128, 16, 256], BF16) for i in range(2)]
    wuc = [P.sb("wuc%d" % i, [128, 16, 256], BF16) for i in range(2)]
    wdc = [P.sb("wdc%d" % i, [128, 12, 512], BF16) for i in range(2)]
    sgs = [P.sb("sgs%d" % i, [128, 512], F32) for i in range(2)]
    ost = [P.sb("ost%d" % i, [128, 512], F32) for i in range(2)]
    sblocks = [(0, 512), (512, 512), (1024, 32)]
    ih = io = iw = 0
    for ee in range(2):
        scat = [("xg", ee, n) for n in range(66)]
        for st in range(9):
            rows = 128 if st < 8 else CCAP
            xt = xts[st % 2]
            P.dma("sync", xt[:rows, :], xg[ee][st * 128:st * 128 + rows, :], reads=scat, writes=[xt])
            scat = []
            for half in range(2):
                for j in range(8):
                    kc = half * 8 + j
                    P.op("tensor", lambda e, xt=xt, rows=rows, j=j, kc=kc: e.transpose(
                        out=tpb[:, j, :rows], in_=xt[:rows, kc * 128:(kc + 1) * 128], identity=identb[:rows, :rows]),
                        reads=[xt, identb], writes=[tpb])
                eng = "scalar" if half == 0 else "vector"
                if eng == "scalar":
                    P.op("scalar", lambda e, rows=rows, half=half, st=st: e.activation(
                        out=XgT[:, half * 8:(half + 1) * 8, st * 128:st * 128 + rows], in_=tpb[:, :, :rows], func=AF.Copy),
                        reads=[tpb], writes=[(XgT, st)])
                else:
                    P.op("vector", lambda e, rows=rows, half=half, st=st: e.tensor_copy(
                        out=XgT[:, half * 8:(half + 1) * 8, st * 128:st * 128 + rows], in_=tpb[:, :, :rows]),
                        reads=[tpb], writes=[(XgT, st)])
        xkeys = [(XgT, st) for st in range(9)]
        for hc in range(6):
            wg_c, wu_c = wgc[iw % 2], wuc[iw % 2]
            iw += 1
            hs = slice(hc * 256, (hc + 1) * 256)
            for q in range(4):
                P.dma("gpsimd", wg_c[:, 4 * q:4 * q + 4, :], wg[ee].rearrange("(kc p) f -> p kc f", p=128)[:, 4 * q:4 * q + 4, hs],
                      writes=[(wg_c, q)])
                P.dma("gpsimd", wu_c[:, 4 * q:4 * q + 4, :], wu[ee].rearrange("(kc p) f -> p kc f", p=128)[:, 4 * q:4 * q + 4, hs],
                      writes=[(wu_c, q)])
            for fl in range(2):
                fc = hc * 2 + fl
                fs = slice(fl * 128, (fl + 1) * 128)
                for (s0, n) in sblocks:
                    gp, up, sg = g_ps[ih % 2], u_ps[ih % 2], sgs[ih % 2]
                    ih += 1
                    for kc in range(16):
                        P.op("tensor", lambda e, gp=gp, wg_c=wg_c, kc=kc, fs=fs, s0=s0, n=n: e.matmul(
                            gp[:, :n], lhsT=wg_c[:, kc, fs], rhs=XgT[:, kc, s0:s0 + n], start=(kc == 0), stop=(kc == 15)),
                            reads=[(wg_c, kc // 4)] + xkeys, writes=[gp])
                    for kc in range(16):
                        P.op("tensor", lambda e, up=up, wu_c=wu_c, kc=kc, fs=fs, s0=s0, n=n: e.matmul(
                            up[:, :n], lhsT=wu_c[:, kc, fs], rhs=XgT[:, kc, s0:s0 + n], start=(kc == 0), stop=(kc == 15)),
                            reads=[(wu_c, kc // 4)] + xkeys, writes=[up])
                    P.op("scalar", lambda e, gp=gp, sg=sg, n=n: e.activation(out=sg[:, :n], in_=gp[:, :n], func=AF.Silu),
                         reads=[gp], writes=[sg])
                    P.op("vector", lambda e, up=up, sg=sg, n=n, fc=fc, s0=s0: e.tensor_tensor(
                        out=hidT[:, fc, s0:s0 + n], in0=up[:, :n], in1=sg[:, :n], op=ALU.mult),
                        reads=[up, sg], writes=[(hidT, fc)])
        hkeys = [(hidT, fc) for fc in range(12)]
        for cb in range(4):
            wd_c = wdc[(ee * 4 + cb) % 2]
            cs = slice(cb * 512, (cb + 1) * 512)
            for q in range(3):
                P.dma("gpsimd", wd_c[:, 4 * q:4 * q + 4, :], wd[ee].rearrange("(fc p) n -> p fc n", p=128)[:, 4 * q:4 * q + 4, cs],
                      writes=[(wd_c, q)])
            for st in range(9):
                rows = 128 if st < 8 else CCAP
                op_, os_ = o_ps[io % 2], ost[io % 2]
                io += 1
                for fc in range(12):
                    P.op("tensor", lambda e, op_=op_, wd_c=wd_c, fc=fc, st=st, rows=rows: e.matmul(
                        op_[:rows, :], lhsT=hidT[:, fc, st * 128:st * 128 + rows], rhs=wd_c[:, fc, :],
                        start=(fc == 0), stop=(fc == 11)), reads=[(wd_c, fc // 4)] + hkeys, writes=[op_])
                P.op("scalar", lambda e, op_=op_, os_=os_, rows=rows, ee=ee, st=st: e.activation(
                    out=os_[:rows, :], in_=op_[:rows, :], func=AF.Copy),
                    reads=[op_], writes=[os_])
                P.dma("sync", out_e[ee, st * 128:st * 128 + rows, cs], os_[:rows, :], reads=[os_])
    P.finish()
    return nc


def run_kd(inputs, l, aff_lat, aff_ctx, h2_lat, h2_ctx, nc=None):
    nc = nc or build_kd()
    maps = []
    for i in range(NCORES):
        es = slice(2 * i, 2 * i + 2)
        maps.append({
            "aff_l": np.ascontiguousarray(aff_lat[:, es].reshape(64, 128, 2).transpose(1, 2, 0)),
            "aff_c": np.ascontiguousarray(aff_ctx[:, es].reshape(2, 128, 2).transpose(1, 2, 0)),
            "h2l": h2_lat, "h2c": h2_ctx,
            "wg": np.ascontiguousarray(inputs["w_gate"][l, es]), "wu": np.ascontiguousarray(inputs["w_up"][l, es]),
            "wd": np.ascontiguousarray(inputs["w_down"][l, es]),
        })
    res = run_bass_kernel_spmd(nc, maps, core_ids=list(range(NCORES)))
    return [(r["out_e"], r["pos_l"], r["pos_c"]) for r in res.results]


def build_kf():
    nc = bass.Bass("TRN2", target_bir_lowering=False)
    x_in = nc.dram_tensor("x_in", [NT * 128, D], F32, kind="ExternalInput").ap()
    fnv = nc.dram_tensor("fnv", [1, D], F32, kind="ExternalInput").ap()
    y = nc.dram_tensor("y", [(NT - 1) * 128, D], F32, kind="ExternalOutput").ap()
    P = Prog(nc)
    comb = Combiner(P, *declare_combine_inputs(nc))
    fn_s = P.sb("fn_s", [128, D], F32)
    P.dma("sync", fn_s[:], fnv.partition_broadcast(128), writes=[fn_s])
    xts = [P.sb("xt%d" % i, [128, D], F32) for i in range(2)]
    junk = P.sb("junk", [128, D], BF16)
    ss = P.sb("ss", [128, 1], F32)
    for t in range(NT - 1):
        xt = xts[t % 2]
        rows = slice(t * 128, (t + 1) * 128)
        P.dma("sync", xt[:], x_in[rows, :], writes=[xt])
        comb.emit(t, xt)
        P.op("scalar", lambda e, xt=xt: e.activation(out=junk[:], in_=xt[:], func=AF.Square, accum_out=ss[:]),
             reads=[xt], writes=[junk, ss])
        emit_rsqrt(P, ss[:], ss[:], 1.0 / D, ss, ss)
        P.op("vector", lambda e, xt=xt: e.scalar_tensor_tensor(out=xt[:], in0=xt[:], scalar=ss[:, 0:1], in1=fn_s[:],
                                                                op0=ALU.mult, op1=ALU.mult), reads=[xt, ss, fn_s], writes=[xt])
        P.dma("sync", y[rows, :], xt[:], reads=[xt])
    P.finish()
    return nc


def run_kf(inputs, x_lat, x_ctx, comb_maps, nc=None):
    nc = nc or build_kf()
    fnv = np.ascontiguousarray(inputs["final_norm"][None]).astype(np.float32)
    maps = []
    for i in range(NCORES):
        m = {"x_in": core_tokens(x_lat, x_ctx, i), "fnv": fnv}
        m.update(comb_maps[i])
        maps.append(m)
    res = run_bass_kernel_spmd(nc, maps, core_ids=list(range(NCORES)))
    return np.concatenate([r["y"] for r in res.results], axis=0)


_NC = {}


def _nc(name, fn):
    if name not in _NC:
        _NC[name] = fn()
    return _NC[name]


def kernel(**inputs):
    inputs = {k: np.asarray(v) for k, v in inputs.items()}
    mods = run_kmod(inputs)
    x_lat = np.ascontiguousarray(inputs["x"][0])
    x_ctx = np.ascontiguousarray(inputs["ctx"][0])
    comb = None
    for l in range(DEPTH):
        if comb is None:
            ka = run_ka(inputs, mods, l, x_lat, x_ctx, nc=_nc("ka0", lambda: build_ka(False)))
        else:
            ka = run_ka(inputs, mods, l, x_lat, x_ctx, nc=_nc("ka1", lambda: build_ka(True)), comb_maps=comb)
            x_lat, x_ctx = gather_tokens([o[2] for o in ka])
        bf_lat, bf_ctx = gather_tokens([o[0] for o in ka])
        f_lat, f_ctx = gather_tokens([o[1] for o in ka])
        attnT, og = run_kb(bf_lat, bf_ctx, f_lat, f_ctx, nc=_nc("kb", build_kb))
        kc = run_kc(inputs, mods, l, attnT, og, f_lat[:, 1024:2048], f_ctx[:, 1024:2048], x_lat, x_ctx,
                    nc=_nc("kc", build_kc))
        x_lat, x_ctx = gather_tokens([o[0] for o in kc])
        h_lat, h_ctx = gather_tokens([o[1] for o in kc])
        a_lat, a_ctx = gather_tokens([o[2] for o in kc])
        kd = run_kd(inputs, l, a_lat, a_ctx, h_lat, h_ctx, nc=_nc("kd", build_kd))
        comb = combine_maps(mods, l, kd, a_lat, a_ctx)
    y = run_kf(inputs, x_lat, x_ctx, comb, nc=_nc("kf", build_kf))
    return np.ascontiguousarray(y[None]).astype(np.float32)
```

```python
import numpy as np
from contextlib import ExitStack
import concourse.bass as bass
import concourse.mybir as mybir
from concourse.bass_utils import run_bass_kernel_spmd

F32 = mybir.dt.float32
BF16 = mybir.dt.bfloat16
I32 = mybir.dt.int32
U32 = mybir.dt.uint32
AF = mybir.ActivationFunctionType
ALU = mybir.AluOpType
AX = mybir.AxisListType

ENGINES = ["sync", "scalar", "vector", "gpsimd", "tensor"]
SEM_EPOCH = 20000
NDSLOT = 6


class Prog:
    def __init__(self, nc, same_engine_sync=True):
        self.nc = nc
        self.stack = ExitStack()
        self.items = {e: [] for e in ENGINES}
        self.cur = {}
        self.sems = {}
        self.known = {e: {} for e in ENGINES}
        self.lastw = {}
        self.readers = {}
        self.same_engine_sync = same_engine_sync
        self.final = {}
        self.nsem = 0
        self.drr = {}

    def const_eps(self):
        if not hasattr(self, "_epsb"):
            self._epsb = self.sb("epsb", [128, 1], F32)
            self.op("gpsimd", lambda e: e.memset(self._epsb[:], EPS), writes=[self._epsb])
        return self._epsb

    def reg(self, e, val):
        if not hasattr(self, "_regs"):
            self._regs = {}
        if val not in self._regs:
            self._regs[val] = e.to_reg(val)
        return self._regs[val]

    def sb(self, name, shape, dt):
        return self.stack.enter_context(self.nc.sbuf_tensor(name, shape, dt))

    def ps(self, name, shape, dt):
        return self.stack.enter_context(self.nc.psum_tensor(name, shape, dt))

    def _sem(self, key):
        if key not in self.sems:
            self.nsem += 1
            self.sems[key] = self.stack.enter_context(
                self.nc.semaphore("s_%s_%s_%d" % key))
        return self.sems[key]

    def _bump(self, kind, eng, amount):
        st = self.cur.setdefault((kind, eng), [0, 0])
        if st[1] + amount > SEM_EPOCH:
            st[0] += 1
            st[1] = 0
        st[1] += amount
        key = (kind, eng, st[0])
        self._sem(key)
        self.final[key] = st[1]
        return key, st[1]

    @staticmethod
    def _k(x):
        if isinstance(x, (str, int)):
            return x
        if isinstance(x, tuple):
            return tuple(Prog._k(y) for y in x)
        return x.name

    def _deps(self, eng, reads, writes):
        reads = [self._k(r) for r in reads]
        writes = [self._k(w) for w in writes]
        need = {}

        def add(dep):
            k, c = dep
            if need.get(k, 0) < c:
                need[k] = c
        for r in reads:
            if r in self.lastw:
                add(self.lastw[r])
        for w in writes:
            if w in self.lastw:
                add(self.lastw[w])
            for d in self.readers.get(w, ()):
                add(d)
        waits = []
        for k, c in need.items():
            kind, e2, _ = k
            if kind == "c" and e2 == eng:
                if eng == "tensor" or not self.same_engine_sync:
                    continue
            if self.known[eng].get(k, 0) >= c:
                continue
            self.known[eng][k] = c
            waits.append((k, c))
        return waits

    def _record(self, reads, writes, dep):
        reads = [self._k(r) for r in reads]
        writes = [self._k(w) for w in writes]
        for r in reads:
            self.readers.setdefault(r, []).append(dep)
        for w in writes:
            self.lastw[w] = dep
            self.readers[w] = []

    def op(self, eng, fn, reads=(), writes=()):
        waits = self._deps(eng, reads, writes)
        key, c = self._bump("c", eng, 1)
        self.items[eng].append((waits, fn, key, 1))
        self._record(reads, writes, (key, c))

    def _dma_slot(self, eng, waits):
        rr = self.drr.get(eng, 0)
        self.drr[eng] = (rr + 1) % NDSLOT
        slot = "%s%d" % (eng, rr)
        st = self.cur.setdefault(("d", slot), [0, 0])
        if st[1] > 0:
            pk = ("d", slot, st[0])
            if self.known[eng].get(pk, 0) < st[1]:
                self.known[eng][pk] = st[1]
                waits.append((pk, st[1]))
        return self._bump("d", slot, 16)

    def dma(self, eng, out, in_, reads=(), writes=(), out_final=False, **kw):
        return self.dma_fn(eng, lambda e: e.dma_start(out=out, in_=in_, **kw), reads, writes, out_final)

    def dma_fn(self, eng, fn, reads=(), writes=(), out_final=False):
        waits = self._deps(eng, reads, writes)
        key, c = self._dma_slot(eng, waits)
        self.items[eng].append((waits, fn, key, 16))
        self._record(reads, writes, (key, c))

    def finish(self):
        nc = self.nc
        fin = dict(self.final)
        items = self.items
        sems = self.sems

        def emit(eng_name, e, extra=None):
            for waits, fn, key, inc in items[eng_name]:
                for k, c in waits:
                    e.wait_ge(sems[k], c)
                ins = fn(e)
                ins.then_inc(sems[key], inc)
            if extra:
                for k, c in extra.items():
                    e.wait_ge(sems[k], c)

        with nc.Block() as block:
            @block.sync
            def _(e):
                emit("sync", e, fin)

            @block.scalar
            def _(e):
                emit("scalar", e)

            @block.vector
            def _(e):
                emit("vector", e)

            @block.gpsimd
            def _(e):
                emit("gpsimd", e)

            @block.tensor
            def _(e):
                emit("tensor", e)
        self.stack.close()


def kernel(**inputs):
    raise NotImplementedError


D = 2048
SEQ = 8192
CTX = 256
DEPTH = 4
NCORES = 8
INW = 4640
EPS = 1e-6
NT = 9
NE = 16
FF = 1536


def make_ident(P, name, dt):
    idf = P.sb(name + "_f", [128, 128], F32)
    P.op("gpsimd", lambda e: e.memset(idf[:], 1.0), writes=[idf])
    P.op("gpsimd", lambda e: e.affine_select(out=idf[:], in_=idf[:], pattern=[[-1, 128]],
                                             compare_op=ALU.is_equal, fill=0.0, base=0,
                                             channel_multiplier=1), reads=[idf], writes=[idf])
    if dt == F32:
        return idf
    idn = P.sb(name, [128, 128], dt)
    P.op("vector", lambda e: e.tensor_copy(out=idn[:], in_=idf[:]), reads=[idf], writes=[idn])
    return idn


def build_kmod():
    nc = bass.Bass("TRN2", target_bir_lowering=False)
    CW = 6 * D // NCORES
    cc = nc.dram_tensor("cc", [128, 16, 2], F32, kind="ExternalInput").ap()
    wm = nc.dram_tensor("wm", [DEPTH, D, CW], F32, kind="ExternalInput").ap()
    bm = nc.dram_tensor("bm", [DEPTH, 1, CW], F32, kind="ExternalInput").ap()
    out = nc.dram_tensor("mods", [DEPTH, 2, CW], F32, kind="ExternalOutput").ap()
    P = Prog(nc)
    cs = P.sb("cs", [128, 16, 2], F32)
    sc = P.sb("scs", [128, 16, 2], F32)
    P.dma("sync", cs[:], cc, writes=[cs])
    P.op("scalar", lambda e: e.activation(out=sc[:], in_=cs[:], func=AF.Silu), reads=[cs], writes=[sc])
    wts = [P.sb("w%d" % i, [128, 16, 512], F32) for i in range(2)]
    pss = [P.ps("ps%d" % i, [2, 512], F32) for i in range(2)]
    bias = P.sb("bias", [2, DEPTH, CW], F32)
    for r in range(2):
        P.dma("sync", bias[r:r + 1, :, :], bm.rearrange("l o n -> o l n"), writes=[(bias, r)])
    res = P.sb("res", [2, DEPTH, CW], F32)
    it = 0
    for l in range(DEPTH):
        for cb in range(CW // 512):
            w = wts[it % 2]
            ps = pss[it % 2]
            P.dma("sync" if it % 2 == 0 else "scalar", w[:],
                  wm[l].rearrange("(kc p) n -> p kc n", p=128)[:, :, cb * 512:(cb + 1) * 512],
                  writes=[w])
            for kc in range(16):
                P.op("tensor", lambda e, w=w, ps=ps, kc=kc: e.matmul(
                    ps[:], lhsT=sc[:, kc, :], rhs=w[:, kc, :], start=(kc == 0), stop=(kc == 15)),
                    reads=[sc, w], writes=[ps])
            P.op("vector", lambda e, ps=ps, l=l, cb=cb: e.tensor_tensor(
                out=res[:, l, cb * 512:(cb + 1) * 512], in0=ps[:], in1=bias[:, l, cb * 512:(cb + 1) * 512],
                op=ALU.add), reads=[ps, (bias, 0), (bias, 1)], writes=[(res, it)])
            it += 1
    P.dma("sync", out.rearrange("l r n -> r l n"), res[:], reads=[(res, i) for i in range(it)],
          out_final=True)
    P.finish()
    return nc


def run_kmod(inputs):
    c2 = np.stack([inputs["c"][0], inputs["c_ctx"]], axis=-1)
    cc = np.ascontiguousarray(c2.reshape(16, 128, 2).transpose(1, 0, 2))
    CW = 6 * D // NCORES
    nc = build_kmod()
    maps = []
    for i in range(NCORES):
        maps.append({
            "cc": cc,
            "wm": np.ascontiguousarray(inputs["w_mod"][:, :, i * CW:(i + 1) * CW]),
            "bm": np.ascontiguousarray(inputs["b_mod"][:, None, i * CW:(i + 1) * CW]),
        })
    res = run_bass_kernel_spmd(nc, maps, core_ids=list(range(NCORES)))
    mods = np.concatenate([r["mods"] for r in res.results], axis=-1)
    return mods


OBF_W = 2560
OF_W = 3072


def emit_rsqrt(P, out, in_, scale, rkey, wkey):
    epsb = P.const_eps()
    n = in_.shape[0]
    P.op("scalar", lambda e: e.activation(out=out, in_=in_, func=AF.Ln, scale=scale, bias=epsb[:n, :]),
         reads=[rkey, epsb], writes=[wkey])
    P.op("scalar", lambda e: e.activation(out=out, in_=out, func=AF.Exp, scale=-0.5),
         reads=[wkey], writes=[wkey])


def emit_modvec(P, vec, name):
    A = P.sb(name, [128, 2, 16], F32)
    for j in range(2):
        P.op("vector", lambda e, j=j: e.scalar_tensor_tensor(
            out=A[:, j, :], in0=vec[:, 1 + 2 * j, :], scalar=1.0, in1=vec[:, 0, :],
            op0=ALU.add, op1=ALU.mult), reads=[vec], writes=[A])
    return A


def declare_combine_inputs(nc):
    oes = [nc.dram_tensor("oe%d" % e, [NSLOT, D], F32, kind="ExternalInput").ap() for e in range(NE)]
    cpos = nc.dram_tensor("cpos", [128, NT, NE], I32, kind="ExternalInput").ap()
    caff = nc.dram_tensor("caff", [128, NT, NE], F32, kind="ExternalInput").ap()
    g2v = nc.dram_tensor("g2v", [2, D], F32, kind="ExternalInput").ap()
    return oes, cpos, caff, g2v


class Combiner:
    def __init__(self, P, oes, cpos, caff, g2v):
        self.P, self.oes, self.g2v = P, oes, g2v
        self.pos = P.sb("cpos_s", [128, NT, NE], I32)
        P.dma("sync", self.pos[:], cpos, writes=[self.pos])
        self.aff = P.sb("caff_s", [128, NT, NE], F32)
        P.dma("sync", self.aff[:], caff, writes=[self.aff])
        selm = P.sb("cselm", [128, NT, NE], F32)
        P.op("vector", lambda e: e.tensor_scalar(out=selm[:], in0=self.pos[:], scalar1=float(NSLOT) - 0.5, scalar2=None,
                                                 op0=ALU.is_lt), reads=[self.pos], writes=[selm])
        P.op("vector", lambda e: e.tensor_tensor(out=self.aff[:], in0=self.aff[:], in1=selm[:], op=ALU.mult),
             reads=[self.aff, selm], writes=[self.aff])
        self.g2 = P.sb("g2_s", [128, D], F32)
        self.acc = P.sb("cacc", [128, D], F32)
        self.gbl = [P.sb("cgb%d" % i, [128, D], F32) for i in range(4)]
        for gb in self.gbl:
            P.op("gpsimd", lambda e, gb=gb: e.memset(gb[:], 0.0), writes=[gb])
        self.n = 0
        self.g2_loaded = None

    def emit(self, t, xt):
        P = self.P
        j = 1 if t == NT - 1 else 0
        if self.g2_loaded != j:
            P.dma("sync", self.g2[:], self.g2v[j:j + 1, :].partition_broadcast(128), writes=[self.g2])
            self.g2_loaded = j
        acc = self.acc
        for e_ in range(NE):
            gb = self.gbl[self.n % 4]
            self.n += 1
            P.dma_fn("gpsimd", lambda e, gb=gb, e_=e_, t=t: e.indirect_dma_start(
                out=gb[:, :], out_offset=None, in_=self.oes[e_],
                in_offset=bass.IndirectOffsetOnAxis(ap=self.pos[:, t, e_:e_ + 1], axis=0),
                bounds_check=P.reg(e, NSLOT - 1), oob_is_err=False), reads=[self.pos, gb], writes=[gb])
            gate = self.aff[:, t, e_:e_ + 1]
            if e_ == 0:
                P.op("vector", lambda e, gb=gb, gate=gate: e.tensor_scalar(
                    out=acc[:], in0=gb[:], scalar1=gate, scalar2=None, op0=ALU.mult), reads=[gb, self.aff], writes=[acc])
            else:
                P.op("vector", lambda e, gb=gb, gate=gate: e.scalar_tensor_tensor(
                    out=acc[:], in0=gb[:], scalar=gate, in1=acc[:], op0=ALU.mult, op1=ALU.add),
                    reads=[gb, acc, self.aff], writes=[acc])
        P.op("vector", lambda e: e.tensor_tensor(out=self.acc[:], in0=self.acc[:], in1=self.g2[:], op=ALU.mult),
             reads=[self.acc, self.g2], writes=[self.acc])
        P.op("vector", lambda e, xt=xt: e.tensor_tensor(out=xt[:], in0=xt[:], in1=self.acc[:], op=ALU.add),
             reads=[xt, self.acc], writes=[xt])


def build_ka(combine=False):
    nc = bass.Bass("TRN2", target_bir_lowering=False)
    x_in = nc.dram_tensor("x_in", [NT * 128, D], F32, kind="ExternalInput").ap()
    w_in = nc.dram_tensor("w_in", [D, INW], F32, kind="ExternalInput").ap()
    vecs = nc.dram_tensor("vecs", [128, 5, 16], F32, kind="ExternalInput").ap()
    gains = nc.dram_tensor("gains", [128, 2, 128], F32, kind="ExternalInput").ap()
    rope = nc.dram_tensor("rope", [128, NT, 2, 64], F32, kind="ExternalInput").ap()
    wa2 = nc.dram_tensor("wa2", [16, 2, 512], F32, kind="ExternalInput").ap()
    ba = nc.dram_tensor("ba", [1, 2, 512], F32, kind="ExternalInput").ap()
    out_bf = nc.dram_tensor("out_bf", [NT * 128, OBF_W], BF16, kind="ExternalOutput").ap()
    out_f = nc.dram_tensor("out_f", [NT * 128, OF_W], F32, kind="ExternalOutput").ap()
    P = Prog(nc)
    comb = x_new = None
    if combine:
        comb = Combiner(P, *declare_combine_inputs(nc))
        x_new = nc.dram_tensor("x_new", [NT * 128, D], F32, kind="ExternalOutput").ap()
    emit_ka_body(P, x_in, w_in, vecs, gains, rope, wa2, ba, out_bf, out_f, comb, x_new)
    P.finish()
    return nc


def emit_ka_body(P, x_in, w_in, vecs, gains, rope, wa2, ba, out_bf, out_f, comb=None, x_new=None):
    identf = make_ident(P, "identf", F32)
    vec = P.sb("vec", [128, 5, 16], F32)
    P.dma("sync", vec[:], vecs, writes=[vec])
    A = emit_modvec(P, vec, "Amod")
    gn = P.sb("gn", [128, 2, 128], F32)
    P.dma("sync", gn[:], gains, writes=[gn])
    rp = P.sb("rp", [128, NT, 2, 64], F32)
    P.dma("sync", rp[:], rope, writes=[rp])
    wa2s = P.sb("wa2s", [16, 2, 512], F32)
    P.dma("sync", wa2s[:], wa2, writes=[wa2s])
    bas = P.sb("bas", [1, 2, 512], F32)
    P.dma("sync", bas[:], ba, writes=[bas])
    ones1 = P.sb("ones1", [1, 128], F32)
    P.op("gpsimd", lambda e: e.memset(ones1[:], 1.0), writes=[ones1])

    hT = P.sb("hT", [128, NT, 16, 128], BF16)
    xts = [P.sb("xt%d" % i, [128, D], F32) for i in range(2)]
    junk = P.sb("junk", [128, D], BF16)
    ss = P.sb("ss", [128, NT], F32)
    rstd = P.sb("rstd", [128, NT], F32)
    tps = [P.ps("tp%d" % i, [128, 8, 128], F32) for i in range(2)]
    def phase1(t):
        xt = xts[t % 2]
        P.dma("sync", xt[:], x_in[t * 128:(t + 1) * 128, :], writes=[xt])
        if comb is not None:
            comb.emit(t, xt)
            P.dma("sync", x_new[t * 128:(t + 1) * 128, :], xt[:], reads=[xt])
        P.op("scalar", lambda e, xt=xt, t=t: e.activation(
            out=junk[:], in_=xt[:], func=AF.Square, accum_out=ss[:, t:t + 1]),
            reads=[xt], writes=[junk, (ss, t)])
        emit_rsqrt(P, rstd[:, t:t + 1], ss[:, t:t + 1], 1.0 / D, (ss, t), (rstd, t))
        P.op("vector", lambda e, xt=xt, t=t: e.tensor_scalar(
            out=xt[:], in0=xt[:], scalar1=rstd[:, t:t + 1], scalar2=None, op0=ALU.mult),
            reads=[xt, (rstd, t)], writes=[xt])
        mi = 1 if t == NT - 1 else 0
        for half in range(2):
            tp = tps[half]
            for j in range(8):
                kc = half * 8 + j
                P.op("tensor", lambda e, tp=tp, j=j, kc=kc, xt=xt: e.transpose(
                    out=tp[:, j, :], in_=xt[:, kc * 128:(kc + 1) * 128], identity=identf[:]),
                    reads=[xt, identf], writes=[tp])
            for j in range(8):
                kc = half * 8 + j
                P.op("scalar", lambda e, tp=tp, j=j, kc=kc, t=t, mi=mi: e.activation(
                    out=hT[:, t, kc, :], in_=tp[:, j, :], func=AF.Identity,
                    scale=A[:, mi, kc:kc + 1], bias=vec[:, 2 + 2 * mi, kc:kc + 1]),
                    reads=[tp, A, vec], writes=[(hT, t)])

    for t in range(NT):
        phase1(t)

    wbs = [P.sb("wb%d" % i, [128, 16, 512], BF16) for i in range(2)]
    mps = [P.ps("mp%d" % i, [128, 512], F32) for i in range(2)]
    zp = P.ps("zp", [128, 512], F32)
    tpa = P.ps("tpa", [16, 128], F32)
    sq = P.sb("sq", [128, 512], F32)
    ssq = P.sb("ssq", [128, 4], F32)
    qn = P.sb("qn", [128, 512], F32)
    rt = [P.sb("rt%d" % i, [128, 256], F32) for i in range(2)]
    sbf = [P.sb("sbf%d" % i, [128, 512], BF16) for i in range(4)]
    sf = [P.sb("sf%d" % i, [128, 512], F32) for i in range(4)]
    asb = P.sb("asb", [128, 32], F32)
    aT = P.sb("aT", [16, 128], F32)
    ez = P.sb("ez", [128, 512], F32)
    cnt = {"bf": 0, "f": 0}
    w_v = w_in.rearrange("(kc p) n -> p kc n", p=128)

    def stage(kind):
        i = cnt[kind]
        cnt[kind] += 1
        return (sbf if kind == "bf" else sf)[i % 4]

    def qk_epi(mp, c0, nh, gi, t, dst):
        w = nh * 128
        P.op("scalar", lambda e: e.activation(out=sq[:, :w], in_=mp[:, c0:c0 + w], func=AF.Square),
             reads=[mp], writes=[sq])
        P.op("vector", lambda e: e.tensor_reduce(
            out=ssq[:, :nh], in_=sq[:, :w].rearrange("p (h d) -> p h d", h=nh), axis=AX.X, op=ALU.add),
            reads=[sq], writes=[ssq])
        emit_rsqrt(P, ssq[:, :nh], ssq[:, :nh], 1.0 / 128, ssq, ssq)
        q3 = qn[:, :w].rearrange("p (h d) -> p h d", h=nh)
        P.op("vector", lambda e: e.tensor_tensor(
            out=q3, in0=mp[:, c0:c0 + w].rearrange("p (h d) -> p h d", h=nh),
            in1=ssq[:, :nh].unsqueeze(2).broadcast_to([128, nh, 128]), op=ALU.mult),
            reads=[mp, ssq], writes=[qn])
        P.op("vector", lambda e: e.tensor_tensor(
            out=q3, in0=q3, in1=gn[:, gi, :].unsqueeze(1).broadcast_to([128, nh, 128]), op=ALU.mult),
            reads=[qn, gn], writes=[qn])
        q4 = qn[:, :w].rearrange("p (h i two) -> p h i two", h=nh, two=2)
        d4 = dst.rearrange("p (h i two) -> p h i two", h=nh, two=2)
        cosb = rp[:, t, 0, :].unsqueeze(1).broadcast_to([128, nh, 64])
        sinb = rp[:, t, 1, :].unsqueeze(1).broadcast_to([128, nh, 64])
        r0 = rt[0][:, :nh * 64].rearrange("p (h i) -> p h i", h=nh)
        r1 = rt[1][:, :nh * 64].rearrange("p (h i) -> p h i", h=nh)
        for (o, a, ca, b, cb_, op) in ((0, 0, cosb, 1, sinb, ALU.subtract), (1, 0, sinb, 1, cosb, ALU.add)):
            P.op("vector", lambda e, a=a, ca=ca: e.tensor_tensor(out=r0, in0=q4[:, :, :, a], in1=ca, op=ALU.mult),
                 reads=[qn, rp], writes=[rt[0]])
            P.op("gpsimd", lambda e, b=b, cb_=cb_: e.tensor_tensor(out=r1, in0=q4[:, :, :, b], in1=cb_, op=ALU.mult),
                 reads=[qn, rp], writes=[rt[1]])
            P.op("vector", lambda e, o=o, op=op: e.tensor_tensor(out=d4[:, :, :, o], in0=r0, in1=r1, op=op),
                 reads=[rt[0], rt[1]], writes=[dst.tensor])

    it = 0
    for cb in range(10):
        n = 512 if cb < 9 else 32
        wb = wbs[cb % 2]
        for q in range(4):
            P.dma("gpsimd", wb[:, 4 * q:4 * q + 4, :n], w_v[:, 4 * q:4 * q + 4, cb * 512:cb * 512 + n],
                  writes=[(wb, q)])
        for t in range(NT):
            mp = mps[it % 2]
            it += 1
            for kc in range(16):
                P.op("tensor", lambda e, mp=mp, t=t, kc=kc, wb=wb, n=n: e.matmul(
                    mp[:, :n], lhsT=hT[:, t, kc, :], rhs=wb[:, kc, :n], start=(kc == 0), stop=(kc == 15)),
                    reads=[(hT, t), (wb, kc // 4)], writes=[mp])
            rows = slice(t * 128, (t + 1) * 128)
            if cb in (0, 1):
                st = stage("bf")
                qk_epi(mp, 0, 4, 0, t, st[:, :])
                P.dma("sync", out_bf[rows, cb * 512:(cb + 1) * 512], st[:], reads=[st])
            elif cb == 2:
                st = stage("bf")
                qk_epi(mp, 0, 2, 1, t, st[:, 0:256])
                P.op("scalar", lambda e, st=st, mp=mp: e.activation(out=st[:, 256:512], in_=mp[:, 256:512], func=AF.Copy),
                     reads=[mp], writes=[st])
                P.dma("sync", out_bf[rows, 1024:1536], st[:], reads=[st])
            elif cb in (3, 4):
                st = stage("f")
                sc_ = 128 ** -0.5 if cb == 3 else 1.0
                P.op("scalar", lambda e, st=st, mp=mp, sc_=sc_: e.activation(out=st[:], in_=mp[:], func=AF.Copy, scale=sc_),
                     reads=[mp], writes=[st])
                P.dma("sync", out_f[rows, (cb - 3) * 512:(cb - 2) * 512], st[:], reads=[st])
            elif cb in (5, 6):
                st = stage("bf")
                P.op("scalar", lambda e, st=st, mp=mp: e.activation(out=st[:], in_=mp[:], func=AF.Copy),
                     reads=[mp], writes=[st])
                P.dma("sync", out_bf[rows, 1536 + (cb - 5) * 512:1536 + (cb - 4) * 512], st[:], reads=[st])
            elif cb in (7, 8):
                st = stage("f")
                P.op("scalar", lambda e, st=st, mp=mp: e.activation(out=st[:], in_=mp[:], func=AF.Silu),
                     reads=[mp], writes=[st])
                P.dma("sync", out_f[rows, 1024 + (cb - 7) * 512:1024 + (cb - 6) * 512], st[:], reads=[st])
            else:
                P.op("scalar", lambda e, mp=mp: e.activation(out=asb[:], in_=mp[:, :32], func=AF.Copy),
                     reads=[mp], writes=[asb])
                for d in range(2):
                    P.op("tensor", lambda e, d=d: e.transpose(out=tpa[:], in_=asb[:, d * 16:(d + 1) * 16], identity=identf[:]),
                         reads=[asb, identf], writes=[tpa])
                    P.op("vector", lambda e: e.tensor_copy(out=aT[:], in_=tpa[:]), reads=[tpa], writes=[aT])
                    P.op("tensor", lambda e, d=d: e.matmul(zp[:], lhsT=aT[:], rhs=wa2s[:, d, :], start=True, stop=False),
                         reads=[aT, wa2s], writes=[zp])
                    P.op("tensor", lambda e, d=d: e.matmul(zp[:], lhsT=ones1[:], rhs=bas[:, d, :], start=False, stop=True),
                         reads=[ones1, bas], writes=[zp])
                    st = stage("f")
                    P.op("scalar", lambda e: e.activation(out=ez[:], in_=zp[:], func=AF.Exp, scale=-1.0),
                         reads=[zp], writes=[ez])
                    P.op("scalar", lambda e: e.activation(out=ez[:], in_=ez[:], func=AF.Ln, bias=1.0),
                         reads=[ez], writes=[ez])
                    P.op("vector", lambda e, st=st: e.tensor_scalar(out=st[:], in0=ez[:], scalar1=-1.0 / 16, scalar2=None, op0=ALU.mult),
                         reads=[ez], writes=[st])
                    P.dma("sync", out_f[rows, 2048 + d * 512:2048 + (d + 1) * 512], st[:], reads=[st])


def fm(v):
    return np.ascontiguousarray(np.asarray(v).reshape(16, 128).T)


def rope_tables():
    rows = SEQ // 64
    row = np.repeat(np.arange(rows, dtype=np.float32), 64)
    col = np.tile(np.arange(64, dtype=np.float32), rows)
    inv = (np.float32(10000.0) ** (-np.arange(0, 64, 2, dtype=np.float32) / np.float32(64))).astype(np.float32)
    ang = np.concatenate([row[:, None] * inv, col[:, None] * inv], axis=-1).astype(np.float32)
    return np.cos(ang).astype(np.float32), np.sin(ang).astype(np.float32)


def core_tokens(a_lat, a_ctx, i):
    return np.concatenate([a_lat[i * 1024:(i + 1) * 1024], a_ctx[(i % 2) * 128:(i % 2) * 128 + 128]], axis=0)


def ka_static_inputs(inputs, mods, l):
    m_lat, m_ctx = mods[l, 0], mods[l, 1]
    vec = np.stack([fm(inputs["norm_mix"][l]), fm(m_lat[D:2 * D]), fm(m_lat[0:D]),
                    fm(m_ctx[D:2 * D]), fm(m_ctx[0:D])], axis=1).astype(np.float32)
    gains = np.ascontiguousarray(np.broadcast_to(
        np.stack([inputs["q_gain"][l], inputs["k_gain"][l]])[None], (128, 2, 128))).astype(np.float32)
    cos, sin = rope_tables()
    ropes = []
    for i in range(NCORES):
        c = core_tokens(cos, np.ones((CTX, 64), np.float32), i)
        s = core_tokens(sin, np.zeros((CTX, 64), np.float32), i)
        r = np.stack([c, s], axis=1).reshape(NT, 128, 2, 64).transpose(1, 0, 2, 3)
        ropes.append(np.ascontiguousarray(r))
    wa2 = np.ascontiguousarray(inputs["w_gla_a2"][l].transpose(1, 0, 2))
    ba = np.ascontiguousarray(inputs["b_gla_a"][l][None])
    return vec, gains, ropes, wa2, ba


def combine_maps(mods, l_prev, kd_outs, aff_lat, aff_ctx):
    oes = {}
    for i, (oe, pl, pc) in enumerate(kd_outs):
        oes["oe%d" % (2 * i)] = np.ascontiguousarray(oe[0])
        oes["oe%d" % (2 * i + 1)] = np.ascontiguousarray(oe[1])
    pos_lat = np.concatenate([o[1].transpose(2, 0, 1).reshape(SEQ, 2) for o in kd_outs], axis=1)
    pos_ctx = np.concatenate([o[2].transpose(2, 0, 1).reshape(CTX, 2) for o in kd_outs], axis=1)
    g2v = np.stack([mods[l_prev, 0][5 * D:6 * D], mods[l_prev, 1][5 * D:6 * D]]).astype(np.float32)
    maps = []
    for i in range(NCORES):
        cp = core_tokens(pos_lat, pos_ctx, i).reshape(NT, 128, NE).transpose(1, 0, 2)
        m = dict(oes)
        m["cpos"] = np.ascontiguousarray(cp).astype(np.int32)
        m["caff"] = np.ascontiguousarray(core_tokens(aff_lat, aff_ctx, i).reshape(NT, 128, NE).transpose(1, 0, 2))
        m["g2v"] = g2v
        maps.append(m)
    return maps


def run_ka(inputs, mods, l, x_lat, x_ctx, nc=None, comb_maps=None):
    nc = nc or build_ka(combine=comb_maps is not None)
    vec, gains, ropes, wa2, ba = ka_static_inputs(inputs, mods, l)
    w_in = np.ascontiguousarray(inputs["w_in"][l])
    maps = []
    for i in range(NCORES):
        maps.append({"x_in": core_tokens(x_lat, x_ctx, i), "w_in": w_in, "vecs": vec, "gains": gains,
                     "rope": ropes[i], "wa2": wa2, "ba": ba})
        if comb_maps is not None:
            maps[-1].update(comb_maps[i])
    res = run_bass_kernel_spmd(nc, maps, core_ids=list(range(NCORES)))
    if comb_maps is not None:
        return [(r["out_bf"], r["out_f"], r["x_new"]) for r in res.results]
    return [(r["out_bf"], r["out_f"]) for r in res.results]


def gather_tokens(per_core):
    lat = np.concatenate([a[:1024] for a in per_core], axis=0)
    ctx = np.concatenate([per_core[0][1024:], per_core[1][1024:]], axis=0)
    return lat, ctx


NTOK = CTX + SEQ
NTT = NTOK // 128
GG = 6
GLA_EVERY = (1, 1)


def build_kb(do_gla=True, do_attn=True, nblk=None, ngrp=None):
    nc = bass.Bass("TRN2", target_bir_lowering=False)
    qT = nc.dram_tensor("qT", [128, NTOK], BF16, kind="ExternalInput").ap()
    kT = nc.dram_tensor("kT", [128, NTOK], BF16, kind="ExternalInput").ap()
    v = nc.dram_tensor("v", [NTOK, 128], BF16, kind="ExternalInput").ap()
    gqT = nc.dram_tensor("gqT", [128, NTOK], F32, kind="ExternalInput").ap()
    gkT = nc.dram_tensor("gkT", [128, NTOK], F32, kind="ExternalInput").ap()
    gk = nc.dram_tensor("gk", [NTOK, 128], F32, kind="ExternalInput").ap()
    gv = nc.dram_tensor("gv", [NTOK, 256], BF16, kind="ExternalInput").ap()
    la = nc.dram_tensor("la", [NTOK, 128], F32, kind="ExternalInput").ap()
    oT = nc.dram_tensor("oT", [128, NTOK], BF16, kind="ExternalOutput").ap()
    og = nc.dram_tensor("og", [NTOK, 256], F32, kind="ExternalOutput").ap()
    P = Prog(nc)
    if do_gla and do_attn:
        emit_attn(P, qT, kT, v, oT, nblk, side=gla_gen(P, gqT, gkT, gk, gv, la, og, ngrp))
    elif do_gla:
        emit_gla(P, gqT, gkT, gk, gv, la, og, ngrp)
    elif do_attn:
        emit_attn(P, qT, kT, v, oT, nblk)
    P.finish()
    return nc


class PV:
    def __init__(self, t, c0, n):
        self.t, self.c0, self.n, self.name = t, c0, n, t.name

    def __getitem__(self, key):
        if not isinstance(key, tuple):
            key = (key, slice(None))
        rows, cols = key
        start = cols.start or 0
        stop = self.n if cols.stop is None else cols.stop
        return self.t[rows, self.c0 + start:self.c0 + stop]


def make_tri(P, name, upper):
    m = P.sb(name, [128, 128], F32)
    P.op("gpsimd", lambda e: e.memset(m[:], 0.0), writes=[m])
    for b in range(2):
        blk = m[b * 64:(b + 1) * 64, b * 64:(b + 1) * 64]
        P.op("gpsimd", lambda e, blk=blk: e.memset(blk, 1.0), reads=[m], writes=[m])
        if upper:
            P.op("gpsimd", lambda e, blk=blk: e.affine_select(
                out=blk, in_=blk, pattern=[[1, 64]], compare_op=ALU.is_ge, fill=0.0, base=0,
                channel_multiplier=-1), reads=[m], writes=[m])
        else:
            P.op("gpsimd", lambda e, blk=blk: e.affine_select(
                out=blk, in_=blk, pattern=[[-1, 64]], compare_op=ALU.is_gt, fill=0.0, base=0,
                channel_multiplier=1), reads=[m], writes=[m])
    return m


def gla_gen(P, gqT, gkT, gk, gv, la, og, ngrp_lim=None):
    U2 = make_tri(P, "U2", True)
    L2 = make_tri(P, "L2", False)
    ngrp = ngrp_lim or NTT // GG
    gqT_s = [P.sb("gqT_s%d" % i, [128, GG * 128], F32) for i in range(2)]
    gkT_s = [P.sb("gkT_s%d" % i, [128, GG * 128], F32) for i in range(2)]
    gk_s = [P.sb("gk_s%d" % i, [128, GG, 128], F32) for i in range(2)]
    gv_s = [P.sb("gv_s%d" % i, [128, GG, 256], BF16) for i in range(2)]
    la_s = [P.sb("la_s%d" % i, [128, GG, 128], F32) for i in range(2)]
    bankA = P.ps("gla_bankA", [128, 512], F32)
    bankB = P.ps("gla_bankB", [128, 512], F32)
    bT_ps, bl_ps, at_ps = PV(bankA, 0, 128), PV(bankA, 128, 128), PV(bankA, 256, 128)
    o_ps = [PV(bankB, 0, 256)] * 2
    ds_ps = [P.ps("ds_ps%d" % i, [128, 256], F32) for i in range(2)]
    eb = [P.sb("eb%d" % i, [128, 128], F32) for i in range(2)]
    enb = P.sb("enb", [128, 128], F32)
    ekh = P.sb("ekh", [128, 128], F32)
    qtT = [P.sb("qtT%d" % i, [128, 128], BF16) for i in range(2)]
    ktT = P.sb("ktT", [128, 128], BF16)
    kh = [P.sb("kh%d" % i, [128, 128], BF16) for i in range(2)]
    atm = P.sb("atm", [128, 128], BF16)
    S = [P.sb("S%d" % i, [128, 256], F32) for i in range(2)]
    Sb = [P.sb("Sb%d" % i, [128, 256], BF16) for i in range(4)]
    ost = [P.sb("ost%d" % i, [128, 256], F32) for i in range(2)]
    P.op("gpsimd", lambda e: e.memset(S[0][:], 0.0), writes=[S[0]])
    P.op("gpsimd", lambda e: e.memset(Sb[0][:], 0.0), writes=[Sb[0]])
    sc = 0
    def load_group(g):
        b = g % 2
        r0 = g * GG * 128
        P.dma("sync", gqT_s[b][:], gqT[:, r0:r0 + GG * 128], writes=[gqT_s[b]])
        P.dma("sync", gkT_s[b][:], gkT[:, r0:r0 + GG * 128], writes=[gkT_s[b]])
        P.dma("sync", gk_s[b][:], gk[r0:r0 + GG * 128, :].rearrange("(t p) d -> p t d", p=128), writes=[gk_s[b]])
        P.dma("sync", gv_s[b][:], gv[r0:r0 + GG * 128, :].rearrange("(t p) d -> p t d", p=128), writes=[gv_s[b]])
        P.dma("sync", la_s[b][:], la[r0:r0 + GG * 128, :].rearrange("(t p) d -> p t d", p=128), writes=[la_s[b]])

    load_group(0)
    for g in range(ngrp):
        b = g % 2
        if g + 1 < ngrp:
            load_group(g + 1)
        gq_b, gkT_b, gk_b, gv_b = gqT_s[b], gkT_s[b], gk_s[b], gv_s[b]
        for tt in range(GG):
            ti = g * GG + tt
            p2 = ti % 2
            la_t = la_s[b][:, tt, :]
            cols = slice(tt * 128, (tt + 1) * 128)
            P.op("tensor", lambda e, la_t=la_t: e.matmul(bT_ps[:], lhsT=la_t, rhs=U2[:], start=True, stop=True),
                 reads=[la_s[b], U2], writes=[bT_ps])
            P.op("tensor", lambda e, la_t=la_t: e.matmul(bl_ps[:], lhsT=L2[:], rhs=la_t, start=True, stop=True),
                 reads=[la_s[b], L2], writes=[bl_ps])
            yield
            ebt = eb[p2]
            P.op("scalar", lambda e, ebt=ebt: e.activation(out=ebt[:], in_=bT_ps[:], func=AF.Exp), reads=[bT_ps], writes=[ebt])
            P.op("scalar", lambda e: e.activation(out=enb[:], in_=bT_ps[:], func=AF.Exp, scale=-1.0), reads=[bT_ps], writes=[enb])
            P.op("scalar", lambda e: e.activation(out=ekh[:], in_=bl_ps[:], func=AF.Exp), reads=[bl_ps], writes=[ekh])
            yield
            q_t = qtT[p2]
            kh_t = kh[p2]
            P.op("vector", lambda e, q_t=q_t, ebt=ebt, cols=cols, gq_b=gq_b: e.tensor_tensor(out=q_t[:], in0=gq_b[:, cols], in1=ebt[:], op=ALU.mult),
                 reads=[gqT_s[b], ebt], writes=[q_t])
            P.op("vector", lambda e, cols=cols, gkT_b=gkT_b: e.tensor_tensor(out=ktT[:], in0=gkT_b[:, cols], in1=enb[:], op=ALU.mult),
                 reads=[gkT_s[b], enb], writes=[ktT])
            P.op("gpsimd", lambda e, kh_t=kh_t, tt=tt, gk_b=gk_b: e.tensor_tensor(out=kh_t[:], in0=gk_b[:, tt, :], in1=ekh[:], op=ALU.mult),
                 reads=[gk_s[b], ekh], writes=[kh_t])
            yield
            P.op("tensor", lambda e, q_t=q_t: e.matmul(at_ps[:], lhsT=ktT[:], rhs=q_t[:], start=True, stop=True),
                 reads=[ktT, q_t], writes=[at_ps])
            yield
            P.op("vector", lambda e: e.tensor_tensor(out=atm[:], in0=at_ps[:], in1=U2[:], op=ALU.mult),
                 reads=[at_ps, U2], writes=[atm])
            yield
            ops_ = o_ps[p2]
            gv_t = gv_s[b][:, tt, :]
            P.op("tensor", lambda e, ops_=ops_, gv_t=gv_t: e.matmul(ops_[:], lhsT=atm[:], rhs=gv_t, start=True, stop=False),
                 reads=[atm, gv_s[b]], writes=[ops_])
            for c in range(2):
                rs = slice(c * 64, (c + 1) * 64)
                Sb_c = Sb[sc % 4]
                P.op("tensor", lambda e, ops_=ops_, q_t=q_t, rs=rs, Sb_c=Sb_c, c=c: e.matmul(
                    ops_[rs, :], lhsT=q_t[:, rs], rhs=Sb_c[:], start=False, stop=True),
                    reads=[q_t, Sb_c], writes=[ops_])
                dsp = ds_ps[sc % 2]
                P.op("tensor", lambda e, dsp=dsp, kh_t=kh_t, rs=rs, tt=tt, gv_b=gv_b: e.matmul(
                    dsp[:], lhsT=kh_t[rs, :], rhs=gv_b[rs, tt, :], start=True, stop=True),
                    reads=[kh_t, gv_s[b]], writes=[dsp])
                yield
                S_cur, S_nxt = S[sc % 2], S[(sc + 1) % 2]
                col = c * 64 + 63
                P.op("vector", lambda e, S_cur=S_cur, S_nxt=S_nxt, ebt=ebt, col=col, dsp=dsp: e.scalar_tensor_tensor(
                    out=S_nxt[:], in0=S_cur[:], scalar=ebt[:, col:col + 1], in1=dsp[:], op0=ALU.mult, op1=ALU.add),
                    reads=[S_cur, ebt, dsp], writes=[S_nxt])
                Sb_n = Sb[(sc + 1) % 4]
                P.op("gpsimd", lambda e, Sb_n=Sb_n, S_nxt=S_nxt: e.tensor_copy(out=Sb_n[:], in_=S_nxt[:]),
                     reads=[S_nxt], writes=[Sb_n])
                sc += 1
                yield
            o_t = ost[p2]
            P.op("scalar", lambda e, o_t=o_t, ops_=ops_: e.activation(out=o_t[:], in_=ops_[:], func=AF.Copy),
                 reads=[ops_], writes=[o_t])
            P.dma("sync", og[ti * 128:(ti + 1) * 128, :], o_t[:], reads=[o_t])
            yield


def emit_gla(P, gqT, gkT, gk, gv, la, og, ngrp_lim=None):
    for _ in gla_gen(P, gqT, gkT, gk, gv, la, og, ngrp_lim):
        pass


def emit_attn(P, qT, kT, v, oT, nblk=None, side=None):
    q_s = P.sb("q_s", [128, NTOK], BF16)
    k_s = P.sb("k_s", [128, NTOK], BF16)
    v_s = P.sb("v_s", [128, NTT, 128], BF16)
    for j in range(4):
        cs = slice(j * (NTOK // 4), (j + 1) * (NTOK // 4))
        P.dma("sync", q_s[:, cs], qT[:, cs], writes=[(q_s, j)])
        P.dma("scalar", k_s[:, cs], kT[:, cs], writes=[(k_s, j)])
    vv = v.rearrange("(t p) d -> p t d", p=128)
    for j in range(3):
        ts_ = slice(j * 22, (j + 1) * 22)
        P.dma("sync", v_s[:, ts_, :], vv[:, ts_, :], writes=[(v_s, j)])
    ones = P.sb("ones_f32", [128, 128], F32)
    P.op("gpsimd", lambda e: e.memset(ones[:], 1.0), writes=[ones])
    pacc = [P.sb("pacc%d" % i, [128, 512], F32) for i in range(2)]
    st_ps = [P.ps("st_ps%d" % i, [128, 512], F32) for i in range(2)]
    oa_ps = [P.ps("oa_ps", [128, 512], F32)] * 2
    dn_ps = [P.ps("dn_ps", [128, 512], F32)] * 2
    pT = [P.sb("pT%d" % i, [128, 512], BF16) for i in range(3)]
    rden = P.sb("rden", [128, 512], F32)
    ob = [P.sb("ob%d" % i, [128, 512], BF16) for i in range(2)]
    blocks = [(0, CTX, 2)] + [(CTX + i * 512, 512, NTT) for i in range(SEQ // 512)]
    qkeys = [(q_s, j) for j in range(4)]
    if nblk:
        blocks = blocks[:nblk]
    iters = [(bi, q0, n, ns, s_) for bi, (q0, n, ns) in enumerate(blocks) for s_ in range(ns)]

    def emit_s(idx):
        bi, q0, n, ns, s_ = iters[idx]
        sp = st_ps[idx % 2]
        kk = (k_s, (s_ * 128) // (NTOK // 4))
        kk2 = (k_s, (s_ * 128 + 127) // (NTOK // 4))
        P.op("tensor", lambda e: e.matmul(sp[:, :n], lhsT=k_s[:, s_ * 128:(s_ + 1) * 128], rhs=q_s[:, q0:q0 + n],
                                          start=True, stop=True), reads=[kk, kk2] + qkeys, writes=[sp])

    emit_s(0)
    for idx, (bi, q0, n, ns, s_) in enumerate(iters):
        if idx + 1 < len(iters):
            emit_s(idx + 1)
        oa, dn = oa_ps[0], dn_ps[0]
        sp, pt = st_ps[idx % 2], pT[idx % 3]
        P.op("scalar", lambda e, sp=sp, pt=pt, n=n: e.activation(
            out=pt[:, :n], in_=sp[:, :n], func=AF.Exp, scale=128 ** -0.5), reads=[sp], writes=[pt])
        P.op("tensor", lambda e, pt=pt, s_=s_, n=n, ns=ns: e.matmul(
            oa[:, :n], lhsT=v_s[:, s_, :], rhs=pt[:, :n], start=(s_ == 0), stop=(s_ == ns - 1)),
            reads=[(v_s, s_ // 22), pt], writes=[oa])
        pa = pacc[bi % 2]
        if s_ == 0:
            P.op("vector", lambda e, pa=pa, pt=pt, n=n: e.tensor_copy(out=pa[:, :n], in_=pt[:, :n]), reads=[pt], writes=[pa])
        else:
            P.op("vector", lambda e, pa=pa, pt=pt, n=n: e.tensor_tensor(out=pa[:, :n], in0=pa[:, :n], in1=pt[:, :n], op=ALU.add),
                 reads=[pt, pa], writes=[pa])
        if s_ == ns - 1:
            P.op("tensor", lambda e, pa=pa, n=n: e.matmul(dn[:, :n], lhsT=ones[:], rhs=pa[:, :n], start=True, stop=True),
                 reads=[ones, pa], writes=[dn])
        if s_ == ns - 1:
            o_b = ob[bi % 2]
            P.op("vector", lambda e, n=n: e.reciprocal(out=rden[:, :n], in_=dn[:, :n]), reads=[dn], writes=[rden])
            P.op("vector", lambda e, o_b=o_b, n=n: e.tensor_tensor(out=o_b[:, :n], in0=oa[:, :n], in1=rden[:, :n], op=ALU.mult),
                 reads=[oa, rden], writes=[o_b])
            P.dma("sync", oT[:, q0:q0 + n], o_b[:, :n], reads=[o_b])
        if side is not None:
            for _ in range(max(1, GLA_EVERY[0] // GLA_EVERY[1]) if idx % GLA_EVERY[1] < GLA_EVERY[0] else 0):
                next(side, None)
    if side is not None:
        for _ in side:
            pass


def run_kb(bf_lat, bf_ctx, f_lat, f_ctx, nc=None):
    nc = nc or build_kb()
    bf = np.concatenate([bf_ctx, bf_lat], axis=0)
    f = np.concatenate([f_ctx, f_lat], axis=0)
    rev = np.concatenate([np.arange(CTX)[::-1], CTX + np.arange(SEQ)[::-1]])
    maps = []
    for i in range(NCORES):
        kv, h, d = i // 4, i % 4, i // 4
        order = rev if d == 1 else np.arange(NTOK)
        la = f[:, 2048 + d * 512 + h * 128:2048 + d * 512 + (h + 1) * 128][order]
        gq = f[:, h * 128:(h + 1) * 128][order]
        gk = f[:, 512 + h * 128:512 + (h + 1) * 128][order]
        gv = bf[:, 1536 + h * 256:1536 + (h + 1) * 256][order]
        maps.append({
            "qT": np.ascontiguousarray(bf[:, i * 128:(i + 1) * 128].T),
            "kT": np.ascontiguousarray(bf[:, 1024 + kv * 128:1024 + (kv + 1) * 128].T),
            "v": np.ascontiguousarray(bf[:, 1280 + kv * 128:1280 + (kv + 1) * 128]),
            "gqT": np.ascontiguousarray(gq.T), "gkT": np.ascontiguousarray(gk.T),
            "gk": np.ascontiguousarray(gk), "gv": np.ascontiguousarray(gv), "la": np.ascontiguousarray(la),
        })
    res = run_bass_kernel_spmd(nc, maps, core_ids=list(range(NCORES)))
    inv = np.argsort(rev)
    attnT = [r["oT"] for r in res.results]
    og = [r["og"] if i < 4 else r["og"][inv] for i, r in enumerate(res.results)]
    return attnT, og


def build_kc():
    nc = bass.Bass("TRN2", target_bir_lowering=False)
    R = NT * 128
    attnT = nc.dram_tensor("attnT", [128, 8, R], BF16, kind="ExternalInput").ap()
    ogf = nc.dram_tensor("ogf", [R, 1024], F32, kind="ExternalInput").ap()
    ogb = nc.dram_tensor("ogb", [R, 1024], F32, kind="ExternalInput").ap()
    sr = nc.dram_tensor("sr", [R, 1024], F32, kind="ExternalInput").ap()
    x_in = nc.dram_tensor("x_in", [R, D], F32, kind="ExternalInput").ap()
    w_out = nc.dram_tensor("w_out", [D, D], F32, kind="ExternalInput").ap()
    w_r = nc.dram_tensor("w_r", [D, NE], F32, kind="ExternalInput").ap()
    vecs = nc.dram_tensor("vecs", [7, D], F32, kind="ExternalInput").ap()
    ggain = nc.dram_tensor("ggain", [1, 256], F32, kind="ExternalInput").ap()
    x_out = nc.dram_tensor("x_out", [R, D], F32, kind="ExternalOutput").ap()
    h2_out = nc.dram_tensor("h2_out", [R, D], BF16, kind="ExternalOutput").ap()
    aff_out = nc.dram_tensor("aff_out", [R, NE], F32, kind="ExternalOutput").ap()
    P = Prog(nc)
    identf = make_ident(P, "identf", F32)
    identb = P.sb("identb", [128, 128], BF16)
    P.op("vector", lambda e: e.tensor_copy(out=identb[:], in_=identf[:]), reads=[identf], writes=[identb])
    wo = P.sb("wo", [128, 16, D], BF16)
    wv = w_out.rearrange("(kc p) n -> p kc n", p=128)
    for q in range(16):
        P.dma("gpsimd", wo[:, q, :].rearrange("p (a n) -> p a n", a=4), wv[:, q, :].rearrange("p (a n) -> p a n", a=4),
              writes=[(wo, q)])
    wr = P.sb("wr", [128, 16, NE], F32)
    P.dma("sync", wr[:], w_r.rearrange("(kc p) n -> p kc n", p=128), writes=[wr])
    gg = P.sb("gg", [128, 256], F32)
    P.dma("sync", gg[:], ggain.partition_broadcast(128), writes=[gg])
    nf = P.sb("nf", [128, D], F32)
    P.dma("sync", nf[:], vecs[0:1, :].partition_broadcast(128), writes=[nf])
    g1 = P.sb("g1", [128, D], F32)
    A2 = P.sb("A2", [128, D], F32)
    B2 = P.sb("B2", [128, D], F32)

    def load_vecs(j):
        P.dma("sync", g1[:], vecs[1 + 3 * j:2 + 3 * j, :].partition_broadcast(128), writes=[g1])
        P.dma("sync", A2[:], vecs[2 + 3 * j:3 + 3 * j, :].partition_broadcast(128), writes=[A2])
        P.dma("sync", B2[:], vecs[3 + 3 * j:4 + 3 * j, :].partition_broadcast(128), writes=[B2])
        P.op("vector", lambda e: e.scalar_tensor_tensor(out=A2[:], in0=A2[:], scalar=1.0, in1=nf[:], op0=ALU.add, op1=ALU.mult),
             reads=[A2, nf], writes=[A2])

    xts = [P.sb("xt%d" % i, [128, D], F32) for i in range(2)]
    of_ts = [P.sb("of_t%d" % i, [128, 1024], F32) for i in range(2)]
    ob_ts = [P.sb("ob_t%d" % i, [128, 1024], F32) for i in range(2)]
    sr_ts = [P.sb("sr_t%d" % i, [128, 1024], F32) for i in range(2)]
    sq = P.sb("sq", [128, 1024], F32)
    gsss = [P.sb("gss%d" % i, [128, 4], F32) for i in range(2)]
    glb = P.sb("glb", [128, 1024], BF16)
    catT = [P.sb("catT%d" % i, [128, 16, 128], BF16) for i in range(2)]
    tpb = P.ps("tpb", [128, 8, 128], BF16)
    yps = [P.ps("yps%d" % i, [128, 512], F32) for i in range(2)]
    tpf = [P.ps("tpf%d" % i, [128, 4, 128], F32) for i in range(2)]
    lg_ps = P.ps("lg_ps", [128, NE], F32)
    ss2 = P.sb("ss2", [128, 1], F32)
    junk = P.sb("junk", [128, D], BF16)
    h2 = P.sb("h2", [128, D], F32)
    h2b = [P.sb("h2b%d" % i, [128, D], BF16) for i in range(2)]
    h2T = P.sb("h2T", [128, 16, 128], F32)
    mx = P.sb("mx", [128, 1], F32)
    ssum = P.sb("ssum", [128, 1], F32)
    ex = P.sb("ex", [128, NE], F32)
    affs = [P.sb("affs%d" % i, [128, NE], F32) for i in range(2)]
    tmp = P.sb("tmp", [128, 512], F32)
    itc = [0]

    def part1(t):
        rows = slice(t * 128, (t + 1) * 128)
        xt = xts[t % 2]
        ct = catT[t % 2]
        of_t, ob_t, sr_t, gss = of_ts[t % 2], ob_ts[t % 2], sr_ts[t % 2], gsss[t % 2]
        P.dma("sync", xt[:], x_in[rows, :], writes=[xt])
        P.dma("sync", ct[:, 0:8, :], attnT[:, :, rows], writes=[(ct, 0)])
        P.dma("sync", of_t[:], ogf[rows, :], writes=[of_t])
        P.dma("sync", ob_t[:], ogb[rows, :], writes=[ob_t])
        P.dma("sync", sr_t[:], sr[rows, :], writes=[sr_t])
        P.op("vector", lambda e: e.tensor_tensor(out=of_t[:], in0=of_t[:], in1=ob_t[:], op=ALU.add),
             reads=[of_t, ob_t], writes=[of_t])
        P.op("gpsimd", lambda e: e.tensor_tensor(out=sq[:], in0=of_t[:], in1=of_t[:], op=ALU.mult),
             reads=[of_t], writes=[sq])
        P.op("vector", lambda e: e.tensor_reduce(out=gss[:], in_=sq[:].rearrange("p (h d) -> p h d", h=4), axis=AX.X, op=ALU.add),
             reads=[sq], writes=[gss])
        emit_rsqrt(P, gss[:], gss[:], 1.0 / 256, gss, gss)
        o3 = of_t[:].rearrange("p (h d) -> p h d", h=4)
        P.op("gpsimd", lambda e: e.tensor_tensor(out=sr_t[:].rearrange("p (h d) -> p h d", h=4), in0=sr_t[:].rearrange("p (h d) -> p h d", h=4),
                                                 in1=gg[:].unsqueeze(1).broadcast_to([128, 4, 256]), op=ALU.mult),
             reads=[sr_t, gg], writes=[sr_t])
        P.op("vector", lambda e, o3=o3: e.tensor_tensor(out=o3, in0=o3, in1=gss[:].unsqueeze(2).broadcast_to([128, 4, 256]), op=ALU.mult),
             reads=[of_t, gss], writes=[of_t])
        P.op("vector", lambda e: e.tensor_tensor(out=glb[:], in0=of_t[:], in1=sr_t[:], op=ALU.mult),
             reads=[of_t, sr_t], writes=[glb])
        for j in range(8):
            P.op("tensor", lambda e, j=j: e.transpose(out=tpb[:, j, :], in_=glb[:, j * 128:(j + 1) * 128], identity=identb[:]),
                 reads=[glb, identb], writes=[tpb])
        P.op("scalar", lambda e, ct=ct: e.activation(out=ct[:, 8:16, :], in_=tpb[:], func=AF.Copy), reads=[tpb], writes=[(ct, 1)])

    def part2(t):
        rows = slice(t * 128, (t + 1) * 128)
        xt = xts[t % 2]
        ct = catT[t % 2]
        if t == 0:
            load_vecs(0)
        if t == NT - 1:
            load_vecs(1)
        for cb in range(4):
            yp = yps[itc[0] % 2]
            itc[0] += 1
            cs = slice(cb * 512, (cb + 1) * 512)
            for kc in range(16):
                P.op("tensor", lambda e, yp=yp, ct=ct, kc=kc, cs=cs: e.matmul(
                    yp[:], lhsT=ct[:, kc, :], rhs=wo[:, kc, cs], start=(kc == 0), stop=(kc == 15)),
                    reads=[(ct, kc // 8), (wo, kc)], writes=[yp])
            P.op("vector", lambda e, yp=yp, cs=cs: e.tensor_tensor(out=tmp[:], in0=yp[:], in1=g1[:, cs], op=ALU.mult),
                 reads=[yp, g1], writes=[tmp])
            P.op("gpsimd", lambda e, xt=xt, cs=cs: e.tensor_tensor(out=xt[:, cs], in0=xt[:, cs], in1=tmp[:], op=ALU.add),
                 reads=[tmp, xt], writes=[xt])
        P.dma("sync", x_out[rows, :], xt[:], reads=[xt])
        P.op("scalar", lambda e, xt=xt: e.activation(out=junk[:], in_=xt[:], func=AF.Square, accum_out=ss2[:]),
             reads=[xt], writes=[junk, ss2])
        emit_rsqrt(P, ss2[:], ss2[:], 1.0 / D, ss2, ss2)
        P.op("vector", lambda e, xt=xt: e.scalar_tensor_tensor(out=h2[:], in0=xt[:], scalar=ss2[:, 0:1], in1=A2[:], op0=ALU.mult, op1=ALU.mult),
             reads=[xt, ss2, A2], writes=[h2])
        P.op("gpsimd", lambda e: e.tensor_tensor(out=h2[:], in0=h2[:], in1=B2[:], op=ALU.add), reads=[h2, B2], writes=[h2])
        hb = h2b[t % 2]
        P.op("scalar", lambda e, hb=hb: e.activation(out=hb[:], in_=h2[:], func=AF.Copy), reads=[h2], writes=[hb])
        P.dma("sync", h2_out[rows, :], hb[:], reads=[hb])
        for q in range(4):
            tp = tpf[q % 2]
            for j in range(4):
                kc = q * 4 + j
                P.op("tensor", lambda e, tp=tp, j=j, kc=kc: e.transpose(out=tp[:, j, :], in_=h2[:, kc * 128:(kc + 1) * 128], identity=identf[:]),
                     reads=[h2, identf], writes=[tp])
            P.op("scalar", lambda e, tp=tp, q=q: e.activation(out=h2T[:, q * 4:(q + 1) * 4, :], in_=tp[:], func=AF.Copy),
                 reads=[tp], writes=[(h2T, q)])
        for kc in range(16):
            P.op("tensor", lambda e, kc=kc: e.matmul(lg_ps[:], lhsT=h2T[:, kc, :], rhs=wr[:, kc, :], start=(kc == 0), stop=(kc == 15)),
                 reads=[(h2T, kc // 4), wr], writes=[lg_ps])
        P.op("vector", lambda e: e.tensor_reduce(out=mx[:], in_=lg_ps[:], axis=AX.X, op=ALU.max), reads=[lg_ps], writes=[mx])
        P.op("vector", lambda e: e.tensor_scalar(out=mx[:], in0=mx[:], scalar1=-1.0, scalar2=None, op0=ALU.mult), reads=[mx], writes=[mx])
        P.op("scalar", lambda e: e.activation(out=ex[:], in_=lg_ps[:], func=AF.Exp, bias=mx[:, 0:1], accum_out=ssum[:]),
             reads=[lg_ps, mx], writes=[ex, ssum])
        P.op("vector", lambda e: e.reciprocal(out=ssum[:], in_=ssum[:]), reads=[ssum], writes=[ssum])
        af = affs[t % 2]
        P.op("vector", lambda e, af=af: e.tensor_scalar(out=af[:], in0=ex[:], scalar1=ssum[:, 0:1], scalar2=None, op0=ALU.mult),
             reads=[ex, ssum], writes=[af])
        P.dma("sync", aff_out[rows, :], af[:], reads=[af])

    part1(0)
    for t in range(NT):
        if t + 1 < NT:
            part1(t + 1)
        part2(t)
    P.finish()
    return nc


def run_kc(inputs, mods, l, attnT, og, sr_lat, sr_ctx, x_lat, x_ctx, nc=None):
    nc = nc or build_kc()
    m_lat, m_ctx = mods[l, 0], mods[l, 1]
    vecs = np.stack([inputs["norm_ffn"][l], m_lat[2 * D:3 * D], m_lat[4 * D:5 * D], m_lat[3 * D:4 * D],
                     m_ctx[2 * D:3 * D], m_ctx[4 * D:5 * D], m_ctx[3 * D:4 * D]]).astype(np.float32)
    ggain = np.ascontiguousarray(inputs["gla_gain"][l][None]).astype(np.float32)
    aT = np.stack(attnT, axis=1)
    ogf = np.concatenate(og[0:4], axis=1)
    ogb = np.concatenate(og[4:8], axis=1)
    w_out = np.ascontiguousarray(inputs["w_out"][l])
    w_r = np.ascontiguousarray(inputs["w_router"][l])
    maps = []
    for i in range(NCORES):
        tok = np.concatenate([CTX + np.arange(i * 1024, (i + 1) * 1024), (i % 2) * 128 + np.arange(128)])
        maps.append({"attnT": np.ascontiguousarray(aT[:, :, tok]), "ogf": ogf[tok], "ogb": ogb[tok],
                     "sr": core_tokens(sr_lat, sr_ctx, i), "x_in": core_tokens(x_lat, x_ctx, i),
                     "w_out": w_out, "w_r": w_r, "vecs": vecs, "ggain": ggain})
    res = run_bass_kernel_spmd(nc, maps, core_ids=list(range(NCORES)))
    return [(r["x_out"], r["h2_out"], r["aff_out"]) for r in res.results]


CAP = 2 * SEQ // NE
CCAP = 2 * CTX // NE
NSLOT = CAP + CCAP
ROWW = D
GW = 16
NBIS = 30
OOB0 = 4096


def build_kd():
    nc = bass.Bass("TRN2", target_bir_lowering=False)
    aff_l = nc.dram_tensor("aff_l", [128, 2, 64], F32, kind="ExternalInput").ap()
    aff_c = nc.dram_tensor("aff_c", [128, 2, 2], F32, kind="ExternalInput").ap()
    h2l = nc.dram_tensor("h2l", [SEQ, D], BF16, kind="ExternalInput").ap()
    h2c = nc.dram_tensor("h2c", [CTX, D], BF16, kind="ExternalInput").ap()
    wg = nc.dram_tensor("wg", [2, D, FF], F32, kind="ExternalInput").ap()
    wu = nc.dram_tensor("wu", [2, D, FF], F32, kind="ExternalInput").ap()
    wd = nc.dram_tensor("wd", [2, FF, D], F32, kind="ExternalInput").ap()
    xg = [nc.dram_tensor("xg%d" % i, [NSLOT, ROWW], BF16, kind="Internal").ap() for i in range(2)]
    out_e = nc.dram_tensor("out_e", [2, NSLOT, D], F32, kind="ExternalOutput").ap()
    pos_l = nc.dram_tensor("pos_l", [128, 2, 64], I32, kind="ExternalOutput").ap()
    pos_c = nc.dram_tensor("pos_c", [128, 2, 2], I32, kind="ExternalOutput").ap()
    P = Prog(nc)
    identf = make_ident(P, "identf", F32)
    identb = P.sb("identb", [128, 128], BF16)
    P.op("vector", lambda e: e.tensor_copy(out=identb[:], in_=identf[:]), reads=[identf], writes=[identb])
    ones = P.sb("ones_f", [128, 128], F32)
    P.op("gpsimd", lambda e: e.memset(ones[:], 1.0), writes=[ones])
    SU = P.sb("SU", [128, 128], F32)
    P.op("gpsimd", lambda e: e.memset(SU[:], 1.0), writes=[SU])
    P.op("gpsimd", lambda e: e.affine_select(out=SU[:], in_=SU[:], pattern=[[1, 128]], compare_op=ALU.is_gt, fill=0.0,
                                             base=0, channel_multiplier=-1), reads=[SU], writes=[SU])
    oobv = P.sb("oobv", [128, 1], F32)
    P.op("gpsimd", lambda e: e.iota(oobv[:], pattern=[[0, 1]], base=OOB0, channel_multiplier=1,
                                    allow_small_or_imprecise_dtypes=True), writes=[oobv])
    zeros = P.sb("zeros", [128, 64], F32)
    P.op("gpsimd", lambda e: e.memset(zeros[:], 0.0), writes=[zeros])
    affl = P.sb("affl", [128, 2, 64], F32)
    affc = P.sb("affc", [128, 2, 2], F32)
    P.dma("sync", affl[:], aff_l, writes=[affl])
    P.dma("sync", affc[:], aff_c, writes=[affc])
    pairs = [(affl, 0, 64, 0, CAP), (affl, 1, 64, 0, CAP), (affc, 0, 2, CAP, CCAP), (affc, 1, 2, CAP, CCAP)]
    kvec = P.sb("kvec", [128, 4], F32)
    P.op("gpsimd", lambda e: e.memset(kvec[:, 0:2], CAP - 0.5), writes=[kvec])
    P.op("gpsimd", lambda e: e.memset(kvec[:, 2:4], CCAP - 0.5), reads=[kvec], writes=[kvec])
    lo = P.sb("lo", [128, 4], F32)
    P.op("gpsimd", lambda e: e.memset(lo[:], 0.0), writes=[lo])
    mid = P.sb("mid", [128, 4], F32)
    pc = P.sb("pc", [128, 4], F32)
    ge = P.sb("ge", [128, 4], F32)
    junk = P.sb("junkb", [128, 64], F32)
    sm_ps = P.ps("sm_ps", [128, 512], F32)
    cnt_ps = PV(sm_ps, 0, 4)
    for k in range(NBIS):
        step = 2.0 ** -(k + 1)
        P.op("vector", lambda e, step=step: e.tensor_scalar(out=mid[:], in0=lo[:], scalar1=step, scalar2=None, op0=ALU.add),
             reads=[lo], writes=[mid])
        for j, (a, ee, n, base, cap) in enumerate(pairs):
            P.op("vector", lambda e, a=a, ee=ee, n=n, j=j: e.tensor_scalar(
                out=junk[:, :n], in0=a[:, ee, :], scalar1=mid[:, j:j + 1], scalar2=0.0, op0=ALU.is_ge, op1=ALU.add,
                accum_out=pc[:, j:j + 1]), reads=[a, mid], writes=[junk, (pc, j)])
        P.op("tensor", lambda e: e.matmul(cnt_ps[:], lhsT=ones[:], rhs=pc[:], start=True, stop=True),
             reads=[ones] + [(pc, j) for j in range(4)], writes=[cnt_ps])
        P.op("vector", lambda e: e.tensor_tensor(out=ge[:], in0=cnt_ps[:], in1=kvec[:], op=ALU.is_ge),
             reads=[cnt_ps, kvec], writes=[ge])
        P.op("vector", lambda e, step=step: e.scalar_tensor_tensor(out=lo[:], in0=ge[:], scalar=step, in1=lo[:],
                                                                    op0=ALU.mult, op1=ALU.add),
             reads=[ge, lo], writes=[lo])
    M = P.sb("Msel", [128, 64], F32)
    Tsb = P.sb("Tsb", [128, 64], F32)
    cum = P.sb("cum", [128, 64], F32)
    pos = P.sb("posf", [128, 64], F32)
    sel = P.sb("sel", [128, 64], F32)
    posl = P.sb("posl", [128, 2, 64], I32)
    posc = P.sb("posc", [128, 2, 2], I32)
    wi_ps, t_ps = PV(sm_ps, 64, 64), PV(sm_ps, 128, 64)
    for j, (a, ee, n, base, cap) in enumerate(pairs):
        dst = (posl if n == 64 else posc)
        P.op("vector", lambda e, a=a, ee=ee, n=n, j=j: e.tensor_scalar(
            out=M[:, :n], in0=a[:, ee, :], scalar1=lo[:, j:j + 1], scalar2=None, op0=ALU.is_ge), reads=[a, lo], writes=[M])
        P.op("tensor", lambda e, n=n: e.matmul(wi_ps[:, :n], lhsT=SU[:], rhs=M[:, :n], start=True, stop=True),
             reads=[SU, M], writes=[sm_ps])
        P.op("tensor", lambda e, n=n: e.matmul(t_ps[:, :n], lhsT=ones[:], rhs=M[:, :n], start=True, stop=True),
             reads=[ones, M], writes=[sm_ps])
        P.op("vector", lambda e, n=n: e.tensor_copy(out=Tsb[:, :n], in_=t_ps[:, :n]), reads=[sm_ps], writes=[Tsb])
        P.op("vector", lambda e, n=n: e.tensor_tensor_scan(out=cum[:, :n], data0=Tsb[:, :n], data1=zeros[:, :n], initial=0.0,
                                                            op0=ALU.add, op1=ALU.add), reads=[Tsb, zeros], writes=[cum])
        P.op("vector", lambda e, n=n: e.tensor_tensor(out=pos[:, :n], in0=wi_ps[:, :n], in1=cum[:, :n], op=ALU.add),
             reads=[sm_ps, cum], writes=[pos])
        P.op("vector", lambda e, n=n, base=base: e.scalar_tensor_tensor(out=pos[:, :n], in0=pos[:, :n], scalar=float(base), in1=Tsb[:, :n],
                                                                         op0=ALU.add, op1=ALU.subtract), reads=[pos, Tsb], writes=[pos])
        P.op("vector", lambda e, n=n, base=base, cap=cap: e.scalar_tensor_tensor(
            out=sel[:, :n], in0=pos[:, :n], scalar=float(base + cap) - 0.5, in1=M[:, :n], op0=ALU.is_lt, op1=ALU.mult),
            reads=[pos, M], writes=[sel])
        P.op("vector", lambda e, n=n: e.tensor_scalar(out=pos[:, :n], in0=pos[:, :n], scalar1=oobv[:, 0:1], scalar2=None, op0=ALU.subtract),
             reads=[pos, oobv], writes=[pos])
        P.op("vector", lambda e, n=n: e.tensor_tensor(out=pos[:, :n], in0=pos[:, :n], in1=sel[:, :n], op=ALU.mult),
             reads=[pos, sel], writes=[pos])
        P.op("vector", lambda e, n=n: e.tensor_scalar(out=pos[:, :n], in0=pos[:, :n], scalar1=oobv[:, 0:1], scalar2=None, op0=ALU.add),
             reads=[pos, oobv], writes=[pos])
        P.op("vector", lambda e, n=n, dst=dst, ee=ee: e.tensor_copy(out=dst[:, ee, :], in_=pos[:, :n]), reads=[pos], writes=[dst])
    P.dma("sync", pos_l, posl[:], reads=[posl])
    P.dma("sync", pos_c, posc[:], reads=[posc])
    rbs = [P.sb("rowbuf%d" % i, [128, ROWW], BF16) for i in range(4)]
    rbn = [0]

    def emit_scatter(n, ee):
        rb = rbs[rbn[0] % 4]
        rbn[0] += 1
        if n < 64:
            src, pp, nn = h2l[n * 128:(n + 1) * 128, :], posl, n
        else:
            src, pp, nn = h2c[(n - 64) * 128:(n - 63) * 128, :], posc, n - 64
        P.dma("sync", rb[:, :D], src, writes=[rb])
        P.dma_fn("gpsimd", lambda e: e.indirect_dma_start(
            out=xg[ee], out_offset=bass.IndirectOffsetOnAxis(ap=pp[:, ee, nn:nn + 1], axis=0),
            in_=rb[:, :], in_offset=None, bounds_check=P.reg(e, NSLOT - 1), oob_is_err=False),
            reads=[rb, pp], writes=[("xg", ee, n)])

    for n in range(66):
        emit_scatter(n, 0)
    pending = list(range(66))
    XgT = P.sb("XgT", [128, 16, NSLOT], BF16)
    hidT = P.sb("hidT", [128, 12, NSLOT], BF16)
    xts = [P.sb("xgt%d" % i, [128, ROWW], BF16) for i in range(2)]
    tpb = P.ps("tpb", [128, 8, 128], BF16)
    g_ps = [P.ps("g_ps%d" % i, [128, 512], F32) for i in range(2)]
    u_ps = [P.ps("u_ps%d" % i, [128, 512], F32) for i in range(2)]
    o_ps = [P.ps("o_ps%d" % i, [128, 512], F32) for i in range(2)]
    wgc = [P.sb("wgc%d" % i, [128, 16, 256], BF16) for i in range(2)]
    wuc = [P.sb("wuc%d" % i, [128, 16, 256], BF16) for i in range(2)]
    wdc = [P.sb("wdc%d" % i, [128, 12, 512], BF16) for i in range(2)]
    sgs = [P.sb("sgs%d" % i, [128, 512], F32) for i in range(2)]
    ost = [P.sb("ost%d" % i, [128, 512], F32) for i in range(2)]
    sblocks = [(0, 512), (512, 512), (1024, 32)]
    ih = io = iw = 0
    for ee in range(2):
        scat = [("xg", ee, n) for n in range(66)]
        for st in range(9):
            rows = 128 if st < 8 else CCAP
            xt = xts[st % 2]
            P.dma("sync", xt[:rows, :], xg[ee][st * 128:st * 128 + rows, :], reads=scat, writes=[xt])
            scat = []
            for half in range(2):
                for j in range(8):
                    kc = half * 8 + j
                    P.op("tensor", lambda e, xt=xt, rows=rows, j=j, kc=kc: e.transpose(
                        out=tpb[:, j, :rows], in_=xt[:rows, kc * 128:(kc + 1) * 128], identity=identb[:rows, :rows]),
                        reads=[xt, identb], writes=[tpb])
                eng = "scalar" if half == 0 else "vector"
                if eng == "scalar":
                    P.op("scalar", lambda e, rows=rows, half=half, st=st: e.activation(
                        out=XgT[:, half * 8:(half + 1) * 8, st * 128:st * 128 + rows], in_=tpb[:, :, :rows], func=AF.Copy),
                        reads=[tpb], writes=[(XgT, st)])
                else:
                    P.op("vector", lambda e, rows=rows, half=half, st=st: e.tensor_copy(
                        out=XgT[:, half * 8:(half + 1) * 8, st * 128:st * 128 + rows], in_=tpb[:, :, :rows]),
                        reads=[tpb], writes=[(XgT, st)])
        xkeys = [(XgT, st) for st in range(9)]
        for hc in range(6):
            wg_c, wu_c = wgc[iw % 2], wuc[iw % 2]
            iw += 1
            hs = slice(hc * 256, (hc + 1) * 256)
            for q in range(4):
                P.dma("gpsimd", wg_c[:, 4 * q:4 * q + 4, :], wg[ee].rearrange("(kc p) f -> p kc f", p=128)[:, 4 * q:4 * q + 4, hs],
                      writes=[(wg_c, q)])
                P.dma("gpsimd", wu_c[:, 4 * q:4 * q + 4, :], wu[ee].rearrange("(kc p) f -> p kc f", p=128)[:, 4 * q:4 * q + 4, hs],
                      writes=[(wu_c, q)])
            if ee == 0:
                for _ in range(11):
                    emit_scatter(pending.pop(0), 1)
            for fl in range(2):
                fc = hc * 2 + fl
                fs = slice(fl * 128, (fl + 1) * 128)
                for (s0, n) in sblocks:
                    gp, up, sg = g_ps[ih % 2], u_ps[ih % 2], sgs[ih % 2]
                    ih += 1
                    for kc in range(16):
                        P.op("tensor", lambda e, gp=gp, wg_c=wg_c, kc=kc, fs=fs, s0=s0, n=n: e.matmul(
                            gp[:, :n], lhsT=wg_c[:, kc, fs], rhs=XgT[:, kc, s0:s0 + n], start=(kc == 0), stop=(kc == 15)),
                            reads=[(wg_c, kc // 4)] + xkeys, writes=[gp])
                    for kc in range(16):
                        P.op("tensor", lambda e, up=up, wu_c=wu_c, kc=kc, fs=fs, s0=s0, n=n: e.matmul(
                            up[:, :n], lhsT=wu_c[:, kc, fs], rhs=XgT[:, kc, s0:s0 + n], start=(kc == 0), stop=(kc == 15)),
                            reads=[(wu_c, kc // 4)] + xkeys, writes=[up])
                    P.op("scalar", lambda e, gp=gp, sg=sg, n=n: e.activation(out=sg[:, :n], in_=gp[:, :n], func=AF.Silu),
                         reads=[gp], writes=[sg])
                    P.op("vector", lambda e, up=up, sg=sg, n=n, fc=fc, s0=s0: e.tensor_tensor(
                        out=hidT[:, fc, s0:s0 + n], in0=up[:, :n], in1=sg[:, :n], op=ALU.mult),
                        reads=[up, sg], writes=[(hidT, fc)])
        hkeys = [(hidT, fc) for fc in range(12)]
        for cb in range(4):
            wd_c = wdc[(ee * 4 + cb) % 2]
            cs = slice(cb * 512, (cb + 1) * 512)
            for q in range(3):
                P.dma("gpsimd", wd_c[:, 4 * q:4 * q + 4, :], wd[ee].rearrange("(fc p) n -> p fc n", p=128)[:, 4 * q:4 * q + 4, cs],
                      writes=[(wd_c, q)])
            for st in range(9):
                rows = 128 if st < 8 else CCAP
                op_, os_ = o_ps[io % 2], ost[io % 2]
                io += 1
                for fc in range(12):
                    P.op("tensor", lambda e, op_=op_, wd_c=wd_c, fc=fc, st=st, rows=rows: e.matmul(
                        op_[:rows, :], lhsT=hidT[:, fc, st * 128:st * 128 + rows], rhs=wd_c[:, fc, :],
                        start=(fc == 0), stop=(fc == 11)), reads=[(wd_c, fc // 4)] + hkeys, writes=[op_])
                P.op("scalar", lambda e, op_=op_, os_=os_, rows=rows, ee=ee, st=st: e.activation(
                    out=os_[:rows, :], in_=op_[:rows, :], func=AF.Copy),
                    reads=[op_], writes=[os_])
                P.dma("sync", out_e[ee, st * 128:st * 128 + rows, cs], os_[:rows, :], reads=[os_])
    P.finish()
    return nc


def run_kd(inputs, l, aff_lat, aff_ctx, h2_lat, h2_ctx, nc=None):
    nc = nc or build_kd()
    maps = []
    for i in range(NCORES):
        es = slice(2 * i, 2 * i + 2)
        maps.append({
            "aff_l": np.ascontiguousarray(aff_lat[:, es].reshape(64, 128, 2).transpose(1, 2, 0)),
            "aff_c": np.ascontiguousarray(aff_ctx[:, es].reshape(2, 128, 2).transpose(1, 2, 0)),
            "h2l": h2_lat, "h2c": h2_ctx,
            "wg": np.ascontiguousarray(inputs["w_gate"][l, es]), "wu": np.ascontiguousarray(inputs["w_up"][l, es]),
            "wd": np.ascontiguousarray(inputs["w_down"][l, es]),
        })
    res = run_bass_kernel_spmd(nc, maps, core_ids=list(range(NCORES)))
    return [(r["out_e"], r["pos_l"], r["pos_c"]) for r in res.results]


def build_kf():
    nc = bass.Bass("TRN2", target_bir_lowering=False)
    x_in = nc.dram_tensor("x_in", [NT * 128, D], F32, kind="ExternalInput").ap()
    fnv = nc.dram_tensor("fnv", [1, D], F32, kind="ExternalInput").ap()
    y = nc.dram_tensor("y", [(NT - 1) * 128, D], F32, kind="ExternalOutput").ap()
    P = Prog(nc)
    comb = Combiner(P, *declare_combine_inputs(nc))
    fn_s = P.sb("fn_s", [128, D], F32)
    P.dma("sync", fn_s[:], fnv.partition_broadcast(128), writes=[fn_s])
    xts = [P.sb("xt%d" % i, [128, D], F32) for i in range(2)]
    junk = P.sb("junk", [128, D], BF16)
    ss = P.sb("ss", [128, 1], F32)
    for t in range(NT - 1):
        xt = xts[t % 2]
        rows = slice(t * 128, (t + 1) * 128)
        P.dma("sync", xt[:], x_in[rows, :], writes=[xt])
        comb.emit(t, xt)
        P.op("scalar", lambda e, xt=xt: e.activation(out=junk[:], in_=xt[:], func=AF.Square, accum_out=ss[:]),
             reads=[xt], writes=[junk, ss])
        emit_rsqrt(P, ss[:], ss[:], 1.0 / D, ss, ss)
        P.op("vector", lambda e, xt=xt: e.scalar_tensor_tensor(out=xt[:], in0=xt[:], scalar=ss[:, 0:1], in1=fn_s[:],
                                                                op0=ALU.mult, op1=ALU.mult), reads=[xt, ss, fn_s], writes=[xt])
        P.dma("sync", y[rows, :], xt[:], reads=[xt])
    P.finish()
    return nc


def run_kf(inputs, x_lat, x_ctx, comb_maps, nc=None):
    nc = nc or build_kf()
    fnv = np.ascontiguousarray(inputs["final_norm"][None]).astype(np.float32)
    maps = []
    for i in range(NCORES):
        m = {"x_in": core_tokens(x_lat, x_ctx, i), "fnv": fnv}
        m.update(comb_maps[i])
        maps.append(m)
    res = run_bass_kernel_spmd(nc, maps, core_ids=list(range(NCORES)))
    return np.concatenate([r["y"] for r in res.results], axis=0)


_NC = {}


def _nc(name, fn):
    if name not in _NC:
        _NC[name] = fn()
    return _NC[name]


def kernel(**inputs):
    inputs = {k: np.asarray(v) for k, v in inputs.items()}
    mods = run_kmod(inputs)
    x_lat = np.ascontiguousarray(inputs["x"][0])
    x_ctx = np.ascontiguousarray(inputs["ctx"][0])
    comb = None
    for l in range(DEPTH):
        if comb is None:
            ka = run_ka(inputs, mods, l, x_lat, x_ctx, nc=_nc("ka0", lambda: build_ka(False)))
        else:
            ka = run_ka(inputs, mods, l, x_lat, x_ctx, nc=_nc("ka1", lambda: build_ka(True)), comb_maps=comb)
            x_lat, x_ctx = gather_tokens([o[2] for o in ka])
        bf_lat, bf_ctx = gather_tokens([o[0] for o in ka])
        f_lat, f_ctx = gather_tokens([o[1] for o in ka])
        attnT, og = run_kb(bf_lat, bf_ctx, f_lat, f_ctx, nc=_nc("kb", build_kb))
        kc = run_kc(inputs, mods, l, attnT, og, f_lat[:, 1024:2048], f_ctx[:, 1024:2048], x_lat, x_ctx,
                    nc=_nc("kc", build_kc))
        x_lat, x_ctx = gather_tokens([o[0] for o in kc])
        h_lat, h_ctx = gather_tokens([o[1] for o in kc])
        a_lat, a_ctx = gather_tokens([o[2] for o in kc])
        kd = run_kd(inputs, l, a_lat, a_ctx, h_lat, h_ctx, nc=_nc("kd", build_kd))
        comb = combine_maps(mods, l, kd, a_lat, a_ctx)
    y = run_kf(inputs, x_lat, x_ctx, comb, nc=_nc("kf", build_kf))
    return np.ascontiguousarray(y[None]).astype(np.float32)
```

```python
import numpy as np
from contextlib import ExitStack
import concourse.bass as bass
import concourse.mybir as mybir
from concourse.bass_utils import run_bass_kernel_spmd

F32 = mybir.dt.float32
BF16 = mybir.dt.bfloat16
I32 = mybir.dt.int32
U32 = mybir.dt.uint32
AF = mybir.ActivationFunctionType
ALU = mybir.AluOpType
AX = mybir.AxisListType

ENGINES = ["sync", "scalar", "vector", "gpsimd", "tensor"]
SEM_EPOCH = 20000
NDSLOT = 6


class Prog:
    def __init__(self, nc, same_engine_sync=True):
        self.nc = nc
        self.stack = ExitStack()
        self.items = {e: [] for e in ENGINES}
        self.cur = {}
        self.sems = {}
        self.known = {e: {} for e in ENGINES}
        self.lastw = {}
        self.readers = {}
        self.same_engine_sync = same_engine_sync
        self.final = {}
        self.nsem = 0
        self.drr = {}

    def const_eps(self):
        if not hasattr(self, "_epsb"):
            self._epsb = self.sb("epsb", [128, 1], F32)
            self.op("gpsimd", lambda e: e.memset(self._epsb[:], EPS), writes=[self._epsb])
        return self._epsb

    def reg(self, e, val):
        if not hasattr(self, "_regs"):
            self._regs = {}
        if val not in self._regs:
            self._regs[val] = e.to_reg(val)
        return self._regs[val]

    def sb(self, name, shape, dt):
        return self.stack.enter_context(self.nc.sbuf_tensor(name, shape, dt))

    def ps(self, name, shape, dt):
        return self.stack.enter_context(self.nc.psum_tensor(name, shape, dt))

    def _sem(self, key):
        if key not in self.sems:
            self.nsem += 1
            self.sems[key] = self.stack.enter_context(
                self.nc.semaphore("s_%s_%s_%d" % key))
        return self.sems[key]

    def _bump(self, kind, eng, amount):
        st = self.cur.setdefault((kind, eng), [0, 0])
        if st[1] + amount > SEM_EPOCH:
            st[0] += 1
            st[1] = 0
        st[1] += amount
        key = (kind, eng, st[0])
        self._sem(key)
        self.final[key] = st[1]
        return key, st[1]

    @staticmethod
    def _k(x):
        if isinstance(x, (str, int)):
            return x
        if isinstance(x, tuple):
            return tuple(Prog._k(y) for y in x)
        return x.name

    def _deps(self, eng, reads, writes):
        reads = [self._k(r) for r in reads]
        writes = [self._k(w) for w in writes]
        need = {}

        def add(dep):
            k, c = dep
            if need.get(k, 0) < c:
                need[k] = c
        for r in reads:
            if r in self.lastw:
                add(self.lastw[r])
        for w in writes:
            if w in self.lastw:
                add(self.lastw[w])
            for d in self.readers.get(w, ()):
                add(d)
        waits = []
        for k, c in need.items():
            kind, e2, _ = k
            if kind == "c" and e2 == eng:
                if eng == "tensor" or not self.same_engine_sync:
                    continue
            if self.known[eng].get(k, 0) >= c:
                continue
            self.known[eng][k] = c
            waits.append((k, c))
        return waits

    def _record(self, reads, writes, dep):
        reads = [self._k(r) for r in reads]
        writes = [self._k(w) for w in writes]
        for r in reads:
            self.readers.setdefault(r, []).append(dep)
        for w in writes:
            self.lastw[w] = dep
            self.readers[w] = []

    def op(self, eng, fn, reads=(), writes=()):
        waits = self._deps(eng, reads, writes)
        key, c = self._bump("c", eng, 1)
        self.items[eng].append((waits, fn, key, 1))
        self._record(reads, writes, (key, c))

    def _dma_slot(self, eng, waits):
        rr = self.drr.get(eng, 0)
        self.drr[eng] = (rr + 1) % NDSLOT
        slot = "%s%d" % (eng, rr)
        st = self.cur.setdefault(("d", slot), [0, 0])
        if st[1] > 0:
            pk = ("d", slot, st[0])
            if self.known[eng].get(pk, 0) < st[1]:
                self.known[eng][pk] = st[1]
                waits.append((pk, st[1]))
        return self._bump("d", slot, 16)

    def dma(self, eng, out, in_, reads=(), writes=(), out_final=False, **kw):
        return self.dma_fn(eng, lambda e: e.dma_start(out=out, in_=in_, **kw), reads, writes, out_final)

    def dma_fn(self, eng, fn, reads=(), writes=(), out_final=False):
        waits = self._deps(eng, reads, writes)
        key, c = self._dma_slot(eng, waits)
        self.items[eng].append((waits, fn, key, 16))
        self._record(reads, writes, (key, c))

    def finish(self):
        nc = self.nc
        fin = dict(self.final)
        items = self.items
        sems = self.sems

        def emit(eng_name, e, extra=None):
            for waits, fn, key, inc in items[eng_name]:
                for k, c in waits:
                    e.wait_ge(sems[k], c)
                ins = fn(e)
                ins.then_inc(sems[key], inc)
            if extra:
                for k, c in extra.items():
                    e.wait_ge(sems[k], c)

        with nc.Block() as block:
            @block.sync
            def _(e):
                emit("sync", e, fin)

            @block.scalar
            def _(e):
                emit("scalar", e)

            @block.vector
            def _(e):
                emit("vector", e)

            @block.gpsimd
            def _(e):
                emit("gpsimd", e)

            @block.tensor
            def _(e):
                emit("tensor", e)
        self.stack.close()


def kernel(**inputs):
    raise NotImplementedError


D = 2048
SEQ = 8192
CTX = 256
DEPTH = 4
NCORES = 8
INW = 4640
EPS = 1e-6
NT = 9
NE = 16
FF = 1536


def make_ident(P, name, dt):
    idf = P.sb(name + "_f", [128, 128], F32)
    P.op("gpsimd", lambda e: e.memset(idf[:], 1.0), writes=[idf])
    P.op("gpsimd", lambda e: e.affine_select(out=idf[:], in_=idf[:], pattern=[[-1, 128]],
                                             compare_op=ALU.is_equal, fill=0.0, base=0,
                                             channel_multiplier=1), reads=[idf], writes=[idf])
    if dt == F32:
        return idf
    idn = P.sb(name, [128, 128], dt)
    P.op("vector", lambda e: e.tensor_copy(out=idn[:], in_=idf[:]), reads=[idf], writes=[idn])
    return idn


def build_kmod():
    nc = bass.Bass("TRN2", target_bir_lowering=False)
    CW = 6 * D // NCORES
    cc = nc.dram_tensor("cc", [128, 16, 2], F32, kind="ExternalInput").ap()
    wm = nc.dram_tensor("wm", [DEPTH, D, CW], F32, kind="ExternalInput").ap()
    bm = nc.dram_tensor("bm", [DEPTH, 1, CW], F32, kind="ExternalInput").ap()
    out = nc.dram_tensor("mods", [DEPTH, 2, CW], F32, kind="ExternalOutput").ap()
    P = Prog(nc)
    cs = P.sb("cs", [128, 16, 2], F32)
    sc = P.sb("scs", [128, 16, 2], F32)
    P.dma("sync", cs[:], cc, writes=[cs])
    P.op("scalar", lambda e: e.activation(out=sc[:], in_=cs[:], func=AF.Silu), reads=[cs], writes=[sc])
    wts = [P.sb("w%d" % i, [128, 16, 512], F32) for i in range(2)]
    pss = [P.ps("ps%d" % i, [2, 512], F32) for i in range(2)]
    bias = P.sb("bias", [2, DEPTH, CW], F32)
    for r in range(2):
        P.dma("sync", bias[r:r + 1, :, :], bm.rearrange("l o n -> o l n"), writes=[(bias, r)])
    res = P.sb("res", [2, DEPTH, CW], F32)
    it = 0
    for l in range(DEPTH):
        for cb in range(CW // 512):
            w = wts[it % 2]
            ps = pss[it % 2]
            P.dma("sync" if it % 2 == 0 else "scalar", w[:],
                  wm[l].rearrange("(kc p) n -> p kc n", p=128)[:, :, cb * 512:(cb + 1) * 512],
                  writes=[w])
            for kc in range(16):
                P.op("tensor", lambda e, w=w, ps=ps, kc=kc: e.matmul(
                    ps[:], lhsT=sc[:, kc, :], rhs=w[:, kc, :], start=(kc == 0), stop=(kc == 15)),
                    reads=[sc, w], writes=[ps])
            P.op("vector", lambda e, ps=ps, l=l, cb=cb: e.tensor_tensor(
                out=res[:, l, cb * 512:(cb + 1) * 512], in0=ps[:], in1=bias[:, l, cb * 512:(cb + 1) * 512],
                op=ALU.add), reads=[ps, (bias, 0), (bias, 1)], writes=[(res, it)])
            it += 1
    P.dma("sync", out.rearrange("l r n -> r l n"), res[:], reads=[(res, i) for i in range(it)],
          out_final=True)
    P.finish()
    return nc


def run_kmod(inputs):
    c2 = np.stack([inputs["c"][0], inputs["c_ctx"]], axis=-1)
    cc = np.ascontiguousarray(c2.reshape(16, 128, 2).transpose(1, 0, 2))
    CW = 6 * D // NCORES
    nc = build_kmod()
    maps = []
    for i in range(NCORES):
        maps.append({
            "cc": cc,
            "wm": np.ascontiguousarray(inputs["w_mod"][:, :, i * CW:(i + 1) * CW]),
            "bm": np.ascontiguousarray(inputs["b_mod"][:, None, i * CW:(i + 1) * CW]),
        })
    res = run_bass_kernel_spmd(nc, maps, core_ids=list(range(NCORES)))
    mods = np.concatenate([r["mods"] for r in res.results], axis=-1)
    return mods


OBF_W = 2560
OF_W = 3072


def emit_rsqrt(P, out, in_, scale, rkey, wkey):
    epsb = P.const_eps()
    n = in_.shape[0]
    P.op("scalar", lambda e: e.activation(out=out, in_=in_, func=AF.Ln, scale=scale, bias=epsb[:n, :]),
         reads=[rkey, epsb], writes=[wkey])
    P.op("scalar", lambda e: e.activation(out=out, in_=out, func=AF.Exp, scale=-0.5),
         reads=[wkey], writes=[wkey])


def emit_modvec(P, vec, name):
    A = P.sb(name, [128, 2, 16], F32)
    for j in range(2):
        P.op("vector", lambda e, j=j: e.scalar_tensor_tensor(
            out=A[:, j, :], in0=vec[:, 1 + 2 * j, :], scalar=1.0, in1=vec[:, 0, :],
            op0=ALU.add, op1=ALU.mult), reads=[vec], writes=[A])
    return A


def declare_combine_inputs(nc):
    oes = [nc.dram_tensor("oe%d" % e, [NSLOT, D], F32, kind="ExternalInput").ap() for e in range(NE)]
    cpos = nc.dram_tensor("cpos", [128, NT, NE], I32, kind="ExternalInput").ap()
    caff = nc.dram_tensor("caff", [128, NT, NE], F32, kind="ExternalInput").ap()
    g2v = nc.dram_tensor("g2v", [2, D], F32, kind="ExternalInput").ap()
    return oes, cpos, caff, g2v


class Combiner:
    def __init__(self, P, oes, cpos, caff, g2v):
        self.P, self.oes, self.g2v = P, oes, g2v
        self.pos = P.sb("cpos_s", [128, NT, NE], I32)
        P.dma("sync", self.pos[:], cpos, writes=[self.pos])
        self.aff = P.sb("caff_s", [128, NT, NE], F32)
        P.dma("sync", self.aff[:], caff, writes=[self.aff])
        selm = P.sb("cselm", [128, NT, NE], F32)
        P.op("vector", lambda e: e.tensor_scalar(out=selm[:], in0=self.pos[:], scalar1=float(NSLOT) - 0.5, scalar2=None,
                                                 op0=ALU.is_lt), reads=[self.pos], writes=[selm])
        P.op("vector", lambda e: e.tensor_tensor(out=self.aff[:], in0=self.aff[:], in1=selm[:], op=ALU.mult),
             reads=[self.aff, selm], writes=[self.aff])
        self.g2 = P.sb("g2_s", [128, D], F32)
        self.acc = P.sb("cacc", [128, D], F32)
        self.gbl = [P.sb("cgb%d" % i, [128, D], F32) for i in range(4)]
        for gb in self.gbl:
            P.op("gpsimd", lambda e, gb=gb: e.memset(gb[:], 0.0), writes=[gb])
        self.n = 0
        self.g2_loaded = None

    def emit(self, t, xt):
        P = self.P
        j = 1 if t == NT - 1 else 0
        if self.g2_loaded != j:
            P.dma("sync", self.g2[:], self.g2v[j:j + 1, :].partition_broadcast(128), writes=[self.g2])
            self.g2_loaded = j
        acc = self.acc
        for e_ in range(NE):
            gb = self.gbl[self.n % 4]
            self.n += 1
            P.dma_fn("gpsimd", lambda e, gb=gb, e_=e_, t=t: e.indirect_dma_start(
                out=gb[:, :], out_offset=None, in_=self.oes[e_],
                in_offset=bass.IndirectOffsetOnAxis(ap=self.pos[:, t, e_:e_ + 1], axis=0),
                bounds_check=P.reg(e, NSLOT - 1), oob_is_err=False), reads=[self.pos, gb], writes=[gb])
            gate = self.aff[:, t, e_:e_ + 1]
            if e_ == 0:
                P.op("vector", lambda e, gb=gb, gate=gate: e.tensor_scalar(
                    out=acc[:], in0=gb[:], scalar1=gate, scalar2=None, op0=ALU.mult), reads=[gb, self.aff], writes=[acc])
            else:
                P.op("vector", lambda e, gb=gb, gate=gate: e.scalar_tensor_tensor(
                    out=acc[:], in0=gb[:], scalar=gate, in1=acc[:], op0=ALU.mult, op1=ALU.add),
                    reads=[gb, acc, self.aff], writes=[acc])
        P.op("vector", lambda e: e.tensor_tensor(out=self.acc[:], in0=self.acc[:], in1=self.g2[:], op=ALU.mult),
             reads=[self.acc, self.g2], writes=[self.acc])
        P.op("vector", lambda e, xt=xt: e.tensor_tensor(out=xt[:], in0=xt[:], in1=self.acc[:], op=ALU.add),
             reads=[xt, self.acc], writes=[xt])


def build_ka(combine=False):
    nc = bass.Bass("TRN2", target_bir_lowering=False)
    x_in = nc.dram_tensor("x_in", [NT * 128, D], F32, kind="ExternalInput").ap()
    w_in = nc.dram_tensor("w_in", [D, INW], F32, kind="ExternalInput").ap()
    vecs = nc.dram_tensor("vecs", [128, 5, 16], F32, kind="ExternalInput").ap()
    gains = nc.dram_tensor("gains", [128, 2, 128], F32, kind="ExternalInput").ap()
    rope = nc.dram_tensor("rope", [128, NT, 2, 64], F32, kind="ExternalInput").ap()
    wa2 = nc.dram_tensor("wa2", [16, 2, 512], F32, kind="ExternalInput").ap()
    ba = nc.dram_tensor("ba", [1, 2, 512], F32, kind="ExternalInput").ap()
    out_bf = nc.dram_tensor("out_bf", [NT * 128, OBF_W], BF16, kind="ExternalOutput").ap()
    out_f = nc.dram_tensor("out_f", [NT * 128, OF_W], F32, kind="ExternalOutput").ap()
    P = Prog(nc)
    comb = x_new = None
    if combine:
        comb = Combiner(P, *declare_combine_inputs(nc))
        x_new = nc.dram_tensor("x_new", [NT * 128, D], F32, kind="ExternalOutput").ap()
    emit_ka_body(P, x_in, w_in, vecs, gains, rope, wa2, ba, out_bf, out_f, comb, x_new)
    P.finish()
    return nc


def emit_ka_body(P, x_in, w_in, vecs, gains, rope, wa2, ba, out_bf, out_f, comb=None, x_new=None):
    identf = make_ident(P, "identf", F32)
    vec = P.sb("vec", [128, 5, 16], F32)
    P.dma("sync", vec[:], vecs, writes=[vec])
    A = emit_modvec(P, vec, "Amod")
    gn = P.sb("gn", [128, 2, 128], F32)
    P.dma("sync", gn[:], gains, writes=[gn])
    rp = P.sb("rp", [128, NT, 2, 64], F32)
    P.dma("sync", rp[:], rope, writes=[rp])
    wa2s = P.sb("wa2s", [16, 2, 512], F32)
    P.dma("sync", wa2s[:], wa2, writes=[wa2s])
    bas = P.sb("bas", [1, 2, 512], F32)
    P.dma("sync", bas[:], ba, writes=[bas])
    ones1 = P.sb("ones1", [1, 128], F32)
    P.op("gpsimd", lambda e: e.memset(ones1[:], 1.0), writes=[ones1])

    hT = P.sb("hT", [128, NT, 16, 128], BF16)
    xts = [P.sb("xt%d" % i, [128, D], F32) for i in range(2)]
    junk = P.sb("junk", [128, D], BF16)
    ss = P.sb("ss", [128, NT], F32)
    rstd = P.sb("rstd", [128, NT], F32)
    tps = [P.ps("tp%d" % i, [128, 8, 128], F32) for i in range(2)]
    def phase1(t):
        xt = xts[t % 2]
        P.dma("sync", xt[:], x_in[t * 128:(t + 1) * 128, :], writes=[xt])
        if comb is not None:
            comb.emit(t, xt)
            P.dma("sync", x_new[t * 128:(t + 1) * 128, :], xt[:], reads=[xt])
        P.op("scalar", lambda e, xt=xt, t=t: e.activation(
            out=junk[:], in_=xt[:], func=AF.Square, accum_out=ss[:, t:t + 1]),
            reads=[xt], writes=[junk, (ss, t)])
        emit_rsqrt(P, rstd[:, t:t + 1], ss[:, t:t + 1], 1.0 / D, (ss, t), (rstd, t))
        P.op("vector", lambda e, xt=xt, t=t: e.tensor_scalar(
            out=xt[:], in0=xt[:], scalar1=rstd[:, t:t + 1], scalar2=None, op0=ALU.mult),
            reads=[xt, (rstd, t)], writes=[xt])
        mi = 1 if t == NT - 1 else 0
        for half in range(2):
            tp = tps[half]
            for j in range(8):
                kc = half * 8 + j
                P.op("tensor", lambda e, tp=tp, j=j, kc=kc, xt=xt: e.transpose(
                    out=tp[:, j, :], in_=xt[:, kc * 128:(kc + 1) * 128], identity=identf[:]),
                    reads=[xt, identf], writes=[tp])
            for j in range(8):
                kc = half * 8 + j
                P.op("scalar", lambda e, tp=tp, j=j, kc=kc, t=t, mi=mi: e.activation(
                    out=hT[:, t, kc, :], in_=tp[:, j, :], func=AF.Identity,
                    scale=A[:, mi, kc:kc + 1], bias=vec[:, 2 + 2 * mi, kc:kc + 1]),
                    reads=[tp, A, vec], writes=[(hT, t)])

    for t in range(NT):
        phase1(t)

    wbs = [P.sb("wb%d" % i, [128, 16, 512], BF16) for i in range(2)]
    mps = [P.ps("mp%d" % i, [128, 512], F32) for i in range(2)]
    zp = P.ps("zp", [128, 512], F32)
    tpa = P.ps("tpa", [16, 128], F32)
    sq = P.sb("sq", [128, 512], F32)
    ssq = P.sb("ssq", [128, 4], F32)
    qn = P.sb("qn", [128, 512], F32)
    rt = [P.sb("rt%d" % i, [128, 256], F32) for i in range(2)]
    sbf = [P.sb("sbf%d" % i, [128, 512], BF16) for i in range(4)]
    sf = [P.sb("sf%d" % i, [128, 512], F32) for i in range(4)]
    asb = P.sb("asb", [128, 32], F32)
    aT = P.sb("aT", [16, 128], F32)
    ez = P.sb("ez", [128, 512], F32)
    cnt = {"bf": 0, "f": 0}
    w_v = w_in.rearrange("(kc p) n -> p kc n", p=128)

    def stage(kind):
        i = cnt[kind]
        cnt[kind] += 1
        return (sbf if kind == "bf" else sf)[i % 4]

    def qk_epi(mp, c0, nh, gi, t, dst):
        w = nh * 128
        P.op("scalar", lambda e: e.activation(out=sq[:, :w], in_=mp[:, c0:c0 + w], func=AF.Square),
             reads=[mp], writes=[sq])
        P.op("vector", lambda e: e.tensor_reduce(
            out=ssq[:, :nh], in_=sq[:, :w].rearrange("p (h d) -> p h d", h=nh), axis=AX.X, op=ALU.add),
            reads=[sq], writes=[ssq])
        emit_rsqrt(P, ssq[:, :nh], ssq[:, :nh], 1.0 / 128, ssq, ssq)
        q3 = qn[:, :w].rearrange("p (h d) -> p h d", h=nh)
        P.op("vector", lambda e: e.tensor_tensor(
            out=q3, in0=mp[:, c0:c0 + w].rearrange("p (h d) -> p h d", h=nh),
            in1=ssq[:, :nh].unsqueeze(2).broadcast_to([128, nh, 128]), op=ALU.mult),
            reads=[mp, ssq], writes=[qn])
        P.op("vector", lambda e: e.tensor_tensor(
            out=q3, in0=q3, in1=gn[:, gi, :].unsqueeze(1).broadcast_to([128, nh, 128]), op=ALU.mult),
            reads=[qn, gn], writes=[qn])
        q4 = qn[:, :w].rearrange("p (h i two) -> p h i two", h=nh, two=2)
        d4 = dst.rearrange("p (h i two) -> p h i two", h=nh, two=2)
        cosb = rp[:, t, 0, :].unsqueeze(1).broadcast_to([128, nh, 64])
        sinb = rp[:, t, 1, :].unsqueeze(1).broadcast_to([128, nh, 64])
        r0 = rt[0][:, :nh * 64].rearrange("p (h i) -> p h i", h=nh)
        r1 = rt[1][:, :nh * 64].rearrange("p (h i) -> p h i", h=nh)
        for (o, a, ca, b, cb_, op) in ((0, 0, cosb, 1, sinb, ALU.subtract), (1, 0, sinb, 1, cosb, ALU.add)):
            P.op("vector", lambda e, a=a, ca=ca: e.tensor_tensor(out=r0, in0=q4[:, :, :, a], in1=ca, op=ALU.mult),
                 reads=[qn, rp], writes=[rt[0]])
            P.op("vector", lambda e, b=b, cb_=cb_: e.tensor_tensor(out=r1, in0=q4[:, :, :, b], in1=cb_, op=ALU.mult),
                 reads=[qn, rp], writes=[rt[1]])
            P.op("vector", lambda e, o=o, op=op: e.tensor_tensor(out=d4[:, :, :, o], in0=r0, in1=r1, op=op),
                 reads=[rt[0], rt[1]], writes=[dst.tensor])

    it = 0
    for cb in range(10):
        n = 512 if cb < 9 else 32
        wb = wbs[cb % 2]
        for q in range(4):
            P.dma("gpsimd", wb[:, 4 * q:4 * q + 4, :n], w_v[:, 4 * q:4 * q + 4, cb * 512:cb * 512 + n],
                  writes=[(wb, q)])
        for t in range(NT):
            mp = mps[it % 2]
            it += 1
            for kc in range(16):
                P.op("tensor", lambda e, mp=mp, t=t, kc=kc, wb=wb, n=n: e.matmul(
                    mp[:, :n], lhsT=hT[:, t, kc, :], rhs=wb[:, kc, :n], start=(kc == 0), stop=(kc == 15)),
                    reads=[(hT, t), (wb, kc // 4)], writes=[mp])
            rows = slice(t * 128, (t + 1) * 128)
            if cb in (0, 1):
                st = stage("bf")
                qk_epi(mp, 0, 4, 0, t, st[:, :])
                P.dma("sync", out_bf[rows, cb * 512:(cb + 1) * 512], st[:], reads=[st])
            elif cb == 2:
                st = stage("bf")
                qk_epi(mp, 0, 2, 1, t, st[:, 0:256])
                P.op("scalar", lambda e, st=st, mp=mp: e.activation(out=st[:, 256:512], in_=mp[:, 256:512], func=AF.Copy),
                     reads=[mp], writes=[st])
                P.dma("sync", out_bf[rows, 1024:1536], st[:], reads=[st])
            elif cb in (3, 4):
                st = stage("f")
                sc_ = 128 ** -0.5 if cb == 3 else 1.0
                P.op("scalar", lambda e, st=st, mp=mp, sc_=sc_: e.activation(out=st[:], in_=mp[:], func=AF.Copy, scale=sc_),
                     reads=[mp], writes=[st])
                P.dma("sync", out_f[rows, (cb - 3) * 512:(cb - 2) * 512], st[:], reads=[st])
            elif cb in (5, 6):
                st = stage("bf")
                P.op("scalar", lambda e, st=st, mp=mp: e.activation(out=st[:], in_=mp[:], func=AF.Copy),
                     reads=[mp], writes=[st])
                P.dma("sync", out_bf[rows, 1536 + (cb - 5) * 512:1536 + (cb - 4) * 512], st[:], reads=[st])
            elif cb in (7, 8):
                st = stage("f")
                P.op("scalar", lambda e, st=st, mp=mp: e.activation(out=st[:], in_=mp[:], func=AF.Silu),
                     reads=[mp], writes=[st])
                P.dma("sync", out_f[rows, 1024 + (cb - 7) * 512:1024 + (cb - 6) * 512], st[:], reads=[st])
            else:
                P.op("scalar", lambda e, mp=mp: e.activation(out=asb[:], in_=mp[:, :32], func=AF.Copy),
                     reads=[mp], writes=[asb])
                for d in range(2):
                    P.op("tensor", lambda e, d=d: e.transpose(out=tpa[:], in_=asb[:, d * 16:(d + 1) * 16], identity=identf[:]),
                         reads=[asb, identf], writes=[tpa])
                    P.op("vector", lambda e: e.tensor_copy(out=aT[:], in_=tpa[:]), reads=[tpa], writes=[aT])
                    P.op("tensor", lambda e, d=d: e.matmul(zp[:], lhsT=aT[:], rhs=wa2s[:, d, :], start=True, stop=False),
                         reads=[aT, wa2s], writes=[zp])
                    P.op("tensor", lambda e, d=d: e.matmul(zp[:], lhsT=ones1[:], rhs=bas[:, d, :], start=False, stop=True),
                         reads=[ones1, bas], writes=[zp])
                    st = stage("f")
                    P.op("scalar", lambda e: e.activation(out=ez[:], in_=zp[:], func=AF.Exp, scale=-1.0),
                         reads=[zp], writes=[ez])
                    P.op("scalar", lambda e: e.activation(out=ez[:], in_=ez[:], func=AF.Ln, bias=1.0),
                         reads=[ez], writes=[ez])
                    P.op("vector", lambda e, st=st: e.tensor_scalar(out=st[:], in0=ez[:], scalar1=-1.0 / 16, scalar2=None, op0=ALU.mult),
                         reads=[ez], writes=[st])
                    P.dma("sync", out_f[rows, 2048 + d * 512:2048 + (d + 1) * 512], st[:], reads=[st])


def fm(v):
    return np.ascontiguousarray(np.asarray(v).reshape(16, 128).T)


def rope_tables():
    rows = SEQ // 64
    row = np.repeat(np.arange(rows, dtype=np.float32), 64)
    col = np.tile(np.arange(64, dtype=np.float32), rows)
    inv = (np.float32(10000.0) ** (-np.arange(0, 64, 2, dtype=np.float32) / np.float32(64))).astype(np.float32)
    ang = np.concatenate([row[:, None] * inv, col[:, None] * inv], axis=-1).astype(np.float32)
    return np.cos(ang).astype(np.float32), np.sin(ang).astype(np.float32)


def core_tokens(a_lat, a_ctx, i):
    return np.concatenate([a_lat[i * 1024:(i + 1) * 1024], a_ctx[(i % 2) * 128:(i % 2) * 128 + 128]], axis=0)


def ka_static_inputs(inputs, mods, l):
    m_lat, m_ctx = mods[l, 0], mods[l, 1]
    vec = np.stack([fm(inputs["norm_mix"][l]), fm(m_lat[D:2 * D]), fm(m_lat[0:D]),
                    fm(m_ctx[D:2 * D]), fm(m_ctx[0:D])], axis=1).astype(np.float32)
    gains = np.ascontiguousarray(np.broadcast_to(
        np.stack([inputs["q_gain"][l], inputs["k_gain"][l]])[None], (128, 2, 128))).astype(np.float32)
    cos, sin = rope_tables()
    ropes = []
    for i in range(NCORES):
        c = core_tokens(cos, np.ones((CTX, 64), np.float32), i)
        s = core_tokens(sin, np.zeros((CTX, 64), np.float32), i)
        r = np.stack([c, s], axis=1).reshape(NT, 128, 2, 64).transpose(1, 0, 2, 3)
        ropes.append(np.ascontiguousarray(r))
    wa2 = np.ascontiguousarray(inputs["w_gla_a2"][l].transpose(1, 0, 2))
    ba = np.ascontiguousarray(inputs["b_gla_a"][l][None])
    return vec, gains, ropes, wa2, ba


def combine_maps(mods, l_prev, kd_outs, aff_lat, aff_ctx):
    oes = {}
    for i, (oe, pl, pc) in enumerate(kd_outs):
        oes["oe%d" % (2 * i)] = np.ascontiguousarray(oe[0])
        oes["oe%d" % (2 * i + 1)] = np.ascontiguousarray(oe[1])
    pos_lat = np.concatenate([o[1].transpose(2, 0, 1).reshape(SEQ, 2) for o in kd_outs], axis=1)
    pos_ctx = np.concatenate([o[2].transpose(2, 0, 1).reshape(CTX, 2) for o in kd_outs], axis=1)
    g2v = np.stack([mods[l_prev, 0][5 * D:6 * D], mods[l_prev, 1][5 * D:6 * D]]).astype(np.float32)
    maps = []
    for i in range(NCORES):
        cp = core_tokens(pos_lat, pos_ctx, i).reshape(NT, 128, NE).transpose(1, 0, 2)
        m = dict(oes)
        m["cpos"] = np.ascontiguousarray(cp).astype(np.int32)
        m["caff"] = np.ascontiguousarray(core_tokens(aff_lat, aff_ctx, i).reshape(NT, 128, NE).transpose(1, 0, 2))
        m["g2v"] = g2v
        maps.append(m)
    return maps


def run_ka(inputs, mods, l, x_lat, x_ctx, nc=None, comb_maps=None):
    nc = nc or build_ka(combine=comb_maps is not None)
    vec, gains, ropes, wa2, ba = ka_static_inputs(inputs, mods, l)
    w_in = np.ascontiguousarray(inputs["w_in"][l])
    maps = []
    for i in range(NCORES):
        maps.append({"x_in": core_tokens(x_lat, x_ctx, i), "w_in": w_in, "vecs": vec, "gains": gains,
                     "rope": ropes[i], "wa2": wa2, "ba": ba})
        if comb_maps is not None:
            maps[-1].update(comb_maps[i])
    res = run_bass_kernel_spmd(nc, maps, core_ids=list(range(NCORES)))
    if comb_maps is not None:
        return [(r["out_bf"], r["out_f"], r["x_new"]) for r in res.results]
    return [(r["out_bf"], r["out_f"]) for r in res.results]


def gather_tokens(per_core):
    lat = np.concatenate([a[:1024] for a in per_core], axis=0)
    ctx = np.concatenate([per_core[0][1024:], per_core[1][1024:]], axis=0)
    return lat, ctx


NTOK = CTX + SEQ
NTT = NTOK // 128
GG = 6
GLA_EVERY = (1, 1)


def build_kb(do_gla=True, do_attn=True, nblk=None, ngrp=None):
    nc = bass.Bass("TRN2", target_bir_lowering=False)
    qT = nc.dram_tensor("qT", [128, NTOK], BF16, kind="ExternalInput").ap()
    kT = nc.dram_tensor("kT", [128, NTOK], BF16, kind="ExternalInput").ap()
    v = nc.dram_tensor("v", [NTOK, 128], BF16, kind="ExternalInput").ap()
    gqT = nc.dram_tensor("gqT", [128, NTOK], F32, kind="ExternalInput").ap()
    gkT = nc.dram_tensor("gkT", [128, NTOK], F32, kind="ExternalInput").ap()
    gk = nc.dram_tensor("gk", [NTOK, 128], F32, kind="ExternalInput").ap()
    gv = nc.dram_tensor("gv", [NTOK, 256], BF16, kind="ExternalInput").ap()
    la = nc.dram_tensor("la", [NTOK, 128], F32, kind="ExternalInput").ap()
    oT = nc.dram_tensor("oT", [128, NTOK], BF16, kind="ExternalOutput").ap()
    og = nc.dram_tensor("og", [NTOK, 256], F32, kind="ExternalOutput").ap()
    P = Prog(nc)
    if do_gla and do_attn:
        emit_attn(P, qT, kT, v, oT, nblk, side=gla_gen(P, gqT, gkT, gk, gv, la, og, ngrp))
    elif do_gla:
        emit_gla(P, gqT, gkT, gk, gv, la, og, ngrp)
    elif do_attn:
        emit_attn(P, qT, kT, v, oT, nblk)
    P.finish()
    return nc


class PV:
    def __init__(self, t, c0, n):
        self.t, self.c0, self.n, self.name = t, c0, n, t.name

    def __getitem__(self, key):
        if not isinstance(key, tuple):
            key = (key, slice(None))
        rows, cols = key
        start = cols.start or 0
        stop = self.n if cols.stop is None else cols.stop
        return self.t[rows, self.c0 + start:self.c0 + stop]


def make_tri(P, name, upper):
    m = P.sb(name, [128, 128], F32)
    P.op("gpsimd", lambda e: e.memset(m[:], 0.0), writes=[m])
    for b in range(2):
        blk = m[b * 64:(b + 1) * 64, b * 64:(b + 1) * 64]
        P.op("gpsimd", lambda e, blk=blk: e.memset(blk, 1.0), reads=[m], writes=[m])
        if upper:
            P.op("gpsimd", lambda e, blk=blk: e.affine_select(
                out=blk, in_=blk, pattern=[[1, 64]], compare_op=ALU.is_ge, fill=0.0, base=0,
                channel_multiplier=-1), reads=[m], writes=[m])
        else:
            P.op("gpsimd", lambda e, blk=blk: e.affine_select(
                out=blk, in_=blk, pattern=[[-1, 64]], compare_op=ALU.is_gt, fill=0.0, base=0,
                channel_multiplier=1), reads=[m], writes=[m])
    return m


def gla_gen(P, gqT, gkT, gk, gv, la, og, ngrp_lim=None):
    U2 = make_tri(P, "U2", True)
    L2 = make_tri(P, "L2", False)
    ngrp = ngrp_lim or NTT // GG
    gqT_s = [P.sb("gqT_s%d" % i, [128, GG * 128], F32) for i in range(2)]
    gkT_s = [P.sb("gkT_s%d" % i, [128, GG * 128], F32) for i in range(2)]
    gk_s = [P.sb("gk_s%d" % i, [128, GG, 128], F32) for i in range(2)]
    gv_s = [P.sb("gv_s%d" % i, [128, GG, 256], BF16) for i in range(2)]
    la_s = [P.sb("la_s%d" % i, [128, GG, 128], F32) for i in range(2)]
    bankA = P.ps("gla_bankA", [128, 512], F32)
    bankB = P.ps("gla_bankB", [128, 512], F32)
    bT_ps, bl_ps, at_ps = PV(bankA, 0, 128), PV(bankA, 128, 128), PV(bankA, 256, 128)
    o_ps = [PV(bankB, 0, 256)] * 2
    ds_ps = [P.ps("ds_ps%d" % i, [128, 256], F32) for i in range(2)]
    eb = [P.sb("eb%d" % i, [128, 128], F32) for i in range(2)]
    enb = P.sb("enb", [128, 128], F32)
    ekh = P.sb("ekh", [128, 128], F32)
    qtT = [P.sb("qtT%d" % i, [128, 128], BF16) for i in range(2)]
    ktT = P.sb("ktT", [128, 128], BF16)
    kh = [P.sb("kh%d" % i, [128, 128], BF16) for i in range(2)]
    atm = P.sb("atm", [128, 128], BF16)
    S = [P.sb("S%d" % i, [128, 256], F32) for i in range(2)]
    Sb = [P.sb("Sb%d" % i, [128, 256], BF16) for i in range(4)]
    ost = [P.sb("ost%d" % i, [128, 256], F32) for i in range(2)]
    P.op("gpsimd", lambda e: e.memset(S[0][:], 0.0), writes=[S[0]])
    P.op("gpsimd", lambda e: e.memset(Sb[0][:], 0.0), writes=[Sb[0]])
    sc = 0
    def load_group(g):
        b = g % 2
        r0 = g * GG * 128
        P.dma("sync", gqT_s[b][:], gqT[:, r0:r0 + GG * 128], writes=[gqT_s[b]])
        P.dma("sync", gkT_s[b][:], gkT[:, r0:r0 + GG * 128], writes=[gkT_s[b]])
        P.dma("sync", gk_s[b][:], gk[r0:r0 + GG * 128, :].rearrange("(t p) d -> p t d", p=128), writes=[gk_s[b]])
        P.dma("sync", gv_s[b][:], gv[r0:r0 + GG * 128, :].rearrange("(t p) d -> p t d", p=128), writes=[gv_s[b]])
        P.dma("sync", la_s[b][:], la[r0:r0 + GG * 128, :].rearrange("(t p) d -> p t d", p=128), writes=[la_s[b]])

    load_group(0)
    for g in range(ngrp):
        b = g % 2
        if g + 1 < ngrp:
            load_group(g + 1)
        gq_b, gkT_b, gk_b, gv_b = gqT_s[b], gkT_s[b], gk_s[b], gv_s[b]
        for tt in range(GG):
            ti = g * GG + tt
            p2 = ti % 2
            la_t = la_s[b][:, tt, :]
            cols = slice(tt * 128, (tt + 1) * 128)
            P.op("tensor", lambda e, la_t=la_t: e.matmul(bT_ps[:], lhsT=la_t, rhs=U2[:], start=True, stop=True),
                 reads=[la_s[b], U2], writes=[bT_ps])
            P.op("tensor", lambda e, la_t=la_t: e.matmul(bl_ps[:], lhsT=L2[:], rhs=la_t, start=True, stop=True),
                 reads=[la_s[b], L2], writes=[bl_ps])
            yield
            ebt = eb[p2]
            P.op("scalar", lambda e, ebt=ebt: e.activation(out=ebt[:], in_=bT_ps[:], func=AF.Exp), reads=[bT_ps], writes=[ebt])
            P.op("scalar", lambda e: e.activation(out=enb[:], in_=bT_ps[:], func=AF.Exp, scale=-1.0), reads=[bT_ps], writes=[enb])
            P.op("scalar", lambda e: e.activation(out=ekh[:], in_=bl_ps[:], func=AF.Exp), reads=[bl_ps], writes=[ekh])
            yield
            q_t = qtT[p2]
            kh_t = kh[p2]
            P.op("vector", lambda e, q_t=q_t, ebt=ebt, cols=cols, gq_b=gq_b: e.tensor_tensor(out=q_t[:], in0=gq_b[:, cols], in1=ebt[:], op=ALU.mult),
                 reads=[gqT_s[b], ebt], writes=[q_t])
            P.op("vector", lambda e, cols=cols, gkT_b=gkT_b: e.tensor_tensor(out=ktT[:], in0=gkT_b[:, cols], in1=enb[:], op=ALU.mult),
                 reads=[gkT_s[b], enb], writes=[ktT])
            P.op("vector", lambda e, kh_t=kh_t, tt=tt, gk_b=gk_b: e.tensor_tensor(out=kh_t[:], in0=gk_b[:, tt, :], in1=ekh[:], op=ALU.mult),
                 reads=[gk_s[b], ekh], writes=[kh_t])
            yield
            P.op("tensor", lambda e, q_t=q_t: e.matmul(at_ps[:], lhsT=ktT[:], rhs=q_t[:], start=True, stop=True),
                 reads=[ktT, q_t], writes=[at_ps])
            yield
            P.op("vector", lambda e: e.tensor_tensor(out=atm[:], in0=at_ps[:], in1=U2[:], op=ALU.mult),
                 reads=[at_ps, U2], writes=[atm])
            yield
            ops_ = o_ps[p2]
            gv_t = gv_s[b][:, tt, :]
            P.op("tensor", lambda e, ops_=ops_, gv_t=gv_t: e.matmul(ops_[:], lhsT=atm[:], rhs=gv_t, start=True, stop=False),
                 reads=[atm, gv_s[b]], writes=[ops_])
            for c in range(2):
                rs = slice(c * 64, (c + 1) * 64)
                Sb_c = Sb[sc % 4]
                P.op("tensor", lambda e, ops_=ops_, q_t=q_t, rs=rs, Sb_c=Sb_c, c=c: e.matmul(
                    ops_[rs, :], lhsT=q_t[:, rs], rhs=Sb_c[:], start=False, stop=True),
                    reads=[q_t, Sb_c], writes=[ops_])
                dsp = ds_ps[sc % 2]
                P.op("tensor", lambda e, dsp=dsp, kh_t=kh_t, rs=rs, tt=tt, gv_b=gv_b: e.matmul(
                    dsp[:], lhsT=kh_t[rs, :], rhs=gv_b[rs, tt, :], start=True, stop=True),
                    reads=[kh_t, gv_s[b]], writes=[dsp])
                yield
                S_cur, S_nxt = S[sc % 2], S[(sc + 1) % 2]
                col = c * 64 + 63
                P.op("vector", lambda e, S_cur=S_cur, S_nxt=S_nxt, ebt=ebt, col=col, dsp=dsp: e.scalar_tensor_tensor(
                    out=S_nxt[:], in0=S_cur[:], scalar=ebt[:, col:col + 1], in1=dsp[:], op0=ALU.mult, op1=ALU.add),
                    reads=[S_cur, ebt, dsp], writes=[S_nxt])
                Sb_n = Sb[(sc + 1) % 4]
                P.op("vector", lambda e, Sb_n=Sb_n, S_nxt=S_nxt: e.tensor_copy(out=Sb_n[:], in_=S_nxt[:]),
                     reads=[S_nxt], writes=[Sb_n])
                sc += 1
                yield
            o_t = ost[p2]
            P.op("scalar", lambda e, o_t=o_t, ops_=ops_: e.activation(out=o_t[:], in_=ops_[:], func=AF.Copy),
                 reads=[ops_], writes=[o_t])
            P.dma("sync", og[ti * 128:(ti + 1) * 128, :], o_t[:], reads=[o_t])
            yield


def emit_gla(P, gqT, gkT, gk, gv, la, og, ngrp_lim=None):
    for _ in gla_gen(P, gqT, gkT, gk, gv, la, og, ngrp_lim):
        pass


def emit_attn(P, qT, kT, v, oT, nblk=None, side=None):
    q_s = P.sb("q_s", [128, NTOK], BF16)
    k_s = P.sb("k_s", [128, NTOK], BF16)
    v_s = P.sb("v_s", [128, NTT, 128], BF16)
    for j in range(4):
        cs = slice(j * (NTOK // 4), (j + 1) * (NTOK // 4))
        P.dma("sync", q_s[:, cs], qT[:, cs], writes=[(q_s, j)])
        P.dma("scalar", k_s[:, cs], kT[:, cs], writes=[(k_s, j)])
    vv = v.rearrange("(t p) d -> p t d", p=128)
    for j in range(3):
        ts_ = slice(j * 22, (j + 1) * 22)
        P.dma("sync", v_s[:, ts_, :], vv[:, ts_, :], writes=[(v_s, j)])
    ones = P.sb("ones_f32", [128, 128], F32)
    P.op("gpsimd", lambda e: e.memset(ones[:], 1.0), writes=[ones])
    pacc = [P.sb("pacc%d" % i, [128, 512], F32) for i in range(2)]
    st_ps = [P.ps("st_ps%d" % i, [128, 512], F32) for i in range(2)]
    oa_ps = [P.ps("oa_ps", [128, 512], F32)] * 2
    dn_ps = [P.ps("dn_ps", [128, 512], F32)] * 2
    pT = [P.sb("pT%d" % i, [128, 512], BF16) for i in range(3)]
    rden = P.sb("rden", [128, 512], F32)
    ob = [P.sb("ob%d" % i, [128, 512], BF16) for i in range(2)]
    blocks = [(0, CTX, 2)] + [(CTX + i * 512, 512, NTT) for i in range(SEQ // 512)]
    qkeys = [(q_s, j) for j in range(4)]
    if nblk:
        blocks = blocks[:nblk]
    iters = [(bi, q0, n, ns, s_) for bi, (q0, n, ns) in enumerate(blocks) for s_ in range(ns)]

    def emit_s(idx):
        bi, q0, n, ns, s_ = iters[idx]
        sp = st_ps[idx % 2]
        kk = (k_s, (s_ * 128) // (NTOK // 4))
        kk2 = (k_s, (s_ * 128 + 127) // (NTOK // 4))
        P.op("tensor", lambda e: e.matmul(sp[:, :n], lhsT=k_s[:, s_ * 128:(s_ + 1) * 128], rhs=q_s[:, q0:q0 + n],
                                          start=True, stop=True), reads=[kk, kk2] + qkeys, writes=[sp])

    emit_s(0)
    for idx, (bi, q0, n, ns, s_) in enumerate(iters):
        if idx + 1 < len(iters):
            emit_s(idx + 1)
        oa, dn = oa_ps[0], dn_ps[0]
        sp, pt = st_ps[idx % 2], pT[idx % 3]
        P.op("scalar", lambda e, sp=sp, pt=pt, n=n: e.activation(
            out=pt[:, :n], in_=sp[:, :n], func=AF.Exp, scale=128 ** -0.5), reads=[sp], writes=[pt])
        P.op("tensor", lambda e, pt=pt, s_=s_, n=n, ns=ns: e.matmul(
            oa[:, :n], lhsT=v_s[:, s_, :], rhs=pt[:, :n], start=(s_ == 0), stop=(s_ == ns - 1)),
            reads=[(v_s, s_ // 22), pt], writes=[oa])
        pa = pacc[bi % 2]
        if s_ == 0:
            P.op("vector", lambda e, pa=pa, pt=pt, n=n: e.tensor_copy(out=pa[:, :n], in_=pt[:, :n]), reads=[pt], writes=[pa])
        else:
            P.op("vector", lambda e, pa=pa, pt=pt, n=n: e.tensor_tensor(out=pa[:, :n], in0=pa[:, :n], in1=pt[:, :n], op=ALU.add),
                 reads=[pt, pa], writes=[pa])
        if s_ == ns - 1:
            P.op("tensor", lambda e, pa=pa, n=n: e.matmul(dn[:, :n], lhsT=ones[:], rhs=pa[:, :n], start=True, stop=True),
                 reads=[ones, pa], writes=[dn])
        if s_ == ns - 1:
            o_b = ob[bi % 2]
            P.op("vector", lambda e, n=n: e.reciprocal(out=rden[:, :n], in_=dn[:, :n]), reads=[dn], writes=[rden])
            P.op("vector", lambda e, o_b=o_b, n=n: e.tensor_tensor(out=o_b[:, :n], in0=oa[:, :n], in1=rden[:, :n], op=ALU.mult),
                 reads=[oa, rden], writes=[o_b])
            P.dma("sync", oT[:, q0:q0 + n], o_b[:, :n], reads=[o_b])
        if side is not None:
            for _ in range(max(1, GLA_EVERY[0] // GLA_EVERY[1]) if idx % GLA_EVERY[1] < GLA_EVERY[0] else 0):
                next(side, None)
    if side is not None:
        for _ in side:
            pass


def run_kb(bf_lat, bf_ctx, f_lat, f_ctx, nc=None):
    nc = nc or build_kb()
    bf = np.concatenate([bf_ctx, bf_lat], axis=0)
    f = np.concatenate([f_ctx, f_lat], axis=0)
    rev = np.concatenate([np.arange(CTX)[::-1], CTX + np.arange(SEQ)[::-1]])
    maps = []
    for i in range(NCORES):
        kv, h, d = i // 4, i % 4, i // 4
        order = rev if d == 1 else np.arange(NTOK)
        la = f[:, 2048 + d * 512 + h * 128:2048 + d * 512 + (h + 1) * 128][order]
        gq = f[:, h * 128:(h + 1) * 128][order]
        gk = f[:, 512 + h * 128:512 + (h + 1) * 128][order]
        gv = bf[:, 1536 + h * 256:1536 + (h + 1) * 256][order]
        maps.append({
            "qT": np.ascontiguousarray(bf[:, i * 128:(i + 1) * 128].T),
            "kT": np.ascontiguousarray(bf[:, 1024 + kv * 128:1024 + (kv + 1) * 128].T),
            "v": np.ascontiguousarray(bf[:, 1280 + kv * 128:1280 + (kv + 1) * 128]),
            "gqT": np.ascontiguousarray(gq.T), "gkT": np.ascontiguousarray(gk.T),
            "gk": np.ascontiguousarray(gk), "gv": np.ascontiguousarray(gv), "la": np.ascontiguousarray(la),
        })
    res = run_bass_kernel_spmd(nc, maps, core_ids=list(range(NCORES)))
    inv = np.argsort(rev)
    attnT = [r["oT"] for r in res.results]
    og = [r["og"] if i < 4 else r["og"][inv] for i, r in enumerate(res.results)]
    return attnT, og


def build_kc():
    nc = bass.Bass("TRN2", target_bir_lowering=False)
    R = NT * 128
    attnT = nc.dram_tensor("attnT", [128, 8, R], BF16, kind="ExternalInput").ap()
    ogf = nc.dram_tensor("ogf", [R, 1024], F32, kind="ExternalInput").ap()
    ogb = nc.dram_tensor("ogb", [R, 1024], F32, kind="ExternalInput").ap()
    sr = nc.dram_tensor("sr", [R, 1024], F32, kind="ExternalInput").ap()
    x_in = nc.dram_tensor("x_in", [R, D], F32, kind="ExternalInput").ap()
    w_out = nc.dram_tensor("w_out", [D, D], F32, kind="ExternalInput").ap()
    w_r = nc.dram_tensor("w_r", [D, NE], F32, kind="ExternalInput").ap()
    vecs = nc.dram_tensor("vecs", [7, D], F32, kind="ExternalInput").ap()
    ggain = nc.dram_tensor("ggain", [1, 256], F32, kind="ExternalInput").ap()
    x_out = nc.dram_tensor("x_out", [R, D], F32, kind="ExternalOutput").ap()
    h2_out = nc.dram_tensor("h2_out", [R, D], BF16, kind="ExternalOutput").ap()
    aff_out = nc.dram_tensor("aff_out", [R, NE], F32, kind="ExternalOutput").ap()
    P = Prog(nc)
    identf = make_ident(P, "identf", F32)
    identb = P.sb("identb", [128, 128], BF16)
    P.op("vector", lambda e: e.tensor_copy(out=identb[:], in_=identf[:]), reads=[identf], writes=[identb])
    wo = P.sb("wo", [128, 16, D], BF16)
    wv = w_out.rearrange("(kc p) n -> p kc n", p=128)
    for q in range(16):
        P.dma("gpsimd", wo[:, q, :].rearrange("p (a n) -> p a n", a=4), wv[:, q, :].rearrange("p (a n) -> p a n", a=4),
              writes=[(wo, q)])
    wr = P.sb("wr", [128, 16, NE], F32)
    P.dma("sync", wr[:], w_r.rearrange("(kc p) n -> p kc n", p=128), writes=[wr])
    gg = P.sb("gg", [128, 256], F32)
    P.dma("sync", gg[:], ggain.partition_broadcast(128), writes=[gg])
    nf = P.sb("nf", [128, D], F32)
    P.dma("sync", nf[:], vecs[0:1, :].partition_broadcast(128), writes=[nf])
    g1 = P.sb("g1", [128, D], F32)
    A2 = P.sb("A2", [128, D], F32)
    B2 = P.sb("B2", [128, D], F32)

    def load_g1(j):
        P.dma("sync", g1[:], vecs[1 + 3 * j:2 + 3 * j, :].partition_broadcast(128), writes=[g1])

    def load_ab(j):
        P.dma("sync", A2[:], vecs[2 + 3 * j:3 + 3 * j, :].partition_broadcast(128), writes=[A2])
        P.dma("sync", B2[:], vecs[3 + 3 * j:4 + 3 * j, :].partition_broadcast(128), writes=[B2])
        P.op("vector", lambda e: e.scalar_tensor_tensor(out=A2[:], in0=A2[:], scalar=1.0, in1=nf[:], op0=ALU.add, op1=ALU.mult),
             reads=[A2, nf], writes=[A2])

    xts = [P.sb("xt%d" % i, [128, D], F32) for i in range(2)]
    of_ts = [P.sb("of_t%d" % i, [128, 1024], F32) for i in range(2)]
    ob_ts = [P.sb("ob_t%d" % i, [128, 1024], F32) for i in range(2)]
    sr_ts = [P.sb("sr_t%d" % i, [128, 1024], F32) for i in range(2)]
    sq = P.sb("sq", [128, 1024], F32)
    gsss = [P.sb("gss%d" % i, [128, 4], F32) for i in range(2)]
    glb = P.sb("glb", [128, 1024], BF16)
    catT = [P.sb("catT%d" % i, [128, 16, 128], BF16) for i in range(2)]
    tpb = P.ps("tpb", [128, 8, 128], BF16)
    yps = [P.ps("yps%d" % i, [128, 512], F32) for i in range(2)]
    tpf = [P.ps("tpf%d" % i, [128, 4, 128], F32) for i in range(2)]
    lg_ps = P.ps("lg_ps", [128, NE], F32)
    ss2 = P.sb("ss2", [128, 1], F32)
    junk = P.sb("junk", [128, D], BF16)
    h2 = P.sb("h2", [128, D], F32)
    h2b = [P.sb("h2b%d" % i, [128, D], BF16) for i in range(2)]
    h2T = P.sb("h2T", [128, 16, 128], F32)
    mx = P.sb("mx", [128, 1], F32)
    ssum = P.sb("ssum", [128, 1], F32)
    ex = P.sb("ex", [128, NE], F32)
    affs = [P.sb("affs%d" % i, [128, NE], F32) for i in range(2)]
    tmp = P.sb("tmp", [128, 512], F32)
    itc = [0]

    def part1(t):
        rows = slice(t * 128, (t + 1) * 128)
        xt = xts[t % 2]
        ct = catT[t % 2]
        of_t, ob_t, sr_t, gss = of_ts[t % 2], ob_ts[t % 2], sr_ts[t % 2], gsss[t % 2]
        P.dma("sync", xt[:], x_in[rows, :], writes=[xt])
        P.dma("sync", ct[:, 0:8, :], attnT[:, :, rows], writes=[(ct, 0)])
        P.dma("sync", of_t[:], ogf[rows, :], writes=[of_t])
        P.dma("sync", ob_t[:], ogb[rows, :], writes=[ob_t])
        P.dma("sync", sr_t[:], sr[rows, :], writes=[sr_t])
        P.op("vector", lambda e: e.tensor_tensor(out=of_t[:], in0=of_t[:], in1=ob_t[:], op=ALU.add),
             reads=[of_t, ob_t], writes=[of_t])
        P.op("scalar", lambda e: e.activation(out=sq[:], in_=of_t[:], func=AF.Square),
             reads=[of_t], writes=[sq])
        P.op("vector", lambda e: e.tensor_reduce(out=gss[:], in_=sq[:].rearrange("p (h d) -> p h d", h=4), axis=AX.X, op=ALU.add),
             reads=[sq], writes=[gss])
        emit_rsqrt(P, gss[:], gss[:], 1.0 / 256, gss, gss)
        o3 = of_t[:].rearrange("p (h d) -> p h d", h=4)
        P.op("vector", lambda e: e.tensor_tensor(out=sr_t[:].rearrange("p (h d) -> p h d", h=4), in0=sr_t[:].rearrange("p (h d) -> p h d", h=4),
                                                 in1=gg[:].unsqueeze(1).broadcast_to([128, 4, 256]), op=ALU.mult),
             reads=[sr_t, gg], writes=[sr_t])
        P.op("vector", lambda e, o3=o3: e.tensor_tensor(out=o3, in0=o3, in1=gss[:].unsqueeze(2).broadcast_to([128, 4, 256]), op=ALU.mult),
             reads=[of_t, gss], writes=[of_t])
        P.op("vector", lambda e: e.tensor_tensor(out=glb[:], in0=of_t[:], in1=sr_t[:], op=ALU.mult),
             reads=[of_t, sr_t], writes=[glb])
        for j in range(8):
            P.op("tensor", lambda e, j=j: e.transpose(out=tpb[:, j, :], in_=glb[:, j * 128:(j + 1) * 128], identity=identb[:]),
                 reads=[glb, identb], writes=[tpb])
        P.op("scalar", lambda e, ct=ct: e.activation(out=ct[:, 8:16, :], in_=tpb[:], func=AF.Copy), reads=[tpb], writes=[(ct, 1)])

    def part2a(t):
        rows = slice(t * 128, (t + 1) * 128)
        xt = xts[t % 2]
        ct = catT[t % 2]
        if t == 0:
            load_g1(0)
        if t == NT - 1:
            load_g1(1)
        for cb in range(4):
            yp = yps[itc[0] % 2]
            itc[0] += 1
            cs = slice(cb * 512, (cb + 1) * 512)
            for kc in range(16):
                P.op("tensor", lambda e, yp=yp, ct=ct, kc=kc, cs=cs: e.matmul(
                    yp[:], lhsT=ct[:, kc, :], rhs=wo[:, kc, cs], start=(kc == 0), stop=(kc == 15)),
                    reads=[(ct, kc // 8), (wo, kc)], writes=[yp])
            P.op("vector", lambda e, yp=yp, cs=cs: e.tensor_tensor(out=tmp[:], in0=yp[:], in1=g1[:, cs], op=ALU.mult),
                 reads=[yp, g1], writes=[tmp])
            P.op("vector", lambda e, xt=xt, cs=cs: e.tensor_tensor(out=xt[:, cs], in0=xt[:, cs], in1=tmp[:], op=ALU.add),
                 reads=[tmp, xt], writes=[xt])
        P.dma("sync", x_out[rows, :], xt[:], reads=[xt])

    def part2b(t):
        rows = slice(t * 128, (t + 1) * 128)
        xt = xts[t % 2]
        if t == 0:
            load_ab(0)
        if t == NT - 1:
            load_ab(1)
        P.op("scalar", lambda e, xt=xt: e.activation(out=junk[:], in_=xt[:], func=AF.Square, accum_out=ss2[:]),
             reads=[xt], writes=[junk, ss2])
        emit_rsqrt(P, ss2[:], ss2[:], 1.0 / D, ss2, ss2)
        P.op("vector", lambda e, xt=xt: e.scalar_tensor_tensor(out=h2[:], in0=xt[:], scalar=ss2[:, 0:1], in1=A2[:], op0=ALU.mult, op1=ALU.mult),
             reads=[xt, ss2, A2], writes=[h2])
        P.op("vector", lambda e: e.tensor_tensor(out=h2[:], in0=h2[:], in1=B2[:], op=ALU.add), reads=[h2, B2], writes=[h2])
        hb = h2b[t % 2]
        P.op("scalar", lambda e, hb=hb: e.activation(out=hb[:], in_=h2[:], func=AF.Copy), reads=[h2], writes=[hb])
        P.dma("sync", h2_out[rows, :], hb[:], reads=[hb])
        for q in range(4):
            tp = tpf[q % 2]
            for j in range(4):
                kc = q * 4 + j
                P.op("tensor", lambda e, tp=tp, j=j, kc=kc: e.transpose(out=tp[:, j, :], in_=h2[:, kc * 128:(kc + 1) * 128], identity=identf[:]),
                     reads=[h2, identf], writes=[tp])
            P.op("scalar", lambda e, tp=tp, q=q: e.activation(out=h2T[:, q * 4:(q + 1) * 4, :], in_=tp[:], func=AF.Copy),
                 reads=[tp], writes=[(h2T, q)])
        for kc in range(16):
            P.op("tensor", lambda e, kc=kc: e.matmul(lg_ps[:], lhsT=h2T[:, kc, :], rhs=wr[:, kc, :], start=(kc == 0), stop=(kc == 15)),
                 reads=[(h2T, kc // 4), wr], writes=[lg_ps])
        P.op("vector", lambda e: e.tensor_reduce(out=mx[:], in_=lg_ps[:], axis=AX.X, op=ALU.max), reads=[lg_ps], writes=[mx])
        P.op("vector", lambda e: e.tensor_scalar(out=mx[:], in0=mx[:], scalar1=-1.0, scalar2=None, op0=ALU.mult), reads=[mx], writes=[mx])
        P.op("scalar", lambda e: e.activation(out=ex[:], in_=lg_ps[:], func=AF.Exp, bias=mx[:, 0:1], accum_out=ssum[:]),
             reads=[lg_ps, mx], writes=[ex, ssum])
        P.op("vector", lambda e: e.reciprocal(out=ssum[:], in_=ssum[:]), reads=[ssum], writes=[ssum])
        af = affs[t % 2]
        P.op("vector", lambda e, af=af: e.tensor_scalar(out=af[:], in0=ex[:], scalar1=ssum[:, 0:1], scalar2=None, op0=ALU.mult),
             reads=[ex, ssum], writes=[af])
        P.dma("sync", aff_out[rows, :], af[:], reads=[af])

    part1(0)
    part2a(0)
    for t in range(NT):
        if t + 1 < NT:
            part1(t + 1)
            part2a(t + 1)
        part2b(t)
    P.finish()
    return nc


def run_kc(inputs, mods, l, attnT, og, sr_lat, sr_ctx, x_lat, x_ctx, nc=None):
    nc = nc or build_kc()
    m_lat, m_ctx = mods[l, 0], mods[l, 1]
    vecs = np.stack([inputs["norm_ffn"][l], m_lat[2 * D:3 * D], m_lat[4 * D:5 * D], m_lat[3 * D:4 * D],
                     m_ctx[2 * D:3 * D], m_ctx[4 * D:5 * D], m_ctx[3 * D:4 * D]]).astype(np.float32)
    ggain = np.ascontiguousarray(inputs["gla_gain"][l][None]).astype(np.float32)
    aT = np.stack(attnT, axis=1)
    ogf = np.concatenate(og[0:4], axis=1)
    ogb = np.concatenate(og[4:8], axis=1)
    w_out = np.ascontiguousarray(inputs["w_out"][l])
    w_r = np.ascontiguousarray(inputs["w_router"][l])
    maps = []
    for i in range(NCORES):
        tok = np.concatenate([CTX + np.arange(i * 1024, (i + 1) * 1024), (i % 2) * 128 + np.arange(128)])
        maps.append({"attnT": np.ascontiguousarray(aT[:, :, tok]), "ogf": ogf[tok], "ogb": ogb[tok],
                     "sr": core_tokens(sr_lat, sr_ctx, i), "x_in": core_tokens(x_lat, x_ctx, i),
                     "w_out": w_out, "w_r": w_r, "vecs": vecs, "ggain": ggain})
    res = run_bass_kernel_spmd(nc, maps, core_ids=list(range(NCORES)))
    return [(r["x_out"], r["h2_out"], r["aff_out"]) for r in res.results]


CAP = 2 * SEQ // NE
CCAP = 2 * CTX // NE
NSLOT = CAP + CCAP
ROWW = D
GW = 16
NBIS = 30
OOB0 = 4096


def build_kd():
    nc = bass.Bass("TRN2", target_bir_lowering=False)
    aff_l = nc.dram_tensor("aff_l", [128, 2, 64], F32, kind="ExternalInput").ap()
    aff_c = nc.dram_tensor("aff_c", [128, 2, 2], F32, kind="ExternalInput").ap()
    h2l = nc.dram_tensor("h2l", [SEQ, D], BF16, kind="ExternalInput").ap()
    h2c = nc.dram_tensor("h2c", [CTX, D], BF16, kind="ExternalInput").ap()
    wg = nc.dram_tensor("wg", [2, D, FF], F32, kind="ExternalInput").ap()
    wu = nc.dram_tensor("wu", [2, D, FF], F32, kind="ExternalInput").ap()
    wd = nc.dram_tensor("wd", [2, FF, D], F32, kind="ExternalInput").ap()
    xg = [nc.dram_tensor("xg%d" % i, [NSLOT, ROWW], BF16, kind="Internal").ap() for i in range(2)]
    out_e = nc.dram_tensor("out_e", [2, NSLOT, D], F32, kind="ExternalOutput").ap()
    pos_l = nc.dram_tensor("pos_l", [128, 2, 64], I32, kind="ExternalOutput").ap()
    pos_c = nc.dram_tensor("pos_c", [128, 2, 2], I32, kind="ExternalOutput").ap()
    P = Prog(nc)
    identf = make_ident(P, "identf", F32)
    identb = P.sb("identb", [128, 128], BF16)
    P.op("vector", lambda e: e.tensor_copy(out=identb[:], in_=identf[:]), reads=[identf], writes=[identb])
    ones = P.sb("ones_f", [128, 128], F32)
    P.op("gpsimd", lambda e: e.memset(ones[:], 1.0), writes=[ones])
    SU = P.sb("SU", [128, 128], F32)
    P.op("gpsimd", lambda e: e.memset(SU[:], 1.0), writes=[SU])
    P.op("gpsimd", lambda e: e.affine_select(out=SU[:], in_=SU[:], pattern=[[1, 128]], compare_op=ALU.is_gt, fill=0.0,
                                             base=0, channel_multiplier=-1), reads=[SU], writes=[SU])
    oobv = P.sb("oobv", [128, 1], F32)
    P.op("gpsimd", lambda e: e.iota(oobv[:], pattern=[[0, 1]], base=OOB0, channel_multiplier=1,
                                    allow_small_or_imprecise_dtypes=True), writes=[oobv])
    zeros = P.sb("zeros", [128, 64], F32)
    P.op("gpsimd", lambda e: e.memset(zeros[:], 0.0), writes=[zeros])
    affl = P.sb("affl", [128, 2, 64], F32)
    affc = P.sb("affc", [128, 2, 2], F32)
    P.dma("sync", affl[:], aff_l, writes=[affl])
    P.dma("sync", affc[:], aff_c, writes=[affc])
    pairs = [(affl, 0, 64, 0, CAP), (affl, 1, 64, 0, CAP), (affc, 0, 2, CAP, CCAP), (affc, 1, 2, CAP, CCAP)]
    kvec = P.sb("kvec", [128, 4], F32)
    P.op("gpsimd", lambda e: e.memset(kvec[:, 0:2], CAP - 0.5), writes=[kvec])
    P.op("gpsimd", lambda e: e.memset(kvec[:, 2:4], CCAP - 0.5), reads=[kvec], writes=[kvec])
    lo = P.sb("lo", [128, 4], F32)
    P.op("gpsimd", lambda e: e.memset(lo[:], 0.0), writes=[lo])
    mid = P.sb("mid", [128, 4], F32)
    pc = P.sb("pc", [128, 4], F32)
    ge = P.sb("ge", [128, 4], F32)
    junk = P.sb("junkb", [128, 64], F32)
    sm_ps = P.ps("sm_ps", [128, 512], F32)
    cnt_ps = PV(sm_ps, 0, 4)
    for k in range(NBIS):
        step = 2.0 ** -(k + 1)
        P.op("vector", lambda e, step=step: e.tensor_scalar(out=mid[:], in0=lo[:], scalar1=step, scalar2=None, op0=ALU.add),
             reads=[lo], writes=[mid])
        for j, (a, ee, n, base, cap) in enumerate(pairs):
            P.op("vector", lambda e, a=a, ee=ee, n=n, j=j: e.tensor_scalar(
                out=junk[:, :n], in0=a[:, ee, :], scalar1=mid[:, j:j + 1], scalar2=0.0, op0=ALU.is_ge, op1=ALU.add,
                accum_out=pc[:, j:j + 1]), reads=[a, mid], writes=[junk, (pc, j)])
        P.op("tensor", lambda e: e.matmul(cnt_ps[:], lhsT=ones[:], rhs=pc[:], start=True, stop=True),
             reads=[ones] + [(pc, j) for j in range(4)], writes=[cnt_ps])
        P.op("vector", lambda e: e.tensor_tensor(out=ge[:], in0=cnt_ps[:], in1=kvec[:], op=ALU.is_ge),
             reads=[cnt_ps, kvec], writes=[ge])
        P.op("vector", lambda e, step=step: e.scalar_tensor_tensor(out=lo[:], in0=ge[:], scalar=step, in1=lo[:],
                                                                    op0=ALU.mult, op1=ALU.add),
             reads=[ge, lo], writes=[lo])
    M = P.sb("Msel", [128, 64], F32)
    Tsb = P.sb("Tsb", [128, 64], F32)
    cum = P.sb("cum", [128, 64], F32)
    pos = P.sb("posf", [128, 64], F32)
    sel = P.sb("sel", [128, 64], F32)
    posl = P.sb("posl", [128, 2, 64], I32)
    posc = P.sb("posc", [128, 2, 2], I32)
    wi_ps, t_ps = PV(sm_ps, 64, 64), PV(sm_ps, 128, 64)
    for j, (a, ee, n, base, cap) in enumerate(pairs):
        dst = (posl if n == 64 else posc)
        P.op("vector", lambda e, a=a, ee=ee, n=n, j=j: e.tensor_scalar(
            out=M[:, :n], in0=a[:, ee, :], scalar1=lo[:, j:j + 1], scalar2=None, op0=ALU.is_ge), reads=[a, lo], writes=[M])
        P.op("tensor", lambda e, n=n: e.matmul(wi_ps[:, :n], lhsT=SU[:], rhs=M[:, :n], start=True, stop=True),
             reads=[SU, M], writes=[sm_ps])
        P.op("tensor", lambda e, n=n: e.matmul(t_ps[:, :n], lhsT=ones[:], rhs=M[:, :n], start=True, stop=True),
             reads=[ones, M], writes=[sm_ps])
        P.op("vector", lambda e, n=n: e.tensor_copy(out=Tsb[:, :n], in_=t_ps[:, :n]), reads=[sm_ps], writes=[Tsb])
        P.op("vector", lambda e, n=n: e.tensor_tensor_scan(out=cum[:, :n], data0=Tsb[:, :n], data1=zeros[:, :n], initial=0.0,
                                                            op0=ALU.add, op1=ALU.add), reads=[Tsb, zeros], writes=[cum])
        P.op("vector", lambda e, n=n: e.tensor_tensor(out=pos[:, :n], in0=wi_ps[:, :n], in1=cum[:, :n], op=ALU.add),
             reads=[sm_ps, cum], writes=[pos])
        P.op("vector", lambda e, n=n, base=base: e.scalar_tensor_tensor(out=pos[:, :n], in0=pos[:, :n], scalar=float(base), in1=Tsb[:, :n],
                                                                         op0=ALU.add, op1=ALU.subtract), reads=[pos, Tsb], writes=[pos])
        P.op("vector", lambda e, n=n, base=base, cap=cap: e.scalar_tensor_tensor(
            out=sel[:, :n], in0=pos[:, :n], scalar=float(base + cap) - 0.5, in1=M[:, :n], op0=ALU.is_lt, op1=ALU.mult),
            reads=[pos, M], writes=[sel])
        P.op("vector", lambda e, n=n: e.tensor_scalar(out=pos[:, :n], in0=pos[:, :n], scalar1=oobv[:, 0:1], scalar2=None, op0=ALU.subtract),
             reads=[pos, oobv], writes=[pos])
        P.op("vector", lambda e, n=n: e.tensor_tensor(out=pos[:, :n], in0=pos[:, :n], in1=sel[:, :n], op=ALU.mult),
             reads=[pos, sel], writes=[pos])
        P.op("vector", lambda e, n=n: e.tensor_scalar(out=pos[:, :n], in0=pos[:, :n], scalar1=oobv[:, 0:1], scalar2=None, op0=ALU.add),
             reads=[pos, oobv], writes=[pos])
        P.op("vector", lambda e, n=n, dst=dst, ee=ee: e.tensor_copy(out=dst[:, ee, :], in_=pos[:, :n]), reads=[pos], writes=[dst])
    P.dma("sync", pos_l, posl[:], reads=[posl])
    P.dma("sync", pos_c, posc[:], reads=[posc])
    rbs = [P.sb("rowbuf%d" % i, [128, ROWW], BF16) for i in range(4)]
    rbn = [0]

    def emit_scatter(n, ee):
        rb = rbs[rbn[0] % 4]
        rbn[0] += 1
        if n < 64:
            src, pp, nn = h2l[n * 128:(n + 1) * 128, :], posl, n
        else:
            src, pp, nn = h2c[(n - 64) * 128:(n - 63) * 128, :], posc, n - 64
        P.dma("sync", rb[:, :D], src, writes=[rb])
        P.dma_fn("gpsimd", lambda e: e.indirect_dma_start(
            out=xg[ee], out_offset=bass.IndirectOffsetOnAxis(ap=pp[:, ee, nn:nn + 1], axis=0),
            in_=rb[:, :], in_offset=None, bounds_check=P.reg(e, NSLOT - 1), oob_is_err=False),
            reads=[rb, pp], writes=[("xg", ee, n)])

    for n in range(66):
        emit_scatter(n, 0)
    pending = list(range(66))
    XgT = P.sb("XgT", [128, 16, NSLOT], BF16)
    hidT = P.sb("hidT", [128, 12, NSLOT], BF16)
    xts = [P.sb("xgt%d" % i, [128, ROWW], BF16) for i in range(2)]
    tpb = P.ps("tpb", [128, 8, 128], BF16)
    g_ps = [P.ps("g_ps%d" % i, [128, 512], F32) for i in range(2)]
    u_ps = [P.ps("u_ps%d" % i, [128, 512], F32) for i in range(2)]
    o_ps = [P.ps("o_ps%d" % i, [128, 512], F32) for i in range(2)]
    wgc = [P.sb("wgc%d" % i, [128, 16, 256], BF16) for i in range(2)]
    wuc = [P.sb("wuc%d" % i, [128, 16, 256], BF16) for i in range(2)]
    wdc = [P.sb("wdc%d" % i, [128, 12, 512], BF16) for i in range(2)]
    sgs = [P.sb("sgs%d" % i, [128, 512], F32) for i in range(2)]
    ost = [P.sb("ost%d" % i, [128, 512], F32) for i in range(2)]
    sblocks = [(0, 512), (512, 512), (1024, 32)]
    ih = io = iw = 0
    for ee in range(2):
        scat = [("xg", ee, n) for n in range(66)]
        for st in range(9):
            rows = 128 if st < 8 else CCAP
            xt = xts[st % 2]
            P.dma("sync", xt[:rows, :], xg[ee][st * 128:st * 128 + rows, :], reads=scat, writes=[xt])
            scat = []
            for half in range(2):
                for j in range(8):
                    kc = half * 8 + j
                    P.op("tensor", lambda e, xt=xt, rows=rows, j=j, kc=kc: e.transpose(
                        out=tpb[:, j, :rows], in_=xt[:rows, kc * 128:(kc + 1) * 128], identity=identb[:rows, :rows]),
                        reads=[xt, identb], writes=[tpb])
                eng = "scalar" if half == 0 else "vector"
                if eng == "scalar":
                    P.op("scalar", lambda e, rows=rows, half=half, st=st: e.activation(
                        out=XgT[:, half * 8:(half + 1) * 8, st * 128:st * 128 + rows], in_=tpb[:, :, :rows], func=AF.Copy),
                        reads=[tpb], writes=[(XgT, st)])
                else:
                    P.op("vector", lambda e, rows=rows, half=half, st=st: e.tensor_copy(
                        out=XgT[:, half * 8:(half + 1) * 8, st * 128:st * 128 + rows], in_=tpb[:, :, :rows]),
                        reads=[tpb], writes=[(XgT, st)])
        xkeys = [(XgT, st) for st in range(9)]
        for hc in range(6):
            wg_c, wu_c = wgc[iw % 2], wuc[iw % 2]
            iw += 1
            hs = slice(hc * 256, (hc + 1) * 256)
            for q in range(4):
                P.dma("gpsimd", wg_c[:, 4 * q:4 * q + 4, :], wg[ee].rearrange("(kc p) f -> p kc f", p=128)[:, 4 * q:4 * q + 4, hs],
                      writes=[(wg_c, q)])
                P.dma("gpsimd", wu_c[:, 4 * q:4 * q + 4, :], wu[ee].rearrange("(kc p) f -> p kc f", p=128)[:, 4 * q:4 * q + 4, hs],
                      writes=[(wu_c, q)])
            if ee == 0:
                for _ in range(11):
                    emit_scatter(pending.pop(0), 1)
            for fl in range(2):
                fc = hc * 2 + fl
                fs = slice(fl * 128, (fl + 1) * 128)
                for (s0, n) in sblocks:
                    gp, up, sg = g_ps[ih % 2], u_ps[ih % 2], sgs[ih % 2]
                    ih += 1
                    for kc in range(16):
                        P.op("tensor", lambda e, gp=gp, wg_c=wg_c, kc=kc, fs=fs, s0=s0, n=n: e.matmul(
                            gp[:, :n], lhsT=wg_c[:, kc, fs], rhs=XgT[:, kc, s0:s0 + n], start=(kc == 0), stop=(kc == 15)),
                            reads=[(wg_c, kc // 4)] + xkeys, writes=[gp])
                    for kc in range(16):
                        P.op("tensor", lambda e, up=up, wu_c=wu_c, kc=kc, fs=fs, s0=s0, n=n: e.matmul(
                            up[:, :n], lhsT=wu_c[:, kc, fs], rhs=XgT[:, kc, s0:s0 + n], start=(kc == 0), stop=(kc == 15)),
                            reads=[(wu_c, kc // 4)] + xkeys, writes=[up])
                    P.op("scalar", lambda e, gp=gp, sg=sg, n=n: e.activation(out=sg[:, :n], in_=gp[:, :n], func=AF.Silu),
                         reads=[gp], writes=[sg])
                    P.op("vector", lambda e, up=up, sg=sg, n=n, fc=fc, s0=s0: e.tensor_tensor(
                        out=hidT[:, fc, s0:s0 + n], in0=up[:, :n], in1=sg[:, :n], op=ALU.mult),
                        reads=[up, sg], writes=[(hidT, fc)])
        hkeys = [(hidT, fc) for fc in range(12)]
        for cb in range(4):
            wd_c = wdc[(ee * 4 + cb) % 2]
            cs = slice(cb * 512, (cb + 1) * 512)
            for q in range(3):
                P.dma("gpsimd", wd_c[:, 4 * q:4 * q + 4, :], wd[ee].rearrange("(fc p) n -> p fc n", p=128)[:, 4 * q:4 * q + 4, cs],
                      writes=[(wd_c, q)])
            for st in range(9):
                rows = 128 if st < 8 else CCAP
                op_, os_ = o_ps[io % 2], ost[io % 2]
                io += 1
                for fc in range(12):
                    P.op("tensor", lambda e, op_=op_, wd_c=wd_c, fc=fc, st=st, rows=rows: e.matmul(
                        op_[:rows, :], lhsT=hidT[:, fc, st * 128:st * 128 + rows], rhs=wd_c[:, fc, :],
                        start=(fc == 0), stop=(fc == 11)), reads=[(wd_c, fc // 4)] + hkeys, writes=[op_])
                P.op("scalar", lambda e, op_=op_, os_=os_, rows=rows, ee=ee, st=st: e.activation(
                    out=os_[:rows, :], in_=op_[:rows, :], func=AF.Copy),
                    reads=[op_], writes=[os_])
                P.dma("sync", out_e[ee, st * 128:st * 128 + rows, cs], os_[:rows, :], reads=[os_])
    P.finish()
    return nc


def run_kd(inputs, l, aff_lat, aff_ctx, h2_lat, h2_ctx, nc=None):
    nc = nc or build_kd()
    maps = []
    for i in range(NCORES):
        es = slice(2 * i, 2 * i + 2)
        maps.append({
            "aff_l": np.ascontiguousarray(aff_lat[:, es].reshape(64, 128, 2).transpose(1, 2, 0)),
            "aff_c": np.ascontiguousarray(aff_ctx[:, es].reshape(2, 128, 2).transpose(1, 2, 0)),
            "h2l": h2_lat, "h2c": h2_ctx,
            "wg": np.ascontiguousarray(inputs["w_gate"][l, es]), "wu": np.ascontiguousarray(inputs["w_up"][l, es]),
            "wd": np.ascontiguousarray(inputs["w_down"][l, es]),
        })
    res = run_bass_kernel_spmd(nc, maps, core_ids=list(range(NCORES)))
    return [(r["out_e"], r["pos_l"], r["pos_c"]) for r in res.results]


def build_kf():
    nc = bass.Bass("TRN2", target_bir_lowering=False)
    x_in = nc.dram_tensor("x_in", [NT * 128, D], F32, kind="ExternalInput").ap()
    fnv = nc.dram_tensor("fnv", [1, D], F32, kind="ExternalInput").ap()
    y = nc.dram_tensor("y", [(NT - 1) * 128, D], F32, kind="ExternalOutput").ap()
    P = Prog(nc)
    comb = Combiner(P, *declare_combine_inputs(nc))
    fn_s = P.sb("fn_s", [128, D], F32)
    P.dma("sync", fn_s[:], fnv.partition_broadcast(128), writes=[fn_s])
    xts = [P.sb("xt%d" % i, [128, D], F32) for i in range(2)]
    junk = P.sb("junk", [128, D], BF16)
    ss = P.sb("ss", [128, 1], F32)
    for t in range(NT - 1):
        xt = xts[t % 2]
        rows = slice(t * 128, (t + 1) * 128)
        P.dma("sync", xt[:], x_in[rows, :], writes=[xt])
        comb.emit(t, xt)
        P.op("scalar", lambda e, xt=xt: e.activation(out=junk[:], in_=xt[:], func=AF.Square, accum_out=ss[:]),
             reads=[xt], writes=[junk, ss])
        emit_rsqrt(P, ss[:], ss[:], 1.0 / D, ss, ss)
        P.op("vector", lambda e, xt=xt: e.scalar_tensor_tensor(out=xt[:], in0=xt[:], scalar=ss[:, 0:1], in1=fn_s[:],
                                                                op0=ALU.mult, op1=ALU.mult), reads=[xt, ss, fn_s], writes=[xt])
        P.dma("sync", y[rows, :], xt[:], reads=[xt])
    P.finish()
    return nc


def run_kf(inputs, x_lat, x_ctx, comb_maps, nc=None):
    nc = nc or build_kf()
    fnv = np.ascontiguousarray(inputs["final_norm"][None]).astype(np.float32)
    maps = []
    for i in range(NCORES):
        m = {"x_in": core_tokens(x_lat, x_ctx, i), "fnv": fnv}
        m.update(comb_maps[i])
        maps.append(m)
    res = run_bass_kernel_spmd(nc, maps, core_ids=list(range(NCORES)))
    return np.concatenate([r["y"] for r in res.results], axis=0)


_NC = {}


def _nc(name, fn):
    if name not in _NC:
        _NC[name] = fn()
    return _NC[name]


def kernel(**inputs):
    inputs = {k: np.asarray(v) for k, v in inputs.items()}
    mods = run_kmod(inputs)
    x_lat = np.ascontiguousarray(inputs["x"][0])
    x_ctx = np.ascontiguousarray(inputs["ctx"][0])
    comb = None
    for l in range(DEPTH):
        if comb is None:
            ka = run_ka(inputs, mods, l, x_lat, x_ctx, nc=_nc("ka0", lambda: build_ka(False)))
        else:
            ka = run_ka(inputs, mods, l, x_lat, x_ctx, nc=_nc("ka1", lambda: build_ka(True)), comb_maps=comb)
            x_lat, x_ctx = gather_tokens([o[2] for o in ka])
        bf_lat, bf_ctx = gather_tokens([o[0] for o in ka])
        f_lat, f_ctx = gather_tokens([o[1] for o in ka])
        attnT, og = run_kb(bf_lat, bf_ctx, f_lat, f_ctx, nc=_nc("kb", build_kb))
        kc = run_kc(inputs, mods, l, attnT, og, f_lat[:, 1024:2048], f_ctx[:, 1024:2048], x_lat, x_ctx,
                    nc=_nc("kc", build_kc))
        x_lat, x_ctx = gather_tokens([o[0] for o in kc])
        h_lat, h_ctx = gather_tokens([o[1] for o in kc])
        a_lat, a_ctx = gather_tokens([o[2] for o in kc])
        kd = run_kd(inputs, l, a_lat, a_ctx, h_lat, h_ctx, nc=_nc("kd", build_kd))
        comb = combine_maps(mods, l, kd, a_lat, a_ctx)
    y = run_kf(inputs, x_lat, x_ctx, comb, nc=_nc("kf", build_kf))
    return np.ascontiguousarray(y[None]).astype(np.float32)
```

```python
import numpy as np
from contextlib import ExitStack
import concourse.bass as bass
import concourse.mybir as mybir
from concourse.bass_utils import run_bass_kernel_spmd

F32 = mybir.dt.float32
BF16 = mybir.dt.bfloat16
I32 = mybir.dt.int32
U32 = mybir.dt.uint32
AF = mybir.ActivationFunctionType
ALU = mybir.AluOpType
AX = mybir.AxisListType

ENGINES = ["sync", "scalar", "vector", "gpsimd", "tensor"]
SEM_EPOCH = 20000
NDSLOT = 6


class Prog:
    def __init__(self, nc, same_engine_sync=True):
        self.nc = nc
        self.stack = ExitStack()
        self.items = {e: [] for e in ENGINES}
        self.cur = {}
        self.sems = {}
        self.known = {e: {} for e in ENGINES}
        self.lastw = {}
        self.readers = {}
        self.same_engine_sync = same_engine_sync
        self.final = {}
        self.nsem = 0
        self.drr = {}

    def const_eps(self):
        if not hasattr(self, "_epsb"):
            self._epsb = self.sb("epsb", [128, 1], F32)
            self.op("gpsimd", lambda e: e.memset(self._epsb[:], EPS), writes=[self._epsb])
        return self._epsb

    def reg(self, e, val):
        if not hasattr(self, "_regs"):
            self._regs = {}
        if val not in self._regs:
            self._regs[val] = e.to_reg(val)
        return self._regs[val]

    def sb(self, name, shape, dt):
        return self.stack.enter_context(self.nc.sbuf_tensor(name, shape, dt))

    def ps(self, name, shape, dt):
        return self.stack.enter_context(self.nc.psum_tensor(name, shape, dt))

    def _sem(self, key):
        if key not in self.sems:
            self.nsem += 1
            self.sems[key] = self.stack.enter_context(
                self.nc.semaphore("s_%s_%s_%d" % key))
        return self.sems[key]

    def _bump(self, kind, eng, amount):
        st = self.cur.setdefault((kind, eng), [0, 0])
        if st[1] + amount > SEM_EPOCH:
            st[0] += 1
            st[1] = 0
        st[1] += amount
        key = (kind, eng, st[0])
        self._sem(key)
        self.final[key] = st[1]
        return key, st[1]

    @staticmethod
    def _k(x):
        if isinstance(x, (str, int)):
            return x
        if isinstance(x, tuple):
            return tuple(Prog._k(y) for y in x)
        return x.name

    def _deps(self, eng, reads, writes):
        reads = [self._k(r) for r in reads]
        writes = [self._k(w) for w in writes]
        need = {}

        def add(dep):
            k, c = dep
            if need.get(k, 0) < c:
                need[k] = c
        for r in reads:
            if r in self.lastw:
                add(self.lastw[r])
        for w in writes:
            if w in self.lastw:
                add(self.lastw[w])
            for d in self.readers.get(w, ()):
                add(d)
        waits = []
        for k, c in need.items():
            kind, e2, _ = k
            if kind == "c" and e2 == eng:
                if eng == "tensor" or not self.same_engine_sync:
                    continue
            if self.known[eng].get(k, 0) >= c:
                continue
            self.known[eng][k] = c
            waits.append((k, c))
        return waits

    def _record(self, reads, writes, dep):
        reads = [self._k(r) for r in reads]
        writes = [self._k(w) for w in writes]
        for r in reads:
            self.readers.setdefault(r, []).append(dep)
        for w in writes:
            self.lastw[w] = dep
            self.readers[w] = []

    def op(self, eng, fn, reads=(), writes=()):
        waits = self._deps(eng, reads, writes)
        key, c = self._bump("c", eng, 1)
        self.items[eng].append((waits, fn, key, 1))
        self._record(reads, writes, (key, c))

    def _dma_slot(self, eng, waits):
        rr = self.drr.get(eng, 0)
        self.drr[eng] = (rr + 1) % NDSLOT
        slot = "%s%d" % (eng, rr)
        st = self.cur.setdefault(("d", slot), [0, 0])
        if st[1] > 0:
            pk = ("d", slot, st[0])
            if self.known[eng].get(pk, 0) < st[1]:
                self.known[eng][pk] = st[1]
                waits.append((pk, st[1]))
        return self._bump("d", slot, 16)

    def dma(self, eng, out, in_, reads=(), writes=(), out_final=False, **kw):
        return self.dma_fn(eng, lambda e: e.dma_start(out=out, in_=in_, **kw), reads, writes, out_final)

    def dma_fn(self, eng, fn, reads=(), writes=(), out_final=False):
        waits = self._deps(eng, reads, writes)
        key, c = self._dma_slot(eng, waits)
        self.items[eng].append((waits, fn, key, 16))
        self._record(reads, writes, (key, c))

    def finish(self):
        nc = self.nc
        fin = dict(self.final)
        items = self.items
        sems = self.sems

        def emit(eng_name, e, extra=None):
            for waits, fn, key, inc in items[eng_name]:
                for k, c in waits:
                    e.wait_ge(sems[k], c)
                ins = fn(e)
                ins.then_inc(sems[key], inc)
            if extra:
                for k, c in extra.items():
                    e.wait_ge(sems[k], c)

        with nc.Block() as block:
            @block.sync
            def _(e):
                emit("sync", e, fin)

            @block.scalar
            def _(e):
                emit("scalar", e)

            @block.vector
            def _(e):
                emit("vector", e)

            @block.gpsimd
            def _(e):
                emit("gpsimd", e)

            @block.tensor
            def _(e):
                emit("tensor", e)
        self.stack.close()


def kernel(**inputs):
    raise NotImplementedError


D = 2048
SEQ = 8192
CTX = 256
DEPTH = 4
NCORES = 8
INW = 4640
EPS = 1e-6
NT = 9
NE = 16
FF = 1536


def make_ident(P, name, dt):
    idf = P.sb(name + "_f", [128, 128], F32)
    P.op("gpsimd", lambda e: e.memset(idf[:], 1.0), writes=[idf])
    P.op("gpsimd", lambda e: e.affine_select(out=idf[:], in_=idf[:], pattern=[[-1, 128]],
                                             compare_op=ALU.is_equal, fill=0.0, base=0,
                                             channel_multiplier=1), reads=[idf], writes=[idf])
    if dt == F32:
        return idf
    idn = P.sb(name, [128, 128], dt)
    P.op("vector", lambda e: e.tensor_copy(out=idn[:], in_=idf[:]), reads=[idf], writes=[idn])
    return idn


def build_kmod():
    nc = bass.Bass("TRN2", target_bir_lowering=False)
    CW = 6 * D // NCORES
    cc = nc.dram_tensor("cc", [128, 16, 2], F32, kind="ExternalInput").ap()
    wm = nc.dram_tensor("wm", [DEPTH, D, CW], F32, kind="ExternalInput").ap()
    bm = nc.dram_tensor("bm", [DEPTH, 1, CW], F32, kind="ExternalInput").ap()
    out = nc.dram_tensor("mods", [DEPTH, 2, CW], F32, kind="ExternalOutput").ap()
    P = Prog(nc)
    cs = P.sb("cs", [128, 16, 2], F32)
    sc = P.sb("scs", [128, 16, 2], F32)
    P.dma("sync", cs[:], cc, writes=[cs])
    P.op("scalar", lambda e: e.activation(out=sc[:], in_=cs[:], func=AF.Silu), reads=[cs], writes=[sc])
    wts = [P.sb("w%d" % i, [128, 16, 512], F32) for i in range(2)]
    pss = [P.ps("ps%d" % i, [2, 512], F32) for i in range(2)]
    bias = P.sb("bias", [2, DEPTH, CW], F32)
    for r in range(2):
        P.dma("sync", bias[r:r + 1, :, :], bm.rearrange("l o n -> o l n"), writes=[(bias, r)])
    res = P.sb("res", [2, DEPTH, CW], F32)
    it = 0
    for l in range(DEPTH):
        for cb in range(CW // 512):
            w = wts[it % 2]
            ps = pss[it % 2]
            P.dma("sync" if it % 2 == 0 else "scalar", w[:],
                  wm[l].rearrange("(kc p) n -> p kc n", p=128)[:, :, cb * 512:(cb + 1) * 512],
                  writes=[w])
            for kc in range(16):
                P.op("tensor", lambda e, w=w, ps=ps, kc=kc: e.matmul(
                    ps[:], lhsT=sc[:, kc, :], rhs=w[:, kc, :], start=(kc == 0), stop=(kc == 15)),
                    reads=[sc, w], writes=[ps])
            P.op("vector", lambda e, ps=ps, l=l, cb=cb: e.tensor_tensor(
                out=res[:, l, cb * 512:(cb + 1) * 512], in0=ps[:], in1=bias[:, l, cb * 512:(cb + 1) * 512],
                op=ALU.add), reads=[ps, (bias, 0), (bias, 1)], writes=[(res, it)])
            it += 1
    P.dma("sync", out.rearrange("l r n -> r l n"), res[:], reads=[(res, i) for i in range(it)],
          out_final=True)
    P.finish()
    return nc


def run_kmod(inputs):
    c2 = np.stack([inputs["c"][0], inputs["c_ctx"]], axis=-1)
    cc = np.ascontiguousarray(c2.reshape(16, 128, 2).transpose(1, 0, 2))
    CW = 6 * D // NCORES
    nc = build_kmod()
    maps = []
    for i in range(NCORES):
        maps.append({
            "cc": cc,
            "wm": np.ascontiguousarray(inputs["w_mod"][:, :, i * CW:(i + 1) * CW]),
            "bm": np.ascontiguousarray(inputs["b_mod"][:, None, i * CW:(i + 1) * CW]),
        })
    res = run_bass_kernel_spmd(nc, maps, core_ids=list(range(NCORES)))
    mods = np.concatenate([r["mods"] for r in res.results], axis=-1)
    return mods


OBF_W = 2560
OF_W = 3072


def emit_rsqrt(P, out, in_, scale, rkey, wkey):
    epsb = P.const_eps()
    n = in_.shape[0]
    P.op("scalar", lambda e: e.activation(out=out, in_=in_, func=AF.Ln, scale=scale, bias=epsb[:n, :]),
         reads=[rkey, epsb], writes=[wkey])
    P.op("scalar", lambda e: e.activation(out=out, in_=out, func=AF.Exp, scale=-0.5),
         reads=[wkey], writes=[wkey])


def emit_modvec(P, vec, name):
    A = P.sb(name, [128, 2, 16], F32)
    for j in range(2):
        P.op("vector", lambda e, j=j: e.scalar_tensor_tensor(
            out=A[:, j, :], in0=vec[:, 1 + 2 * j, :], scalar=1.0, in1=vec[:, 0, :],
            op0=ALU.add, op1=ALU.mult), reads=[vec], writes=[A])
    return A


def declare_combine_inputs(nc):
    oes = [nc.dram_tensor("oe%d" % e, [NSLOT, D], F32, kind="ExternalInput").ap() for e in range(NE)]
    cpos = nc.dram_tensor("cpos", [128, NT, NE], I32, kind="ExternalInput").ap()
    caff = nc.dram_tensor("caff", [128, NT, NE], F32, kind="ExternalInput").ap()
    g2v = nc.dram_tensor("g2v", [2, D], F32, kind="ExternalInput").ap()
    return oes, cpos, caff, g2v


class Combiner:
    def __init__(self, P, oes, cpos, caff, g2v):
        self.P, self.oes, self.g2v = P, oes, g2v
        self.pos = P.sb("cpos_s", [128, NT, NE], I32)
        P.dma("sync", self.pos[:], cpos, writes=[self.pos])
        self.aff = P.sb("caff_s", [128, NT, NE], F32)
        P.dma("sync", self.aff[:], caff, writes=[self.aff])
        selm = P.sb("cselm", [128, NT, NE], F32)
        P.op("vector", lambda e: e.tensor_scalar(out=selm[:], in0=self.pos[:], scalar1=float(NSLOT) - 0.5, scalar2=None,
                                                 op0=ALU.is_lt), reads=[self.pos], writes=[selm])
        P.op("vector", lambda e: e.tensor_tensor(out=self.aff[:], in0=self.aff[:], in1=selm[:], op=ALU.mult),
             reads=[self.aff, selm], writes=[self.aff])
        self.g2 = P.sb("g2_s", [128, D], F32)
        self.acc = P.sb("cacc", [128, D], F32)
        self.gbl = [P.sb("cgb%d" % i, [128, D], F32) for i in range(4)]
        for gb in self.gbl:
            P.op("gpsimd", lambda e, gb=gb: e.memset(gb[:], 0.0), writes=[gb])
        self.n = 0
        self.g2_loaded = None

    def emit(self, t, xt):
        P = self.P
        j = 1 if t == NT - 1 else 0
        if self.g2_loaded != j:
            P.dma("sync", self.g2[:], self.g2v[j:j + 1, :].partition_broadcast(128), writes=[self.g2])
            self.g2_loaded = j
        acc = self.acc
        for e_ in range(NE):
            gb = self.gbl[self.n % 4]
            self.n += 1
            P.dma_fn("gpsimd", lambda e, gb=gb, e_=e_, t=t: e.indirect_dma_start(
                out=gb[:, :], out_offset=None, in_=self.oes[e_],
                in_offset=bass.IndirectOffsetOnAxis(ap=self.pos[:, t, e_:e_ + 1], axis=0),
                bounds_check=P.reg(e, NSLOT - 1), oob_is_err=False), reads=[self.pos, gb], writes=[gb])
            gate = self.aff[:, t, e_:e_ + 1]
            if e_ == 0:
                P.op("vector", lambda e, gb=gb, gate=gate: e.tensor_scalar(
                    out=acc[:], in0=gb[:], scalar1=gate, scalar2=None, op0=ALU.mult), reads=[gb, self.aff], writes=[acc])
            else:
                P.op("vector", lambda e, gb=gb, gate=gate: e.scalar_tensor_tensor(
                    out=acc[:], in0=gb[:], scalar=gate, in1=acc[:], op0=ALU.mult, op1=ALU.add),
                    reads=[gb, acc, self.aff], writes=[acc])
        P.op("vector", lambda e: e.tensor_tensor(out=self.acc[:], in0=self.acc[:], in1=self.g2[:], op=ALU.mult),
             reads=[self.acc, self.g2], writes=[self.acc])
        P.op("vector", lambda e, xt=xt: e.tensor_tensor(out=xt[:], in0=xt[:], in1=self.acc[:], op=ALU.add),
             reads=[xt, self.acc], writes=[xt])


def build_ka(combine=False):
    nc = bass.Bass("TRN2", target_bir_lowering=False)
    x_in = nc.dram_tensor("x_in", [NT * 128, D], F32, kind="ExternalInput").ap()
    w_in = nc.dram_tensor("w_in", [D, INW], F32, kind="ExternalInput").ap()
    vecs = nc.dram_tensor("vecs", [128, 5, 16], F32, kind="ExternalInput").ap()
    gains = nc.dram_tensor("gains", [128, 2, 128], F32, kind="ExternalInput").ap()
    rope = nc.dram_tensor("rope", [128, NT, 2, 64], F32, kind="ExternalInput").ap()
    wa2 = nc.dram_tensor("wa2", [16, 2, 512], F32, kind="ExternalInput").ap()
    ba = nc.dram_tensor("ba", [1, 2, 512], F32, kind="ExternalInput").ap()
    out_bf = nc.dram_tensor("out_bf", [NT * 128, OBF_W], BF16, kind="ExternalOutput").ap()
    out_f = nc.dram_tensor("out_f", [NT * 128, OF_W], F32, kind="ExternalOutput").ap()
    P = Prog(nc)
    comb = x_new = None
    if combine:
        comb = Combiner(P, *declare_combine_inputs(nc))
        x_new = nc.dram_tensor("x_new", [NT * 128, D], F32, kind="ExternalOutput").ap()
    emit_ka_body(P, x_in, w_in, vecs, gains, rope, wa2, ba, out_bf, out_f, comb, x_new)
    P.finish()
    return nc


def emit_ka_body(P, x_in, w_in, vecs, gains, rope, wa2, ba, out_bf, out_f, comb=None, x_new=None):
    identf = make_ident(P, "identf", F32)
    vec = P.sb("vec", [128, 5, 16], F32)
    P.dma("sync", vec[:], vecs, writes=[vec])
    A = emit_modvec(P, vec, "Amod")
    gn = P.sb("gn", [128, 2, 128], F32)
    P.dma("sync", gn[:], gains, writes=[gn])
    rp = P.sb("rp", [128, NT, 2, 64], F32)
    P.dma("sync", rp[:], rope, writes=[rp])
    wa2s = P.sb("wa2s", [16, 2, 512], F32)
    P.dma("sync", wa2s[:], wa2, writes=[wa2s])
    bas = P.sb("bas", [1, 2, 512], F32)
    P.dma("sync", bas[:], ba, writes=[bas])
    ones1 = P.sb("ones1", [1, 128], F32)
    P.op("gpsimd", lambda e: e.memset(ones1[:], 1.0), writes=[ones1])

    hT = P.sb("hT", [128, NT, 16, 128], BF16)
    xts = [P.sb("xt%d" % i, [128, D], F32) for i in range(2)]
    junk = P.sb("junk", [128, D], BF16)
    ss = P.sb("ss", [128, NT], F32)
    rstd = P.sb("rstd", [128, NT], F32)
    tps = [P.ps("tp%d" % i, [128, 8, 128], F32) for i in range(2)]
    def phase1a(t):
        xt = xts[t % 2]
        P.dma("sync", xt[:], x_in[t * 128:(t + 1) * 128, :], writes=[xt])
        if comb is not None:
            comb.emit(t, xt)
            P.dma("sync", x_new[t * 128:(t + 1) * 128, :], xt[:], reads=[xt])
        P.op("scalar", lambda e, xt=xt, t=t: e.activation(
            out=junk[:], in_=xt[:], func=AF.Square, accum_out=ss[:, t:t + 1]),
            reads=[xt], writes=[junk, (ss, t)])
        emit_rsqrt(P, rstd[:, t:t + 1], ss[:, t:t + 1], 1.0 / D, (ss, t), (rstd, t))
        P.op("vector", lambda e, xt=xt, t=t: e.tensor_scalar(
            out=xt[:], in0=xt[:], scalar1=rstd[:, t:t + 1], scalar2=None, op0=ALU.mult),
            reads=[xt, (rstd, t)], writes=[xt])

    def phase1b(t):
        xt = xts[t % 2]
        mi = 1 if t == NT - 1 else 0
        for half in range(2):
            tp = tps[half]
            for j in range(8):
                kc = half * 8 + j
                P.op("tensor", lambda e, tp=tp, j=j, kc=kc, xt=xt: e.transpose(
                    out=tp[:, j, :], in_=xt[:, kc * 128:(kc + 1) * 128], identity=identf[:]),
                    reads=[xt, identf], writes=[tp])
            for j in range(8):
                kc = half * 8 + j
                P.op("scalar", lambda e, tp=tp, j=j, kc=kc, t=t, mi=mi: e.activation(
                    out=hT[:, t, kc, :], in_=tp[:, j, :], func=AF.Identity,
                    scale=A[:, mi, kc:kc + 1], bias=vec[:, 2 + 2 * mi, kc:kc + 1]),
                    reads=[tp, A, vec], writes=[(hT, t)])

    phase1a(0)
    for t in range(NT):
        if t + 1 < NT:
            phase1a(t + 1)
        phase1b(t)

    wbs = [P.sb("wb%d" % i, [128, 16, 512], BF16) for i in range(2)]
    mps = [P.ps("mp%d" % i, [128, 512], F32) for i in range(2)]
    zp = P.ps("zp", [128, 512], F32)
    tpa = P.ps("tpa", [16, 128], F32)
    sq = P.sb("sq", [128, 512], F32)
    ssq = P.sb("ssq", [128, 4], F32)
    qn = P.sb("qn", [128, 512], F32)
    rt = [P.sb("rt%d" % i, [128, 256], F32) for i in range(2)]
    sbf = [P.sb("sbf%d" % i, [128, 512], BF16) for i in range(4)]
    sf = [P.sb("sf%d" % i, [128, 512], F32) for i in range(4)]
    asb = P.sb("asb", [128, 32], F32)
    aT = P.sb("aT", [16, 128], F32)
    ez = P.sb("ez", [128, 512], F32)
    cnt = {"bf": 0, "f": 0}
    w_v = w_in.rearrange("(kc p) n -> p kc n", p=128)

    def stage(kind):
        i = cnt[kind]
        cnt[kind] += 1
        return (sbf if kind == "bf" else sf)[i % 4]

    def qk_epi(mp, c0, nh, gi, t, dst):
        w = nh * 128
        P.op("scalar", lambda e: e.activation(out=sq[:, :w], in_=mp[:, c0:c0 + w], func=AF.Square),
             reads=[mp], writes=[sq])
        P.op("vector", lambda e: e.tensor_reduce(
            out=ssq[:, :nh], in_=sq[:, :w].rearrange("p (h d) -> p h d", h=nh), axis=AX.X, op=ALU.add),
            reads=[sq], writes=[ssq])
        emit_rsqrt(P, ssq[:, :nh], ssq[:, :nh], 1.0 / 128, ssq, ssq)
        q3 = qn[:, :w].rearrange("p (h d) -> p h d", h=nh)
        P.op("vector", lambda e: e.tensor_tensor(
            out=q3, in0=mp[:, c0:c0 + w].rearrange("p (h d) -> p h d", h=nh),
            in1=ssq[:, :nh].unsqueeze(2).broadcast_to([128, nh, 128]), op=ALU.mult),
            reads=[mp, ssq], writes=[qn])
        P.op("vector", lambda e: e.tensor_tensor(
            out=q3, in0=q3, in1=gn[:, gi, :].unsqueeze(1).broadcast_to([128, nh, 128]), op=ALU.mult),
            reads=[qn, gn], writes=[qn])
        q4 = qn[:, :w].rearrange("p (h i two) -> p h i two", h=nh, two=2)
        d4 = dst.rearrange("p (h i two) -> p h i two", h=nh, two=2)
        cosb = rp[:, t, 0, :].unsqueeze(1).broadcast_to([128, nh, 64])
        sinb = rp[:, t, 1, :].unsqueeze(1).broadcast_to([128, nh, 64])
        r0 = rt[0][:, :nh * 64].rearrange("p (h i) -> p h i", h=nh)
        r1 = rt[1][:, :nh * 64].rearrange("p (h i) -> p h i", h=nh)
        for (o, a, ca, b, cb_, op) in ((0, 0, cosb, 1, sinb, ALU.subtract), (1, 0, sinb, 1, cosb, ALU.add)):
            P.op("vector", lambda e, a=a, ca=ca: e.tensor_tensor(out=r0, in0=q4[:, :, :, a], in1=ca, op=ALU.mult),
                 reads=[qn, rp], writes=[rt[0]])
            P.op("vector", lambda e, b=b, cb_=cb_: e.tensor_tensor(out=r1, in0=q4[:, :, :, b], in1=cb_, op=ALU.mult),
                 reads=[qn, rp], writes=[rt[1]])
            P.op("vector", lambda e, o=o, op=op: e.tensor_tensor(out=d4[:, :, :, o], in0=r0, in1=r1, op=op),
                 reads=[rt[0], rt[1]], writes=[dst.tensor])

    it = 0
    for cb in range(10):
        n = 512 if cb < 9 else 32
        wb = wbs[cb % 2]
        for q in range(4):
            P.dma("gpsimd", wb[:, 4 * q:4 * q + 4, :n], w_v[:, 4 * q:4 * q + 4, cb * 512:cb * 512 + n],
                  writes=[(wb, q)])
        for t in range(NT):
            mp = mps[it % 2]
            it += 1
            for kc in range(16):
                P.op("tensor", lambda e, mp=mp, t=t, kc=kc, wb=wb, n=n: e.matmul(
                    mp[:, :n], lhsT=hT[:, t, kc, :], rhs=wb[:, kc, :n], start=(kc == 0), stop=(kc == 15)),
                    reads=[(hT, t), (wb, kc // 4)], writes=[mp])
            rows = slice(t * 128, (t + 1) * 128)
            if cb in (0, 1):
                st = stage("bf")
                qk_epi(mp, 0, 4, 0, t, st[:, :])
                P.dma("sync", out_bf[rows, cb * 512:(cb + 1) * 512], st[:], reads=[st])
            elif cb == 2:
                st = stage("bf")
                qk_epi(mp, 0, 2, 1, t, st[:, 0:256])
                P.op("scalar", lambda e, st=st, mp=mp: e.activation(out=st[:, 256:512], in_=mp[:, 256:512], func=AF.Copy),
                     reads=[mp], writes=[st])
                P.dma("sync", out_bf[rows, 1024:1536], st[:], reads=[st])
            elif cb in (3, 4):
                st = stage("f")
                sc_ = 128 ** -0.5 if cb == 3 else 1.0
                P.op("scalar", lambda e, st=st, mp=mp, sc_=sc_: e.activation(out=st[:], in_=mp[:], func=AF.Copy, scale=sc_),
                     reads=[mp], writes=[st])
                P.dma("sync", out_f[rows, (cb - 3) * 512:(cb - 2) * 512], st[:], reads=[st])
            elif cb in (5, 6):
                st = stage("bf")
                P.op("scalar", lambda e, st=st, mp=mp: e.activation(out=st[:], in_=mp[:], func=AF.Copy),
                     reads=[mp], writes=[st])
                P.dma("sync", out_bf[rows, 1536 + (cb - 5) * 512:1536 + (cb - 4) * 512], st[:], reads=[st])
            elif cb in (7, 8):
                st = stage("f")
                P.op("scalar", lambda e, st=st, mp=mp: e.activation(out=st[:], in_=mp[:], func=AF.Silu),
                     reads=[mp], writes=[st])
                P.dma("sync", out_f[rows, 1024 + (cb - 7) * 512:1024 + (cb - 6) * 512], st[:], reads=[st])
            else:
                P.op("scalar", lambda e, mp=mp: e.activation(out=asb[:], in_=mp[:, :32], func=AF.Copy),
                     reads=[mp], writes=[asb])
                for d in range(2):
                    P.op("tensor", lambda e, d=d: e.transpose(out=tpa[:], in_=asb[:, d * 16:(d + 1) * 16], identity=identf[:]),
                         reads=[asb, identf], writes=[tpa])
                    P.op("vector", lambda e: e.tensor_copy(out=aT[:], in_=tpa[:]), reads=[tpa], writes=[aT])
                    P.op("tensor", lambda e, d=d: e.matmul(zp[:], lhsT=aT[:], rhs=wa2s[:, d, :], start=True, stop=False),
                         reads=[aT, wa2s], writes=[zp])
                    P.op("tensor", lambda e, d=d: e.matmul(zp[:], lhsT=ones1[:], rhs=bas[:, d, :], start=False, stop=True),
                         reads=[ones1, bas], writes=[zp])
                    st = stage("f")
                    P.op("scalar", lambda e: e.activation(out=ez[:], in_=zp[:], func=AF.Exp, scale=-1.0),
                         reads=[zp], writes=[ez])
                    P.op("scalar", lambda e: e.activation(out=ez[:], in_=ez[:], func=AF.Ln, bias=1.0),
                         reads=[ez], writes=[ez])
                    P.op("vector", lambda e, st=st: e.tensor_scalar(out=st[:], in0=ez[:], scalar1=-1.0 / 16, scalar2=None, op0=ALU.mult),
                         reads=[ez], writes=[st])
                    P.dma("sync", out_f[rows, 2048 + d * 512:2048 + (d + 1) * 512], st[:], reads=[st])


def fm(v):
    return np.ascontiguousarray(np.asarray(v).reshape(16, 128).T)


def rope_tables():
    rows = SEQ // 64
    row = np.repeat(np.arange(rows, dtype=np.float32), 64)
    col = np.tile(np.arange(64, dtype=np.float32), rows)
    inv = (np.float32(10000.0) ** (-np.arange(0, 64, 2, dtype=np.float32) / np.float32(64))).astype(np.float32)
    ang = np.concatenate([row[:, None] * inv, col[:, None] * inv], axis=-1).astype(np.float32)
    return np.cos(ang).astype(np.float32), np.sin(ang).astype(np.float32)


def core_tokens(a_lat, a_ctx, i):
    return np.concatenate([a_lat[i * 1024:(i + 1) * 1024], a_ctx[(i % 2) * 128:(i % 2) * 128 + 128]], axis=0)


def ka_static_inputs(inputs, mods, l):
    m_lat, m_ctx = mods[l, 0], mods[l, 1]
    vec = np.stack([fm(inputs["norm_mix"][l]), fm(m_lat[D:2 * D]), fm(m_lat[0:D]),
                    fm(m_ctx[D:2 * D]), fm(m_ctx[0:D])], axis=1).astype(np.float32)
    gains = np.ascontiguousarray(np.broadcast_to(
        np.stack([inputs["q_gain"][l], inputs["k_gain"][l]])[None], (128, 2, 128))).astype(np.float32)
    cos, sin = rope_tables()
    ropes = []
    for i in range(NCORES):
        c = core_tokens(cos, np.ones((CTX, 64), np.float32), i)
        s = core_tokens(sin, np.zeros((CTX, 64), np.float32), i)
        r = np.stack([c, s], axis=1).reshape(NT, 128, 2, 64).transpose(1, 0, 2, 3)
        ropes.append(np.ascontiguousarray(r))
    wa2 = np.ascontiguousarray(inputs["w_gla_a2"][l].transpose(1, 0, 2))
    ba = np.ascontiguousarray(inputs["b_gla_a"][l][None])
    return vec, gains, ropes, wa2, ba


def combine_maps(mods, l_prev, kd_outs, aff_lat, aff_ctx):
    oes = {}
    for i, (oe, pl, pc) in enumerate(kd_outs):
        oes["oe%d" % (2 * i)] = np.ascontiguousarray(oe[0])
        oes["oe%d" % (2 * i + 1)] = np.ascontiguousarray(oe[1])
    pos_lat = np.concatenate([o[1].transpose(2, 0, 1).reshape(SEQ, 2) for o in kd_outs], axis=1)
    pos_ctx = np.concatenate([o[2].transpose(2, 0, 1).reshape(CTX, 2) for o in kd_outs], axis=1)
    g2v = np.stack([mods[l_prev, 0][5 * D:6 * D], mods[l_prev, 1][5 * D:6 * D]]).astype(np.float32)
    maps = []
    for i in range(NCORES):
        cp = core_tokens(pos_lat, pos_ctx, i).reshape(NT, 128, NE).transpose(1, 0, 2)
        m = dict(oes)
        m["cpos"] = np.ascontiguousarray(cp).astype(np.int32)
        m["caff"] = np.ascontiguousarray(core_tokens(aff_lat, aff_ctx, i).reshape(NT, 128, NE).transpose(1, 0, 2))
        m["g2v"] = g2v
        maps.append(m)
    return maps


def run_ka(inputs, mods, l, x_lat, x_ctx, nc=None, comb_maps=None):
    nc = nc or build_ka(combine=comb_maps is not None)
    vec, gains, ropes, wa2, ba = ka_static_inputs(inputs, mods, l)
    w_in = np.ascontiguousarray(inputs["w_in"][l])
    maps = []
    for i in range(NCORES):
        maps.append({"x_in": core_tokens(x_lat, x_ctx, i), "w_in": w_in, "vecs": vec, "gains": gains,
                     "rope": ropes[i], "wa2": wa2, "ba": ba})
        if comb_maps is not None:
            maps[-1].update(comb_maps[i])
    res = run_bass_kernel_spmd(nc, maps, core_ids=list(range(NCORES)))
    if comb_maps is not None:
        return [(r["out_bf"], r["out_f"], r["x_new"]) for r in res.results]
    return [(r["out_bf"], r["out_f"]) for r in res.results]


def gather_tokens(per_core):
    lat = np.concatenate([a[:1024] for a in per_core], axis=0)
    ctx = np.concatenate([per_core[0][1024:], per_core[1][1024:]], axis=0)
    return lat, ctx


NTOK = CTX + SEQ
NTT = NTOK // 128
GG = 6
GLA_EVERY = (1, 1)


def build_kb(do_gla=True, do_attn=True, nblk=None, ngrp=None):
    nc = bass.Bass("TRN2", target_bir_lowering=False)
    qT = nc.dram_tensor("qT", [128, NTOK], BF16, kind="ExternalInput").ap()
    kT = nc.dram_tensor("kT", [128, NTOK], BF16, kind="ExternalInput").ap()
    v = nc.dram_tensor("v", [NTOK, 128], BF16, kind="ExternalInput").ap()
    gqT = nc.dram_tensor("gqT", [128, NTOK], F32, kind="ExternalInput").ap()
    gkT = nc.dram_tensor("gkT", [128, NTOK], F32, kind="ExternalInput").ap()
    gk = nc.dram_tensor("gk", [NTOK, 128], F32, kind="ExternalInput").ap()
    gv = nc.dram_tensor("gv", [NTOK, 256], BF16, kind="ExternalInput").ap()
    la = nc.dram_tensor("la", [NTOK, 128], F32, kind="ExternalInput").ap()
    oT = nc.dram_tensor("oT", [128, NTOK], BF16, kind="ExternalOutput").ap()
    og = nc.dram_tensor("og", [NTOK, 256], F32, kind="ExternalOutput").ap()
    P = Prog(nc)
    if do_gla and do_attn:
        emit_attn(P, qT, kT, v, oT, nblk, side=gla_gen(P, gqT, gkT, gk, gv, la, og, ngrp))
    elif do_gla:
        emit_gla(P, gqT, gkT, gk, gv, la, og, ngrp)
    elif do_attn:
        emit_attn(P, qT, kT, v, oT, nblk)
    P.finish()
    return nc


class PV:
    def __init__(self, t, c0, n):
        self.t, self.c0, self.n, self.name = t, c0, n, t.name

    def __getitem__(self, key):
        if not isinstance(key, tuple):
            key = (key, slice(None))
        rows, cols = key
        start = cols.start or 0
        stop = self.n if cols.stop is None else cols.stop
        return self.t[rows, self.c0 + start:self.c0 + stop]


def make_tri(P, name, upper):
    m = P.sb(name, [128, 128], F32)
    P.op("gpsimd", lambda e: e.memset(m[:], 0.0), writes=[m])
    for b in range(2):
        blk = m[b * 64:(b + 1) * 64, b * 64:(b + 1) * 64]
        P.op("gpsimd", lambda e, blk=blk: e.memset(blk, 1.0), reads=[m], writes=[m])
        if upper:
            P.op("gpsimd", lambda e, blk=blk: e.affine_select(
                out=blk, in_=blk, pattern=[[1, 64]], compare_op=ALU.is_ge, fill=0.0, base=0,
                channel_multiplier=-1), reads=[m], writes=[m])
        else:
            P.op("gpsimd", lambda e, blk=blk: e.affine_select(
                out=blk, in_=blk, pattern=[[-1, 64]], compare_op=ALU.is_gt, fill=0.0, base=0,
                channel_multiplier=1), reads=[m], writes=[m])
    return m


def gla_gen(P, gqT, gkT, gk, gv, la, og, ngrp_lim=None):
    U2 = make_tri(P, "U2", True)
    L2 = make_tri(P, "L2", False)
    ngrp = ngrp_lim or NTT // GG
    gqT_s = [P.sb("gqT_s%d" % i, [128, GG * 128], F32) for i in range(2)]
    gkT_s = [P.sb("gkT_s%d" % i, [128, GG * 128], F32) for i in range(2)]
    gk_s = [P.sb("gk_s%d" % i, [128, GG, 128], F32) for i in range(2)]
    gv_s = [P.sb("gv_s%d" % i, [128, GG, 256], BF16) for i in range(2)]
    la_s = [P.sb("la_s%d" % i, [128, GG, 128], F32) for i in range(2)]
    bankA = P.ps("gla_bankA", [128, 512], F32)
    bankB = P.ps("gla_bankB", [128, 512], F32)
    bT_ps, bl_ps, at_ps = PV(bankA, 0, 128), PV(bankA, 128, 128), PV(bankA, 256, 128)
    o_ps = [PV(bankB, 0, 256)] * 2
    ds_ps = [P.ps("ds_ps%d" % i, [128, 256], F32) for i in range(2)]
    eb = [P.sb("eb%d" % i, [128, 128], F32) for i in range(2)]
    enb = P.sb("enb", [128, 128], F32)
    ekh = P.sb("ekh", [128, 128], F32)
    qtT = [P.sb("qtT%d" % i, [128, 128], BF16) for i in range(2)]
    ktT = P.sb("ktT", [128, 128], BF16)
    kh = [P.sb("kh%d" % i, [128, 128], BF16) for i in range(2)]
    atm = P.sb("atm", [128, 128], BF16)
    S = [P.sb("S%d" % i, [128, 256], F32) for i in range(2)]
    Sb = [P.sb("Sb%d" % i, [128, 256], BF16) for i in range(4)]
    ost = [P.sb("ost%d" % i, [128, 256], F32) for i in range(2)]
    P.op("gpsimd", lambda e: e.memset(S[0][:], 0.0), writes=[S[0]])
    P.op("gpsimd", lambda e: e.memset(Sb[0][:], 0.0), writes=[Sb[0]])
    sc = 0
    def load_group(g):
        b = g % 2
        r0 = g * GG * 128
        P.dma("sync", gqT_s[b][:], gqT[:, r0:r0 + GG * 128], writes=[gqT_s[b]])
        P.dma("sync", gkT_s[b][:], gkT[:, r0:r0 + GG * 128], writes=[gkT_s[b]])
        P.dma("sync", gk_s[b][:], gk[r0:r0 + GG * 128, :].rearrange("(t p) d -> p t d", p=128), writes=[gk_s[b]])
        P.dma("sync", gv_s[b][:], gv[r0:r0 + GG * 128, :].rearrange("(t p) d -> p t d", p=128), writes=[gv_s[b]])
        P.dma("sync", la_s[b][:], la[r0:r0 + GG * 128, :].rearrange("(t p) d -> p t d", p=128), writes=[la_s[b]])

    load_group(0)
    for g in range(ngrp):
        b = g % 2
        if g + 1 < ngrp:
            load_group(g + 1)
        gq_b, gkT_b, gk_b, gv_b = gqT_s[b], gkT_s[b], gk_s[b], gv_s[b]
        for tt in range(GG):
            ti = g * GG + tt
            p2 = ti % 2
            la_t = la_s[b][:, tt, :]
            cols = slice(tt * 128, (tt + 1) * 128)
            P.op("tensor", lambda e, la_t=la_t: e.matmul(bT_ps[:], lhsT=la_t, rhs=U2[:], start=True, stop=True),
                 reads=[la_s[b], U2], writes=[bT_ps])
            P.op("tensor", lambda e, la_t=la_t: e.matmul(bl_ps[:], lhsT=L2[:], rhs=la_t, start=True, stop=True),
                 reads=[la_s[b], L2], writes=[bl_ps])
            yield
            ebt = eb[p2]
            P.op("scalar", lambda e, ebt=ebt: e.activation(out=ebt[:], in_=bT_ps[:], func=AF.Exp), reads=[bT_ps], writes=[ebt])
            P.op("scalar", lambda e: e.activation(out=enb[:], in_=bT_ps[:], func=AF.Exp, scale=-1.0), reads=[bT_ps], writes=[enb])
            P.op("scalar", lambda e: e.activation(out=ekh[:], in_=bl_ps[:], func=AF.Exp), reads=[bl_ps], writes=[ekh])
            yield
            q_t = qtT[p2]
            kh_t = kh[p2]
            P.op("vector", lambda e, q_t=q_t, ebt=ebt, cols=cols, gq_b=gq_b: e.tensor_tensor(out=q_t[:], in0=gq_b[:, cols], in1=ebt[:], op=ALU.mult),
                 reads=[gqT_s[b], ebt], writes=[q_t])
            P.op("vector", lambda e, cols=cols, gkT_b=gkT_b: e.tensor_tensor(out=ktT[:], in0=gkT_b[:, cols], in1=enb[:], op=ALU.mult),
                 reads=[gkT_s[b], enb], writes=[ktT])
            P.op("vector", lambda e, kh_t=kh_t, tt=tt, gk_b=gk_b: e.tensor_tensor(out=kh_t[:], in0=gk_b[:, tt, :], in1=ekh[:], op=ALU.mult),
                 reads=[gk_s[b], ekh], writes=[kh_t])
            yield
            P.op("tensor", lambda e, q_t=q_t: e.matmul(at_ps[:], lhsT=ktT[:], rhs=q_t[:], start=True, stop=True),
                 reads=[ktT, q_t], writes=[at_ps])
            yield
            P.op("vector", lambda e: e.tensor_tensor(out=atm[:], in0=at_ps[:], in1=U2[:], op=ALU.mult),
                 reads=[at_ps, U2], writes=[atm])
            yield
            ops_ = o_ps[p2]
            gv_t = gv_s[b][:, tt, :]
            P.op("tensor", lambda e, ops_=ops_, gv_t=gv_t: e.matmul(ops_[:], lhsT=atm[:], rhs=gv_t, start=True, stop=False),
                 reads=[atm, gv_s[b]], writes=[ops_])
            for c in range(2):
                rs = slice(c * 64, (c + 1) * 64)
                Sb_c = Sb[sc % 4]
                P.op("tensor", lambda e, ops_=ops_, q_t=q_t, rs=rs, Sb_c=Sb_c, c=c: e.matmul(
                    ops_[rs, :], lhsT=q_t[:, rs], rhs=Sb_c[:], start=False, stop=True),
                    reads=[q_t, Sb_c], writes=[ops_])
                dsp = ds_ps[sc % 2]
                P.op("tensor", lambda e, dsp=dsp, kh_t=kh_t, rs=rs, tt=tt, gv_b=gv_b: e.matmul(
                    dsp[:], lhsT=kh_t[rs, :], rhs=gv_b[rs, tt, :], start=True, stop=True),
                    reads=[kh_t, gv_s[b]], writes=[dsp])
                yield
                S_cur, S_nxt = S[sc % 2], S[(sc + 1) % 2]
                col = c * 64 + 63
                P.op("vector", lambda e, S_cur=S_cur, S_nxt=S_nxt, ebt=ebt, col=col, dsp=dsp: e.scalar_tensor_tensor(
                    out=S_nxt[:], in0=S_cur[:], scalar=ebt[:, col:col + 1], in1=dsp[:], op0=ALU.mult, op1=ALU.add),
                    reads=[S_cur, ebt, dsp], writes=[S_nxt])
                Sb_n = Sb[(sc + 1) % 4]
                P.op("vector", lambda e, Sb_n=Sb_n, S_nxt=S_nxt: e.tensor_copy(out=Sb_n[:], in_=S_nxt[:]),
                     reads=[S_nxt], writes=[Sb_n])
                sc += 1
                yield
            o_t = ost[p2]
            P.op("scalar", lambda e, o_t=o_t, ops_=ops_: e.activation(out=o_t[:], in_=ops_[:], func=AF.Copy),
                 reads=[ops_], writes=[o_t])
            P.dma("sync", og[ti * 128:(ti + 1) * 128, :], o_t[:], reads=[o_t])
            yield


def emit_gla(P, gqT, gkT, gk, gv, la, og, ngrp_lim=None):
    for _ in gla_gen(P, gqT, gkT, gk, gv, la, og, ngrp_lim):
        pass


def emit_attn(P, qT, kT, v, oT, nblk=None, side=None):
    q_s = P.sb("q_s", [128, NTOK], BF16)
    k_s = P.sb("k_s", [128, NTOK], BF16)
    v_s = P.sb("v_s", [128, NTT, 128], BF16)
    for j in range(4):
        cs = slice(j * (NTOK // 4), (j + 1) * (NTOK // 4))
        P.dma("sync", q_s[:, cs], qT[:, cs], writes=[(q_s, j)])
        P.dma("scalar", k_s[:, cs], kT[:, cs], writes=[(k_s, j)])
    vv = v.rearrange("(t p) d -> p t d", p=128)
    for j in range(3):
        ts_ = slice(j * 22, (j + 1) * 22)
        P.dma("sync", v_s[:, ts_, :], vv[:, ts_, :], writes=[(v_s, j)])
    ones = P.sb("ones_f32", [128, 128], F32)
    P.op("gpsimd", lambda e: e.memset(ones[:], 1.0), writes=[ones])
    pacc = [P.sb("pacc%d" % i, [128, 512], F32) for i in range(2)]
    st_ps = [P.ps("st_ps%d" % i, [128, 512], F32) for i in range(2)]
    oa_ps = [P.ps("oa_ps", [128, 512], F32)] * 2
    dn_ps = [P.ps("dn_ps", [128, 512], F32)] * 2
    pT = [P.sb("pT%d" % i, [128, 512], BF16) for i in range(3)]
    rden = P.sb("rden", [128, 512], F32)
    ob = [P.sb("ob%d" % i, [128, 512], BF16) for i in range(2)]
    blocks = [(0, CTX, 2)] + [(CTX + i * 512, 512, NTT) for i in range(SEQ // 512)]
    qkeys = [(q_s, j) for j in range(4)]
    if nblk:
        blocks = blocks[:nblk]
    iters = [(bi, q0, n, ns, s_) for bi, (q0, n, ns) in enumerate(blocks) for s_ in range(ns)]

    def emit_s(idx):
        bi, q0, n, ns, s_ = iters[idx]
        sp = st_ps[idx % 2]
        kk = (k_s, (s_ * 128) // (NTOK // 4))
        kk2 = (k_s, (s_ * 128 + 127) // (NTOK // 4))
        P.op("tensor", lambda e: e.matmul(sp[:, :n], lhsT=k_s[:, s_ * 128:(s_ + 1) * 128], rhs=q_s[:, q0:q0 + n],
                                          start=True, stop=True), reads=[kk, kk2] + qkeys, writes=[sp])

    emit_s(0)
    for idx, (bi, q0, n, ns, s_) in enumerate(iters):
        if idx + 1 < len(iters):
            emit_s(idx + 1)
        oa, dn = oa_ps[0], dn_ps[0]
        sp, pt = st_ps[idx % 2], pT[idx % 3]
        P.op("scalar", lambda e, sp=sp, pt=pt, n=n: e.activation(
            out=pt[:, :n], in_=sp[:, :n], func=AF.Exp, scale=128 ** -0.5), reads=[sp], writes=[pt])
        P.op("tensor", lambda e, pt=pt, s_=s_, n=n, ns=ns: e.matmul(
            oa[:, :n], lhsT=v_s[:, s_, :], rhs=pt[:, :n], start=(s_ == 0), stop=(s_ == ns - 1)),
            reads=[(v_s, s_ // 22), pt], writes=[oa])
        pa = pacc[bi % 2]
        if s_ == 0:
            P.op("vector", lambda e, pa=pa, pt=pt, n=n: e.tensor_copy(out=pa[:, :n], in_=pt[:, :n]), reads=[pt], writes=[pa])
        else:
            P.op("vector", lambda e, pa=pa, pt=pt, n=n: e.tensor_tensor(out=pa[:, :n], in0=pa[:, :n], in1=pt[:, :n], op=ALU.add),
                 reads=[pt, pa], writes=[pa])
        if s_ == ns - 1:
            P.op("tensor", lambda e, pa=pa, n=n: e.matmul(dn[:, :n], lhsT=ones[:], rhs=pa[:, :n], start=True, stop=True),
                 reads=[ones, pa], writes=[dn])
        if s_ == ns - 1:
            o_b = ob[bi % 2]
            P.op("vector", lambda e, n=n: e.reciprocal(out=rden[:, :n], in_=dn[:, :n]), reads=[dn], writes=[rden])
            P.op("vector", lambda e, o_b=o_b, n=n: e.tensor_tensor(out=o_b[:, :n], in0=oa[:, :n], in1=rden[:, :n], op=ALU.mult),
                 reads=[oa, rden], writes=[o_b])
            P.dma("sync", oT[:, q0:q0 + n], o_b[:, :n], reads=[o_b])
        if side is not None:
            for _ in range(max(1, GLA_EVERY[0] // GLA_EVERY[1]) if idx % GLA_EVERY[1] < GLA_EVERY[0] else 0):
                next(side, None)
    if side is not None:
        for _ in side:
            pass


def run_kb(bf_lat, bf_ctx, f_lat, f_ctx, nc=None):
    nc = nc or build_kb()
    bf = np.concatenate([bf_ctx, bf_lat], axis=0)
    f = np.concatenate([f_ctx, f_lat], axis=0)
    rev = np.concatenate([np.arange(CTX)[::-1], CTX + np.arange(SEQ)[::-1]])
    maps = []
    for i in range(NCORES):
        kv, h, d = i // 4, i % 4, i // 4
        order = rev if d == 1 else np.arange(NTOK)
        la = f[:, 2048 + d * 512 + h * 128:2048 + d * 512 + (h + 1) * 128][order]
        gq = f[:, h * 128:(h + 1) * 128][order]
        gk = f[:, 512 + h * 128:512 + (h + 1) * 128][order]
        gv = bf[:, 1536 + h * 256:1536 + (h + 1) * 256][order]
        maps.append({
            "qT": np.ascontiguousarray(bf[:, i * 128:(i + 1) * 128].T),
            "kT": np.ascontiguousarray(bf[:, 1024 + kv * 128:1024 + (kv + 1) * 128].T),
            "v": np.ascontiguousarray(bf[:, 1280 + kv * 128:1280 + (kv + 1) * 128]),
            "gqT": np.ascontiguousarray(gq.T), "gkT": np.ascontiguousarray(gk.T),
            "gk": np.ascontiguousarray(gk), "gv": np.ascontiguousarray(gv), "la": np.ascontiguousarray(la),
        })
    res = run_bass_kernel_spmd(nc, maps, core_ids=list(range(NCORES)))
    inv = np.argsort(rev)
    attnT = [r["oT"] for r in res.results]
    og = [r["og"] if i < 4 else r["og"][inv] for i, r in enumerate(res.results)]
    return attnT, og


def build_kc():
    nc = bass.Bass("TRN2", target_bir_lowering=False)
    R = NT * 128
    attnT = nc.dram_tensor("attnT", [128, 8, R], BF16, kind="ExternalInput").ap()
    ogf = nc.dram_tensor("ogf", [R, 1024], F32, kind="ExternalInput").ap()
    ogb = nc.dram_tensor("ogb", [R, 1024], F32, kind="ExternalInput").ap()
    sr = nc.dram_tensor("sr", [R, 1024], F32, kind="ExternalInput").ap()
    x_in = nc.dram_tensor("x_in", [R, D], F32, kind="ExternalInput").ap()
    w_out = nc.dram_tensor("w_out", [D, D], F32, kind="ExternalInput").ap()
    w_r = nc.dram_tensor("w_r", [D, NE], F32, kind="ExternalInput").ap()
    vecs = nc.dram_tensor("vecs", [7, D], F32, kind="ExternalInput").ap()
    ggain = nc.dram_tensor("ggain", [1, 256], F32, kind="ExternalInput").ap()
    x_out = nc.dram_tensor("x_out", [R, D], F32, kind="ExternalOutput").ap()
    h2_out = nc.dram_tensor("h2_out", [R, D], BF16, kind="ExternalOutput").ap()
    aff_out = nc.dram_tensor("aff_out", [R, NE], F32, kind="ExternalOutput").ap()
    P = Prog(nc)
    identf = make_ident(P, "identf", F32)
    identb = P.sb("identb", [128, 128], BF16)
    P.op("vector", lambda e: e.tensor_copy(out=identb[:], in_=identf[:]), reads=[identf], writes=[identb])
    wo = P.sb("wo", [128, 16, D], BF16)
    wv = w_out.rearrange("(kc p) n -> p kc n", p=128)
    for q in range(16):
        P.dma("gpsimd", wo[:, q, :].rearrange("p (a n) -> p a n", a=4), wv[:, q, :].rearrange("p (a n) -> p a n", a=4),
              writes=[(wo, q)])
    wr = P.sb("wr", [128, 16, NE], F32)
    P.dma("sync", wr[:], w_r.rearrange("(kc p) n -> p kc n", p=128), writes=[wr])
    gg = P.sb("gg", [128, 256], F32)
    P.dma("sync", gg[:], ggain.partition_broadcast(128), writes=[gg])
    nf = P.sb("nf", [128, D], F32)
    P.dma("sync", nf[:], vecs[0:1, :].partition_broadcast(128), writes=[nf])
    g1 = P.sb("g1", [128, D], F32)
    A2 = P.sb("A2", [128, D], F32)
    B2 = P.sb("B2", [128, D], F32)

    def load_g1(j):
        P.dma("sync", g1[:], vecs[1 + 3 * j:2 + 3 * j, :].partition_broadcast(128), writes=[g1])

    def load_ab(j):
        P.dma("sync", A2[:], vecs[2 + 3 * j:3 + 3 * j, :].partition_broadcast(128), writes=[A2])
        P.dma("sync", B2[:], vecs[3 + 3 * j:4 + 3 * j, :].partition_broadcast(128), writes=[B2])
        P.op("vector", lambda e: e.scalar_tensor_tensor(out=A2[:], in0=A2[:], scalar=1.0, in1=nf[:], op0=ALU.add, op1=ALU.mult),
             reads=[A2, nf], writes=[A2])

    xts = [P.sb("xt%d" % i, [128, D], F32) for i in range(2)]
    of_ts = [P.sb("of_t%d" % i, [128, 1024], F32) for i in range(2)]
    ob_ts = [P.sb("ob_t%d" % i, [128, 1024], F32) for i in range(2)]
    sr_ts = [P.sb("sr_t%d" % i, [128, 1024], F32) for i in range(2)]
    sq = P.sb("sq", [128, 1024], F32)
    gsss = [P.sb("gss%d" % i, [128, 4], F32) for i in range(2)]
    glb = P.sb("glb", [128, 1024], BF16)
    catT = [P.sb("catT%d" % i, [128, 16, 128], BF16) for i in range(2)]
    tpb = P.ps("tpb", [128, 8, 128], BF16)
    yps = [P.ps("yps%d" % i, [128, 512], F32) for i in range(2)]
    tpf = [P.ps("tpf%d" % i, [128, 4, 128], F32) for i in range(2)]
    lg_ps = P.ps("lg_ps", [128, NE], F32)
    ss2 = P.sb("ss2", [128, 1], F32)
    junk = P.sb("junk", [128, D], BF16)
    h2 = P.sb("h2", [128, D], F32)
    h2b = [P.sb("h2b%d" % i, [128, D], BF16) for i in range(2)]
    h2T = P.sb("h2T", [128, 16, 128], F32)
    mx = P.sb("mx", [128, 1], F32)
    ssum = P.sb("ssum", [128, 1], F32)
    ex = P.sb("ex", [128, NE], F32)
    affs = [P.sb("affs%d" % i, [128, NE], F32) for i in range(2)]
    tmp = P.sb("tmp", [128, 512], F32)
    itc = [0]

    def part1(t):
        rows = slice(t * 128, (t + 1) * 128)
        xt = xts[t % 2]
        ct = catT[t % 2]
        of_t, ob_t, sr_t, gss = of_ts[t % 2], ob_ts[t % 2], sr_ts[t % 2], gsss[t % 2]
        P.dma("sync", xt[:], x_in[rows, :], writes=[xt])
        P.dma("sync", ct[:, 0:8, :], attnT[:, :, rows], writes=[(ct, 0)])
        P.dma("sync", of_t[:], ogf[rows, :], writes=[of_t])
        P.dma("sync", ob_t[:], ogb[rows, :], writes=[ob_t])
        P.dma("sync", sr_t[:], sr[rows, :], writes=[sr_t])
        P.op("vector", lambda e: e.tensor_tensor(out=of_t[:], in0=of_t[:], in1=ob_t[:], op=ALU.add),
             reads=[of_t, ob_t], writes=[of_t])
        P.op("scalar", lambda e: e.activation(out=sq[:], in_=of_t[:], func=AF.Square),
             reads=[of_t], writes=[sq])
        P.op("vector", lambda e: e.tensor_reduce(out=gss[:], in_=sq[:].rearrange("p (h d) -> p h d", h=4), axis=AX.X, op=ALU.add),
             reads=[sq], writes=[gss])
        emit_rsqrt(P, gss[:], gss[:], 1.0 / 256, gss, gss)
        o3 = of_t[:].rearrange("p (h d) -> p h d", h=4)
        P.op("vector", lambda e: e.tensor_tensor(out=sr_t[:].rearrange("p (h d) -> p h d", h=4), in0=sr_t[:].rearrange("p (h d) -> p h d", h=4),
                                                 in1=gg[:].unsqueeze(1).broadcast_to([128, 4, 256]), op=ALU.mult),
             reads=[sr_t, gg], writes=[sr_t])
        P.op("vector", lambda e, o3=o3: e.tensor_tensor(out=o3, in0=o3, in1=gss[:].unsqueeze(2).broadcast_to([128, 4, 256]), op=ALU.mult),
             reads=[of_t, gss], writes=[of_t])
        P.op("vector", lambda e: e.tensor_tensor(out=glb[:], in0=of_t[:], in1=sr_t[:], op=ALU.mult),
             reads=[of_t, sr_t], writes=[glb])
        for j in range(8):
            P.op("tensor", lambda e, j=j: e.transpose(out=tpb[:, j, :], in_=glb[:, j * 128:(j + 1) * 128], identity=identb[:]),
                 reads=[glb, identb], writes=[tpb])
        P.op("scalar", lambda e, ct=ct: e.activation(out=ct[:, 8:16, :], in_=tpb[:], func=AF.Copy), reads=[tpb], writes=[(ct, 1)])

    def part2a(t):
        rows = slice(t * 128, (t + 1) * 128)
        xt = xts[t % 2]
        ct = catT[t % 2]
        if t == 0:
            load_g1(0)
        if t == NT - 1:
            load_g1(1)
        for cb in range(4):
            yp = yps[itc[0] % 2]
            itc[0] += 1
            cs = slice(cb * 512, (cb + 1) * 512)
            for kc in range(16):
                P.op("tensor", lambda e, yp=yp, ct=ct, kc=kc, cs=cs: e.matmul(
                    yp[:], lhsT=ct[:, kc, :], rhs=wo[:, kc, cs], start=(kc == 0), stop=(kc == 15)),
                    reads=[(ct, kc // 8), (wo, kc)], writes=[yp])
            P.op("vector", lambda e, yp=yp, cs=cs: e.tensor_tensor(out=tmp[:], in0=yp[:], in1=g1[:, cs], op=ALU.mult),
                 reads=[yp, g1], writes=[tmp])
            P.op("vector", lambda e, xt=xt, cs=cs: e.tensor_tensor(out=xt[:, cs], in0=xt[:, cs], in1=tmp[:], op=ALU.add),
                 reads=[tmp, xt], writes=[xt])
        P.dma("sync", x_out[rows, :], xt[:], reads=[xt])

    def part2b(t):
        rows = slice(t * 128, (t + 1) * 128)
        xt = xts[t % 2]
        if t == 0:
            load_ab(0)
        if t == NT - 1:
            load_ab(1)
        P.op("scalar", lambda e, xt=xt: e.activation(out=junk[:], in_=xt[:], func=AF.Square, accum_out=ss2[:]),
             reads=[xt], writes=[junk, ss2])
        emit_rsqrt(P, ss2[:], ss2[:], 1.0 / D, ss2, ss2)
        P.op("vector", lambda e, xt=xt: e.scalar_tensor_tensor(out=h2[:], in0=xt[:], scalar=ss2[:, 0:1], in1=A2[:], op0=ALU.mult, op1=ALU.mult),
             reads=[xt, ss2, A2], writes=[h2])
        P.op("vector", lambda e: e.tensor_tensor(out=h2[:], in0=h2[:], in1=B2[:], op=ALU.add), reads=[h2, B2], writes=[h2])
        hb = h2b[t % 2]
        P.op("scalar", lambda e, hb=hb: e.activation(out=hb[:], in_=h2[:], func=AF.Copy), reads=[h2], writes=[hb])
        P.dma("sync", h2_out[rows, :], hb[:], reads=[hb])
        for q in range(4):
            tp = tpf[q % 2]
            for j in range(4):
                kc = q * 4 + j
                P.op("tensor", lambda e, tp=tp, j=j, kc=kc: e.transpose(out=tp[:, j, :], in_=h2[:, kc * 128:(kc + 1) * 128], identity=identf[:]),
                     reads=[h2, identf], writes=[tp])
            P.op("scalar", lambda e, tp=tp, q=q: e.activation(out=h2T[:, q * 4:(q + 1) * 4, :], in_=tp[:], func=AF.Copy),
                 reads=[tp], writes=[(h2T, q)])
        for kc in range(16):
            P.op("tensor", lambda e, kc=kc: e.matmul(lg_ps[:], lhsT=h2T[:, kc, :], rhs=wr[:, kc, :], start=(kc == 0), stop=(kc == 15)),
                 reads=[(h2T, kc // 4), wr], writes=[lg_ps])
        P.op("vector", lambda e: e.tensor_reduce(out=mx[:], in_=lg_ps[:], axis=AX.X, op=ALU.max), reads=[lg_ps], writes=[mx])
        P.op("vector", lambda e: e.tensor_scalar(out=mx[:], in0=mx[:], scalar1=-1.0, scalar2=None, op0=ALU.mult), reads=[mx], writes=[mx])
        P.op("scalar", lambda e: e.activation(out=ex[:], in_=lg_ps[:], func=AF.Exp, bias=mx[:, 0:1], accum_out=ssum[:]),
             reads=[lg_ps, mx], writes=[ex, ssum])
        P.op("vector", lambda e: e.reciprocal(out=ssum[:], in_=ssum[:]), reads=[ssum], writes=[ssum])
        af = affs[t % 2]
        P.op("vector", lambda e, af=af: e.tensor_scalar(out=af[:], in0=ex[:], scalar1=ssum[:, 0:1], scalar2=None, op0=ALU.mult),
             reads=[ex, ssum], writes=[af])
        P.dma("sync", aff_out[rows, :], af[:], reads=[af])

    part1(0)
    part2a(0)
    for t in range(NT):
        if t + 1 < NT:
            part1(t + 1)
            part2a(t + 1)
        part2b(t)
    P.finish()
    return nc


def run_kc(inputs, mods, l, attnT, og, sr_lat, sr_ctx, x_lat, x_ctx, nc=None):
    nc = nc or build_kc()
    m_lat, m_ctx = mods[l, 0], mods[l, 1]
    vecs = np.stack([inputs["norm_ffn"][l], m_lat[2 * D:3 * D], m_lat[4 * D:5 * D], m_lat[3 * D:4 * D],
                     m_ctx[2 * D:3 * D], m_ctx[4 * D:5 * D], m_ctx[3 * D:4 * D]]).astype(np.float32)
    ggain = np.ascontiguousarray(inputs["gla_gain"][l][None]).astype(np.float32)
    aT = np.stack(attnT, axis=1)
    ogf = np.concatenate(og[0:4], axis=1)
    ogb = np.concatenate(og[4:8], axis=1)
    w_out = np.ascontiguousarray(inputs["w_out"][l])
    w_r = np.ascontiguousarray(inputs["w_router"][l])
    maps = []
    for i in range(NCORES):
        tok = np.concatenate([CTX + np.arange(i * 1024, (i + 1) * 1024), (i % 2) * 128 + np.arange(128)])
        maps.append({"attnT": np.ascontiguousarray(aT[:, :, tok]), "ogf": ogf[tok], "ogb": ogb[tok],
                     "sr": core_tokens(sr_lat, sr_ctx, i), "x_in": core_tokens(x_lat, x_ctx, i),
                     "w_out": w_out, "w_r": w_r, "vecs": vecs, "ggain": ggain})
    res = run_bass_kernel_spmd(nc, maps, core_ids=list(range(NCORES)))
    return [(r["x_out"], r["h2_out"], r["aff_out"]) for r in res.results]


CAP = 2 * SEQ // NE
CCAP = 2 * CTX // NE
NSLOT = CAP + CCAP
ROWW = D
GW = 16
NBIS = 30
OOB0 = 4096


def build_kd():
    nc = bass.Bass("TRN2", target_bir_lowering=False)
    aff_l = nc.dram_tensor("aff_l", [128, 2, 64], F32, kind="ExternalInput").ap()
    aff_c = nc.dram_tensor("aff_c", [128, 2, 2], F32, kind="ExternalInput").ap()
    h2l = nc.dram_tensor("h2l", [SEQ, D], BF16, kind="ExternalInput").ap()
    h2c = nc.dram_tensor("h2c", [CTX, D], BF16, kind="ExternalInput").ap()
    wg = nc.dram_tensor("wg", [2, D, FF], F32, kind="ExternalInput").ap()
    wu = nc.dram_tensor("wu", [2, D, FF], F32, kind="ExternalInput").ap()
    wd = nc.dram_tensor("wd", [2, FF, D], F32, kind="ExternalInput").ap()
    xg = [nc.dram_tensor("xg%d" % i, [NSLOT, ROWW], BF16, kind="Internal").ap() for i in range(2)]
    out_e = nc.dram_tensor("out_e", [2, NSLOT, D], F32, kind="ExternalOutput").ap()
    pos_l = nc.dram_tensor("pos_l", [128, 2, 64], I32, kind="ExternalOutput").ap()
    pos_c = nc.dram_tensor("pos_c", [128, 2, 2], I32, kind="ExternalOutput").ap()
    P = Prog(nc)
    identf = make_ident(P, "identf", F32)
    identb = P.sb("identb", [128, 128], BF16)
    P.op("vector", lambda e: e.tensor_copy(out=identb[:], in_=identf[:]), reads=[identf], writes=[identb])
    ones = P.sb("ones_f", [128, 128], F32)
    P.op("gpsimd", lambda e: e.memset(ones[:], 1.0), writes=[ones])
    SU = P.sb("SU", [128, 128], F32)
    P.op("gpsimd", lambda e: e.memset(SU[:], 1.0), writes=[SU])
    P.op("gpsimd", lambda e: e.affine_select(out=SU[:], in_=SU[:], pattern=[[1, 128]], compare_op=ALU.is_gt, fill=0.0,
                                             base=0, channel_multiplier=-1), reads=[SU], writes=[SU])
    oobv = P.sb("oobv", [128, 1], F32)
    P.op("gpsimd", lambda e: e.iota(oobv[:], pattern=[[0, 1]], base=OOB0, channel_multiplier=1,
                                    allow_small_or_imprecise_dtypes=True), writes=[oobv])
    zeros = P.sb("zeros", [128, 64], F32)
    P.op("gpsimd", lambda e: e.memset(zeros[:], 0.0), writes=[zeros])
    affl = P.sb("affl", [128, 2, 64], F32)
    affc = P.sb("affc", [128, 2, 2], F32)
    P.dma("sync", affl[:], aff_l, writes=[affl])
    P.dma("sync", affc[:], aff_c, writes=[affc])
    pairs = [(affl, 0, 64, 0, CAP), (affl, 1, 64, 0, CAP), (affc, 0, 2, CAP, CCAP), (affc, 1, 2, CAP, CCAP)]
    kvec = P.sb("kvec", [128, 4], F32)
    P.op("gpsimd", lambda e: e.memset(kvec[:, 0:2], CAP - 0.5), writes=[kvec])
    P.op("gpsimd", lambda e: e.memset(kvec[:, 2:4], CCAP - 0.5), reads=[kvec], writes=[kvec])
    lo = P.sb("lo", [128, 4], F32)
    P.op("gpsimd", lambda e: e.memset(lo[:], 0.0), writes=[lo])
    mid = P.sb("mid", [128, 4], F32)
    pc = P.sb("pc", [128, 4], F32)
    ge = P.sb("ge", [128, 4], F32)
    junk = P.sb("junkb", [128, 64], F32)
    sm_ps = P.ps("sm_ps", [128, 512], F32)
    cnt_ps = PV(sm_ps, 0, 4)
    for k in range(NBIS):
        step = 2.0 ** -(k + 1)
        P.op("vector", lambda e, step=step: e.tensor_scalar(out=mid[:], in0=lo[:], scalar1=step, scalar2=None, op0=ALU.add),
             reads=[lo], writes=[mid])
        for j, (a, ee, n, base, cap) in enumerate(pairs):
            P.op("vector", lambda e, a=a, ee=ee, n=n, j=j: e.tensor_scalar(
                out=junk[:, :n], in0=a[:, ee, :], scalar1=mid[:, j:j + 1], scalar2=0.0, op0=ALU.is_ge, op1=ALU.add,
                accum_out=pc[:, j:j + 1]), reads=[a, mid], writes=[junk, (pc, j)])
        P.op("tensor", lambda e: e.matmul(cnt_ps[:], lhsT=ones[:], rhs=pc[:], start=True, stop=True),
             reads=[ones] + [(pc, j) for j in range(4)], writes=[cnt_ps])
        P.op("vector", lambda e: e.tensor_tensor(out=ge[:], in0=cnt_ps[:], in1=kvec[:], op=ALU.is_ge),
             reads=[cnt_ps, kvec], writes=[ge])
        P.op("vector", lambda e, step=step: e.scalar_tensor_tensor(out=lo[:], in0=ge[:], scalar=step, in1=lo[:],
                                                                    op0=ALU.mult, op1=ALU.add),
             reads=[ge, lo], writes=[lo])
    M = P.sb("Msel", [128, 64], F32)
    Tsb = P.sb("Tsb", [128, 64], F32)
    cum = P.sb("cum", [128, 64], F32)
    pos = P.sb("posf", [128, 64], F32)
    sel = P.sb("sel", [128, 64], F32)
    posl = P.sb("posl", [128, 2, 64], I32)
    posc = P.sb("posc", [128, 2, 2], I32)
    wi_ps, t_ps = PV(sm_ps, 64, 64), PV(sm_ps, 128, 64)
    for j, (a, ee, n, base, cap) in enumerate(pairs):
        dst = (posl if n == 64 else posc)
        P.op("vector", lambda e, a=a, ee=ee, n=n, j=j: e.tensor_scalar(
            out=M[:, :n], in0=a[:, ee, :], scalar1=lo[:, j:j + 1], scalar2=None, op0=ALU.is_ge), reads=[a, lo], writes=[M])
        P.op("tensor", lambda e, n=n: e.matmul(wi_ps[:, :n], lhsT=SU[:], rhs=M[:, :n], start=True, stop=True),
             reads=[SU, M], writes=[sm_ps])
        P.op("tensor", lambda e, n=n: e.matmul(t_ps[:, :n], lhsT=ones[:], rhs=M[:, :n], start=True, stop=True),
             reads=[ones, M], writes=[sm_ps])
        P.op("vector", lambda e, n=n: e.tensor_copy(out=Tsb[:, :n], in_=t_ps[:, :n]), reads=[sm_ps], writes=[Tsb])
        P.op("vector", lambda e, n=n: e.tensor_tensor_scan(out=cum[:, :n], data0=Tsb[:, :n], data1=zeros[:, :n], initial=0.0,
                                                            op0=ALU.add, op1=ALU.add), reads=[Tsb, zeros], writes=[cum])
        P.op("vector", lambda e, n=n: e.tensor_tensor(out=pos[:, :n], in0=wi_ps[:, :n], in1=cum[:, :n], op=ALU.add),
             reads=[sm_ps, cum], writes=[pos])
        P.op("vector", lambda e, n=n, base=base: e.scalar_tensor_tensor(out=pos[:, :n], in0=pos[:, :n], scalar=float(base), in1=Tsb[:, :n],
                                                                         op0=ALU.add, op1=ALU.subtract), reads=[pos, Tsb], writes=[pos])
        P.op("vector", lambda e, n=n, base=base, cap=cap: e.scalar_tensor_tensor(
            out=sel[:, :n], in0=pos[:, :n], scalar=float(base + cap) - 0.5, in1=M[:, :n], op0=ALU.is_lt, op1=ALU.mult),
            reads=[pos, M], writes=[sel])
        P.op("vector", lambda e, n=n: e.tensor_scalar(out=pos[:, :n], in0=pos[:, :n], scalar1=oobv[:, 0:1], scalar2=None, op0=ALU.subtract),
             reads=[pos, oobv], writes=[pos])
        P.op("vector", lambda e, n=n: e.tensor_tensor(out=pos[:, :n], in0=pos[:, :n], in1=sel[:, :n], op=ALU.mult),
             reads=[pos, sel], writes=[pos])
        P.op("vector", lambda e, n=n: e.tensor_scalar(out=pos[:, :n], in0=pos[:, :n], scalar1=oobv[:, 0:1], scalar2=None, op0=ALU.add),
             reads=[pos, oobv], writes=[pos])
        P.op("vector", lambda e, n=n, dst=dst, ee=ee: e.tensor_copy(out=dst[:, ee, :], in_=pos[:, :n]), reads=[pos], writes=[dst])
    P.dma("sync", pos_l, posl[:], reads=[posl])
    P.dma("sync", pos_c, posc[:], reads=[posc])
    rbs = [P.sb("rowbuf%d" % i, [128, ROWW], BF16) for i in range(4)]
    rbn = [0]

    def emit_scatter(n, ee):
        rb = rbs[rbn[0] % 4]
        rbn[0] += 1
        if n < 64:
            src, pp, nn = h2l[n * 128:(n + 1) * 128, :], posl, n
        else:
            src, pp, nn = h2c[(n - 64) * 128:(n - 63) * 128, :], posc, n - 64
        P.dma("sync", rb[:, :D], src, writes=[rb])
        P.dma_fn("gpsimd", lambda e: e.indirect_dma_start(
            out=xg[ee], out_offset=bass.IndirectOffsetOnAxis(ap=pp[:, ee, nn:nn + 1], axis=0),
            in_=rb[:, :], in_offset=None, bounds_check=P.reg(e, NSLOT - 1), oob_is_err=False),
            reads=[rb, pp], writes=[("xg", ee, n)])

    for n in range(66):
        emit_scatter(n, 0)
    pending = list(range(66))
    XgT = P.sb("XgT", [128, 16, NSLOT], BF16)
    hidT = P.sb("hidT", [128, 12, NSLOT], BF16)
    xts = [P.sb("xgt%d" % i, [128, ROWW], BF16) for i in range(2)]
    tpb = P.ps("tpb", [128, 8, 128], BF16)
    g_ps = [P.ps("g_ps%d" % i, [128, 512], F32) for i in range(2)]
    u_ps = [P.ps("u_ps%d" % i, [128, 512], F32) for i in range(2)]
    o_ps = [P.ps("o_ps%d" % i, [128, 512], F32) for i in range(2)]
    wgc = [P.sb("wgc%d" % i, [128, 16, 256], BF16) for i in range(2)]
    wuc = [P.sb("wuc%d" % i, [128, 16, 256], BF16) for i in range(2)]
    wdc = [P.sb("wdc%d" % i, [128, 12, 512], BF16) for i in range(2)]
    sgs = [P.sb("sgs%d" % i, [128, 512], F32) for i in range(2)]
    ost = [P.sb("ost%d" % i, [128, 512], F32) for i in range(2)]
    sblocks = [(0, 512), (512, 512), (1024, 32)]
    ih = io = iw = 0
    for ee in range(2):
        scat = [("xg", ee, n) for n in range(66)]
        for st in range(9):
            rows = 128 if st < 8 else CCAP
            xt = xts[st % 2]
            P.dma("sync", xt[:rows, :], xg[ee][st * 128:st * 128 + rows, :], reads=scat, writes=[xt])
            scat = []
            for half in range(2):
                for j in range(8):
                    kc = half * 8 + j
                    P.op("tensor", lambda e, xt=xt, rows=rows, j=j, kc=kc: e.transpose(
                        out=tpb[:, j, :rows], in_=xt[:rows, kc * 128:(kc + 1) * 128], identity=identb[:rows, :rows]),
                        reads=[xt, identb], writes=[tpb])
                eng = "scalar" if half == 0 else "vector"
                if eng == "scalar":
                    P.op("scalar", lambda e, rows=rows, half=half, st=st: e.activation(
                        out=XgT[:, half * 8:(half + 1) * 8, st * 128:st * 128 + rows], in_=tpb[:, :, :rows], func=AF.Copy),
                        reads=[tpb], writes=[(XgT, st)])
                else:
                    P.op("vector", lambda e, rows=rows, half=half, st=st: e.tensor_copy(
                        out=XgT[:, half * 8:(half + 1) * 8, st * 128:st * 128 + rows], in_=tpb[:, :, :rows]),
                        reads=[tpb], writes=[(XgT, st)])
        xkeys = [(XgT, st) for st in range(9)]
        for hc in range(6):
            wg_c, wu_c = wgc[iw % 2], wuc[iw % 2]
            iw += 1
            hs = slice(hc * 256, (hc + 1) * 256)
            for q in range(4):
                P.dma("gpsimd", wg_c[:, 4 * q:4 * q + 4, :], wg[ee].rearrange("(kc p) f -> p kc f", p=128)[:, 4 * q:4 * q + 4, hs],
                      writes=[(wg_c, q)])
                P.dma("gpsimd", wu_c[:, 4 * q:4 * q + 4, :], wu[ee].rearrange("(kc p) f -> p kc f", p=128)[:, 4 * q:4 * q + 4, hs],
                      writes=[(wu_c, q)])
            if ee == 0:
                for _ in range(11):
                    emit_scatter(pending.pop(0), 1)
            for fl in range(2):
                fc = hc * 2 + fl
                fs = slice(fl * 128, (fl + 1) * 128)
                for (s0, n) in sblocks:
                    gp, up, sg = g_ps[ih % 2], u_ps[ih % 2], sgs[ih % 2]
                    ih += 1
                    for kc in range(16):
                        P.op("tensor", lambda e, gp=gp, wg_c=wg_c, kc=kc, fs=fs, s0=s0, n=n: e.matmul(
                            gp[:, :n], lhsT=wg_c[:, kc, fs], rhs=XgT[:, kc, s0:s0 + n], start=(kc == 0), stop=(kc == 15)),
                            reads=[(wg_c, kc // 4)] + xkeys, writes=[gp])
                    for kc in range(16):
                        P.op("tensor", lambda e, up=up, wu_c=wu_c, kc=kc, fs=fs, s0=s0, n=n: e.matmul(
                            up[:, :n], lhsT=wu_c[:, kc, fs], rhs=XgT[:, kc, s0:s0 + n], start=(kc == 0), stop=(kc == 15)),
                            reads=[(wu_c, kc // 4)] + xkeys, writes=[up])
                    P.op("scalar", lambda e, gp=gp, sg=sg, n=n: e.activation(out=sg[:, :n], in_=gp[:, :n], func=AF.Silu),
                         reads=[gp], writes=[sg])
                    P.op("vector", lambda e, up=up, sg=sg, n=n, fc=fc, s0=s0: e.tensor_tensor(
                        out=hidT[:, fc, s0:s0 + n], in0=up[:, :n], in1=sg[:, :n], op=ALU.mult),
                        reads=[up, sg], writes=[(hidT, fc)])
        hkeys = [(hidT, fc) for fc in range(12)]
        for cb in range(4):
            wd_c = wdc[(ee * 4 + cb) % 2]
            cs = slice(cb * 512, (cb + 1) * 512)
            for q in range(3):
                P.dma("gpsimd", wd_c[:, 4 * q:4 * q + 4, :], wd[ee].rearrange("(fc p) n -> p fc n", p=128)[:, 4 * q:4 * q + 4, cs],
                      writes=[(wd_c, q)])
            for st in range(9):
                rows = 128 if st < 8 else CCAP
                op_, os_ = o_ps[io % 2], ost[io % 2]
                io += 1
                for fc in range(12):
                    P.op("tensor", lambda e, op_=op_, wd_c=wd_c, fc=fc, st=st, rows=rows: e.matmul(
                        op_[:rows, :], lhsT=hidT[:, fc, st * 128:st * 128 + rows], rhs=wd_c[:, fc, :],
                        start=(fc == 0), stop=(fc == 11)), reads=[(wd_c, fc // 4)] + hkeys, writes=[op_])
                P.op("scalar", lambda e, op_=op_, os_=os_, rows=rows, ee=ee, st=st: e.activation(
                    out=os_[:rows, :], in_=op_[:rows, :], func=AF.Copy),
                    reads=[op_], writes=[os_])
                P.dma("sync", out_e[ee, st * 128:st * 128 + rows, cs], os_[:rows, :], reads=[os_])
    P.finish()
    return nc


def run_kd(inputs, l, aff_lat, aff_ctx, h2_lat, h2_ctx, nc=None):
    nc = nc or build_kd()
    maps = []
    for i in range(NCORES):
        es = slice(2 * i, 2 * i + 2)
        maps.append({
            "aff_l": np.ascontiguousarray(aff_lat[:, es].reshape(64, 128, 2).transpose(1, 2, 0)),
            "aff_c": np.ascontiguousarray(aff_ctx[:, es].reshape(2, 128, 2).transpose(1, 2, 0)),
            "h2l": h2_lat, "h2c": h2_ctx,
            "wg": np.ascontiguousarray(inputs["w_gate"][l, es]), "wu": np.ascontiguousarray(inputs["w_up"][l, es]),
            "wd": np.ascontiguousarray(inputs["w_down"][l, es]),
        })
    res = run_bass_kernel_spmd(nc, maps, core_ids=list(range(NCORES)))
    return [(r["out_e"], r["pos_l"], r["pos_c"]) for r in res.results]


def build_kf():
    nc = bass.Bass("TRN2", target_bir_lowering=False)
    x_in = nc.dram_tensor("x_in", [NT * 128, D], F32, kind="ExternalInput").ap()
    fnv = nc.dram_tensor("fnv", [1, D], F32, kind="ExternalInput").ap()
    y = nc.dram_tensor("y", [(NT - 1) * 128, D], F32, kind="ExternalOutput").ap()
    P = Prog(nc)
    comb = Combiner(P, *declare_combine_inputs(nc))
    fn_s = P.sb("fn_s", [128, D], F32)
    P.dma("sync", fn_s[:], fnv.partition_broadcast(128), writes=[fn_s])
    xts = [P.sb("xt%d" % i, [128, D], F32) for i in range(2)]
    junk = P.sb("junk", [128, D], BF16)
    ss = P.sb("ss", [128, 1], F32)
    for t in range(NT - 1):
        xt = xts[t % 2]
        rows = slice(t * 128, (t + 1) * 128)
        P.dma("sync", xt[:], x_in[rows, :], writes=[xt])
        comb.emit(t, xt)
        P.op("scalar", lambda e, xt=xt: e.activation(out=junk[:], in_=xt[:], func=AF.Square, accum_out=ss[:]),
             reads=[xt], writes=[junk, ss])
        emit_rsqrt(P, ss[:], ss[:], 1.0 / D, ss, ss)
        P.op("vector", lambda e, xt=xt: e.scalar_tensor_tensor(out=xt[:], in0=xt[:], scalar=ss[:, 0:1], in1=fn_s[:],
                                                                op0=ALU.mult, op1=ALU.mult), reads=[xt, ss, fn_s], writes=[xt])
        P.dma("sync", y[rows, :], xt[:], reads=[xt])
    P.finish()
    return nc


def run_kf(inputs, x_lat, x_ctx, comb_maps, nc=None):
    nc = nc or build_kf()
    fnv = np.ascontiguousarray(inputs["final_norm"][None]).astype(np.float32)
    maps = []
    for i in range(NCORES):
        m = {"x_in": core_tokens(x_lat, x_ctx, i), "fnv": fnv}
        m.update(comb_maps[i])
        maps.append(m)
    res = run_bass_kernel_spmd(nc, maps, core_ids=list(range(NCORES)))
    return np.concatenate([r["y"] for r in res.results], axis=0)


_NC = {}


def _nc(name, fn):
    if name not in _NC:
        _NC[name] = fn()
    return _NC[name]


def kernel(**inputs):
    inputs = {k: np.asarray(v) for k, v in inputs.items()}
    mods = run_kmod(inputs)
    x_lat = np.ascontiguousarray(inputs["x"][0])
    x_ctx = np.ascontiguousarray(inputs["ctx"][0])
    comb = None
    for l in range(DEPTH):
        if comb is None:
            ka = run_ka(inputs, mods, l, x_lat, x_ctx, nc=_nc("ka0", lambda: build_ka(False)))
        else:
            ka = run_ka(inputs, mods, l, x_lat, x_ctx, nc=_nc("ka1", lambda: build_ka(True)), comb_maps=comb)
            x_lat, x_ctx = gather_tokens([o[2] for o in ka])
        bf_lat, bf_ctx = gather_tokens([o[0] for o in ka])
        f_lat, f_ctx = gather_tokens([o[1] for o in ka])
        attnT, og = run_kb(bf_lat, bf_ctx, f_lat, f_ctx, nc=_nc("kb", build_kb))
        kc = run_kc(inputs, mods, l, attnT, og, f_lat[:, 1024:2048], f_ctx[:, 1024:2048], x_lat, x_ctx,
                    nc=_nc("kc", build_kc))
        x_lat, x_ctx = gather_tokens([o[0] for o in kc])
        h_lat, h_ctx = gather_tokens([o[1] for o in kc])
        a_lat, a_ctx = gather_tokens([o[2] for o in kc])
        kd = run_kd(inputs, l, a_lat, a_ctx, h_lat, h_ctx, nc=_nc("kd", build_kd))
        comb = combine_maps(mods, l, kd, a_lat, a_ctx)
    y = run_kf(inputs, x_lat, x_ctx, comb, nc=_nc("kf", build_kf))
    return np.ascontiguousarray(y[None]).astype(np.float32)
```
